# Optimizing a Trainium2 kernel written in Bass

```python
import math
import jax
import jax.numpy as jnp
from jax import lax
import numpy as np


D_MODEL = 2048
BATCH = 16
SEQ = 2048
DEPTH = 1

GRID_W = 64
CTX_LEN = 256
HY_WIDTH = D_MODEL // 2
RW_WIDTH = D_MODEL - HY_WIDTH
HY_GROUP = 64
RW_HEAD = 64
RW_HEADS = RW_WIDTH // RW_HEAD
PROJ_WIDTH = 3 * HY_WIDTH + 3 * RW_WIDTH
FILTER_BANDS = 16
FILTER_EMB = 2 * FILTER_BANDS + 1
FILTER_WIDTH = 64
FILTER_TARGET = 1e-2
FAST_DECAY_PCT = 0.3
SLOW_DECAY_PCT = 1.5
DECAY_LORA = 64
ICLR_LORA = 64
GATE_LORA = 128
N_EXPERTS = 32
TOP_K = 4
D_FF = D_MODEL
SWIGLU_LIMIT = 7.0
SWIGLU_ALPHA = 1.702
EXPERT_BLOCK = 128
NORM_EPS = 1e-6
LNX_EPS = 64e-5

kernel_name = 'hyena_rwkv7_moe_prefix_dit_block'


def rmsnorm(x, g):
    xf = x.astype(jnp.float32)
    y = xf * lax.rsqrt(jnp.mean(xf * xf, axis=-1, keepdims=True) + NORM_EPS)
    return (y * g).astype(x.dtype)


def modulate(h, shift, scale):
    return h * (1 + scale) + shift


def short_conv(z, w, b):
    zp = jnp.pad(z, ((0, 0), (1, 1), (0, 0)))
    return zp[:, :-2] * w[0] + zp[:, 1:-1] * w[1] + zp[:, 2:] * w[2] + b


def group_rmsnorm(y, g, group):
    b_, l_, ch = y.shape
    yf = y.astype(jnp.float32).reshape(b_, l_, ch // group, group)
    yf = yf * lax.rsqrt(jnp.mean(yf * yf, axis=-1, keepdims=True) + NORM_EPS)
    return (yf.reshape(b_, l_, ch) * g).astype(y.dtype)


def both_dirs(a):
    return jnp.stack([a, a[:, ::-1]])


def orient(a):
    return jnp.stack([a[0], a[1][:, ::-1]])


def hyena_filter(length, w1, b1, w2, b2, w3, b3, freq, w4):
    t = jnp.linspace(0.0, 1.0, length, dtype=jnp.float32)[:, None]
    ang = (2.0 * math.pi / length) * jnp.arange(length, dtype=jnp.float32)[:, None]
    bands = jnp.linspace(1e-4, FILTER_BANDS - 1, FILTER_BANDS, dtype=jnp.float32)[None, :]
    feats = jnp.concatenate([t, jnp.cos(bands * ang), -jnp.sin(bands * ang)], axis=-1)
    h = jnp.sin(freq * (feats @ w1 + b1))
    h = jnp.sin(freq * (h @ w2 + b2))
    h = jnp.sin(freq * (h @ w3 + b3))
    h = (h @ w4).astype(jnp.float32)
    deltas = jnp.abs(jnp.linspace(math.log(FILTER_TARGET) / SLOW_DECAY_PCT,
                                  math.log(FILTER_TARGET) / FAST_DECAY_PCT, HY_WIDTH, dtype=jnp.float32))
    h = h * jnp.exp(-t * jnp.tile(deltas, 2))
    return h[:, :HY_WIDTH], h[:, HY_WIDTH:]


def long_conv(u, h_fwd, h_bwd):
    length, ch = h_fwd.shape
    taps = jnp.concatenate([h_fwd, jnp.zeros((1, ch), h_fwd.dtype), h_bwd[:0:-1]], axis=0)
    u_f = jnp.fft.rfft(u.astype(jnp.float32), n=2 * length, axis=1)
    t_f = jnp.fft.rfft(taps, n=2 * length, axis=0)
    return jnp.fft.irfft(u_f * t_f[None], n=2 * length, axis=1)[:, :length].astype(u.dtype)


def hyena_mixer(z, w1, b1, w2, b2, w3, b3, freq, w4, bias, norm_g):
    x0, x1, v = jnp.split(z, 3, axis=-1)
    h_fwd, h_bwd = hyena_filter(z.shape[1], w1, b1, w2, b2, w3, b3, freq, w4)
    u = v * x1
    y = x0 * (long_conv(u, h_fwd, h_bwd) + bias * u)
    return group_rmsnorm(y, norm_g, HY_GROUP)


def rwkv7_prep(z, h, w0, w1, w2, a0, a1, a2, k_k, k_a, r_k):
    b_, l_, _ = z.shape
    r, k, v = jnp.split(z, 3, axis=-1)
    heads = lambda t: t.reshape(*t.shape[:-1], RW_HEADS, RW_HEAD)
    kk = heads((k * k_k).astype(jnp.float32))
    kk = (kk / jnp.maximum(jnp.linalg.norm(kk, axis=-1, keepdims=True), 1e-12)).reshape(b_, l_, RW_WIDTH)
    w_log = -jax.nn.softplus(-(w0[:, None, None, :] + jnp.einsum(
        'nblr,nrc->nblc', jnp.tanh(jnp.einsum('bld,ndr->nblr', h, w1)), w2))) - 0.5
    decay = jnp.exp(-jnp.exp(w_log.astype(jnp.float32)))
    a = jax.nn.sigmoid(a0[:, None, None, :] + jnp.einsum(
        'nblr,nrc->nblc', jnp.einsum('bld,ndr->nblr', h, a1), a2))
    k_dir = k * (1 + (a - 1) * k_a)
    bonus = jnp.sum(heads(r * k_dir) * r_k, axis=-1, keepdims=True) * heads(v)
    bonus = jnp.sum(bonus, axis=0).reshape(b_, l_, RW_WIDTH)
    scan_in = (both_dirs(r), orient(decay), orient(k_dir), both_dirs(v), both_dirs(-kk), orient(kk * a))
    return scan_in, bonus


def rwkv7_scan(r, decay, k, v, aa, bb, s0):
    out_shape = r.shape

    def tm(t):
        return jnp.moveaxis(t.astype(jnp.float32).reshape(*t.shape[:3], RW_HEADS, RW_HEAD), 2, 0)

    def step(s, inp):
        r_t, w_t, k_t, v_t, a_t, b_t = inp
        sa = jnp.einsum('nbhvk,nbhk->nbhv', s, a_t)
        s = s * w_t[..., None, :] + sa[..., :, None] * b_t[..., None, :] + v_t[..., :, None] * k_t[..., None, :]
        return s, jnp.einsum('nbhvk,nbhk->nbhv', s, r_t)

    s_fin, ys = lax.scan(step, s0, (tm(r), tm(decay), tm(k), tm(v), tm(aa), tm(bb)))
    return jnp.moveaxis(ys, 0, 2).reshape(out_shape), s_fin


def rwkv7_out(ys, bonus, h, g1, g2, lnx_g, lnx_b):
    ys = orient(ys)
    b_, l_, _ = h.shape
    y = (ys[0] + ys[1]).reshape(b_, l_, RW_HEADS, RW_HEAD)
    mu = jnp.mean(y, axis=-1, keepdims=True)
    var = jnp.mean(jnp.square(y - mu), axis=-1, keepdims=True)
    y = ((y - mu) * lax.rsqrt(var + LNX_EPS)).reshape(b_, l_, RW_WIDTH) * lnx_g + lnx_b
    gate = jax.nn.sigmoid(h @ g1) @ g2
    return ((y + bonus) * gate).astype(h.dtype)


def token_mixer(hx, hc, in_w, conv_w, conv_b, hy, rw_pre, rw_post, out_w, need_ctx):
    n_hy = 3 * HY_WIDTH
    zx = short_conv(hx @ in_w, conv_w, conv_b)
    zc = short_conv(hc @ in_w, conv_w, conv_b)
    s0 = jnp.zeros((2, hc.shape[0], RW_HEADS, RW_HEAD, RW_HEAD), jnp.float32)
    ins_c, bonus_c = rwkv7_prep(zc[..., n_hy:], hc, *rw_pre)
    ys_c, s_ctx = rwkv7_scan(*ins_c, s0)
    ins_x, bonus_x = rwkv7_prep(zx[..., n_hy:], hx, *rw_pre)
    ys_x, _ = rwkv7_scan(*ins_x, s_ctx)
    y_x = jnp.concatenate([hyena_mixer(zx[..., :n_hy], *hy),
                           rwkv7_out(ys_x, bonus_x, hx, *rw_post)], axis=-1) @ out_w
    if not need_ctx:
        return y_x, None
    y_c = jnp.concatenate([hyena_mixer(zc[..., :n_hy], *hy),
                           rwkv7_out(ys_c, bonus_c, hc, *rw_post)], axis=-1) @ out_w
    return y_x, y_c


def moe_ffn(h, router_w, router_b, w_gate, b_gate, w_up, b_up, w_down, b_down):
    b_, l_, d_ = h.shape
    t = h.reshape(b_ * l_, d_)
    n_tok = t.shape[0]
    logits = (t @ router_w + router_b).astype(jnp.float32)
    top_logits, top_idx = lax.top_k(logits, TOP_K)
    gates = jax.nn.softmax(top_logits, axis=-1).astype(h.dtype)
    n_asg = n_tok * TOP_K
    e_flat = top_idx.reshape(n_asg)
    order = jnp.argsort(e_flat)
    e_sorted = e_flat[order]
    tok_sorted = (order // TOP_K).astype(jnp.int32)
    gate_sorted = gates.reshape(n_asg)[order]
    counts = jnp.bincount(e_flat, length=N_EXPERTS)
    padded = (counts + EXPERT_BLOCK - 1) // EXPERT_BLOCK * EXPERT_BLOCK
    pad_end = jnp.cumsum(padded)
    pad_start = pad_end - padded
    raw_start = jnp.cumsum(counts) - counts
    dest = pad_start[e_sorted] + jnp.arange(n_asg) - raw_start[e_sorted]
    n_blocks = -(-n_asg // EXPERT_BLOCK) + N_EXPERTS
    n_rows = n_blocks * EXPERT_BLOCK
    row_tok = jnp.zeros((n_rows,), jnp.int32).at[dest].set(tok_sorted)
    row_gate = jnp.zeros((n_rows,), h.dtype).at[dest].set(gate_sorted)
    block_expert = jnp.minimum(
        jnp.searchsorted(pad_end, jnp.arange(n_blocks) * EXPERT_BLOCK, side='right'), N_EXPERTS - 1)

    def expert_block(args):
        rows, e = args
        xb = t[rows]
        g = jnp.minimum(xb @ w_gate[e] + b_gate[e], SWIGLU_LIMIT)
        u = jnp.clip(xb @ w_up[e] + b_up[e], -SWIGLU_LIMIT, SWIGLU_LIMIT)
        return ((u + 1) * (g * jax.nn.sigmoid(SWIGLU_ALPHA * g))) @ w_down[e] + b_down[e]

    y = lax.map(expert_block, (row_tok.reshape(n_blocks, EXPERT_BLOCK), block_expert))
    y = y.reshape(n_rows, d_) * row_gate[:, None]
    return jax.ops.segment_sum(y, row_tok, num_segments=n_tok).reshape(b_, l_, d_)


def setup_inputs(seed: int = 0) -> dict:
    key = jax.random.key(seed)
    ks = iter(jax.random.split(key, 48))

    def nrm(shape, scale):
        return scale * jax.random.normal(next(ks), shape, jnp.float32)

    nl, d, e, f = DEPTH, D_MODEL, N_EXPERTS, D_FF
    conv_base = jnp.array([0.25, 0.5, 0.25], jnp.float32)[None, :, None]
    return {
        'x': nrm((BATCH, SEQ, d), 1.0),
        'c': nrm((BATCH, d), 1.0),
        'ctx': nrm((BATCH, CTX_LEN, d), 1.0),
        'c_ctx': nrm((d,), 1.0),
        'ada_w': nrm((nl, d, 6 * d), 0.5 * d ** -0.5),
        'ada_b': nrm((nl, 6 * d), 0.02),
        'norm1_g': 1.0 + nrm((nl, d), 0.05),
        'norm2_g': 1.0 + nrm((nl, d), 0.05),
        'in_w': nrm((nl, d, PROJ_WIDTH), d ** -0.5),
        'conv_w': conv_base + nrm((nl, 3, PROJ_WIDTH), 0.2),
        'conv_b': nrm((nl, PROJ_WIDTH), 0.02),
        'hy_w1': nrm((nl, FILTER_EMB, FILTER_WIDTH), FILTER_EMB ** -0.5),
        'hy_b1': nrm((nl, FILTER_WIDTH), 0.1),
        'hy_w2': nrm((nl, FILTER_WIDTH, FILTER_WIDTH), FILTER_WIDTH ** -0.5),
        'hy_b2': nrm((nl, FILTER_WIDTH), 0.1),
        'hy_w3': nrm((nl, FILTER_WIDTH, FILTER_WIDTH), FILTER_WIDTH ** -0.5),
        'hy_b3': nrm((nl, FILTER_WIDTH), 0.1),
        'hy_freq': 1.0 + nrm((nl, FILTER_WIDTH), 0.1),
        'hy_w4': nrm((nl, FILTER_WIDTH, 2 * HY_WIDTH), FILTER_WIDTH ** -0.5),
        'hy_bias': nrm((nl, HY_WIDTH), 0.5),
        'hy_norm_g': 1.0 + nrm((nl, HY_WIDTH), 0.05),
        'rw_w0': -1.0 + nrm((nl, 2, RW_WIDTH), 0.5),
        'rw_w1': nrm((nl, 2, d, DECAY_LORA), d ** -0.5),
        'rw_w2': nrm((nl, 2, DECAY_LORA, RW_WIDTH), 0.5 * DECAY_LORA ** -0.5),
        'rw_a0': nrm((nl, 2, RW_WIDTH), 0.5),
        'rw_a1': nrm((nl, 2, d, ICLR_LORA), d ** -0.5),
        'rw_a2': nrm((nl, 2, ICLR_LORA, RW_WIDTH), 0.5 * ICLR_LORA ** -0.5),
        'rw_kk': 0.85 + nrm((nl, RW_WIDTH), 0.05),
        'rw_ka': 1.0 + nrm((nl, RW_WIDTH), 0.05),
        'rw_rk': nrm((nl, RW_HEADS, RW_HEAD), 0.1),
        'rw_g1': nrm((nl, d, GATE_LORA), d ** -0.5),
        'rw_g2': nrm((nl, GATE_LORA, RW_WIDTH), GATE_LORA ** -0.5),
        'rw_lnx_g': 1.0 + nrm((nl, RW_WIDTH), 0.05),
        'rw_lnx_b': nrm((nl, RW_WIDTH), 0.02),
        'out_w': nrm((nl, HY_WIDTH + RW_WIDTH, d), (HY_WIDTH + RW_WIDTH) ** -0.5),
        'router_w': nrm((nl, d, e), d ** -0.5),
        'router_b': nrm((nl, e), 0.01),
        'ex_w_gate': nrm((nl, e, d, f), d ** -0.5),
        'ex_b_gate': nrm((nl, e, f), 0.01),
        'ex_w_up': nrm((nl, e, d, f), d ** -0.5),
        'ex_b_up': nrm((nl, e, f), 0.01),
        'ex_w_down': nrm((nl, e, f, d), f ** -0.5),
        'ex_b_down': nrm((nl, e, d), 0.01),
        'final_g': 1.0 + nrm((d,), 0.05),
    }


def reference(x, c, ctx, c_ctx, ada_w, ada_b, norm1_g, norm2_g, in_w, conv_w, conv_b,
              hy_w1, hy_b1, hy_w2, hy_b2, hy_w3, hy_b3, hy_freq, hy_w4, hy_bias, hy_norm_g,
              rw_w0, rw_w1, rw_w2, rw_a0, rw_a1, rw_a2, rw_kk, rw_ka, rw_rk,
              rw_g1, rw_g2, rw_lnx_g, rw_lnx_b, out_w,
              router_w, router_b, ex_w_gate, ex_b_gate, ex_w_up, ex_b_up, ex_w_down, ex_b_down,
              final_g):
    cond_x = jax.nn.silu(c)[:, None, :]
    cond_c = jax.nn.silu(c_ctx)[None, None, :]
    for i in range(DEPTH):
        need_ctx = i < DEPTH - 1
        mod_x = jnp.split(cond_x @ ada_w[i] + ada_b[i], 6, axis=-1)
        mod_c = jnp.split(cond_c @ ada_w[i] + ada_b[i], 6, axis=-1)
        hx = modulate(rmsnorm(x, norm1_g[i]), mod_x[0], mod_x[1])
        hc = modulate(rmsnorm(ctx, norm1_g[i]), mod_c[0], mod_c[1])
        hy = (hy_w1[i], hy_b1[i], hy_w2[i], hy_b2[i], hy_w3[i], hy_b3[i], hy_freq[i], hy_w4[i],
              hy_bias[i], hy_norm_g[i])
        rw_pre = (rw_w0[i], rw_w1[i], rw_w2[i], rw_a0[i], rw_a1[i], rw_a2[i], rw_kk[i], rw_ka[i], rw_rk[i])
        rw_post = (rw_g1[i], rw_g2[i], rw_lnx_g[i], rw_lnx_b[i])
        mix_x, mix_c = token_mixer(hx, hc, in_w[i], conv_w[i], conv_b[i], hy, rw_pre, rw_post,
                                   out_w[i], need_ctx)
        x = x + mod_x[2] * mix_x
        moe_w = (router_w[i], router_b[i], ex_w_gate[i], ex_b_gate[i], ex_w_up[i], ex_b_up[i],
                 ex_w_down[i], ex_b_down[i])
        x = x + mod_x[5] * moe_ffn(modulate(rmsnorm(x, norm2_g[i]), mod_x[3], mod_x[4]), *moe_w)
        if need_ctx:
            ctx = ctx + mod_c[2] * mix_c
            ctx = ctx + mod_c[5] * moe_ffn(modulate(rmsnorm(ctx, norm2_g[i]), mod_c[3], mod_c[4]), *moe_w)
    return rmsnorm(x, final_g)
```

```python
import contextlib
import numpy as np
import ml_dtypes
import concourse.bass as bass
import concourse.mybir as mybir
from concourse.bass_utils import run_bass_kernel_spmd

F32 = mybir.dt.float32
BF16 = mybir.dt.bfloat16
I32 = mybir.dt.int32
ALU = mybir.AluOpType
AF = mybir.ActivationFunctionType
AX = mybir.AxisListType

NCORES = 8
D = 2048
KC = 16
SEQ = 2048
CTX = 256
TB = SEQ + CTX
BPC = 2
EPS = 1e-6


class Sched:
    ENGS = ("pe", "act", "dve", "pool", "sp")

    _uid = [0]

    def __init__(self, nc, stack, ndma=24):
        self.nc = nc
        Sched._uid[0] += 1
        u = "s%d_" % Sched._uid[0]
        self.sem = {e: stack.enter_context(nc.semaphore(u + "pg_" + e)) for e in self.ENGS}
        self.dsem = [stack.enter_context(nc.semaphore(u + "dq%d" % i)) for i in range(ndma)]
        self.dval = [0] * ndma
        self.dnext = {"sp": 0, "pool": 0}
        self.dpool = {"sp": list(range(0, ndma // 2)), "pool": list(range(ndma // 2, ndma))}
        self.cnt = {e: 0 for e in self.ENGS}
        self.prog = {e: [] for e in self.ENGS}
        self.waited = {e: {} for e in self.ENGS}
        self.res = {}

    def _r(self, k):
        r = self.res.get(k)
        if r is None:
            r = self.res[k] = {"w": None, "r": {}}
        return r

    def _deps(self, eng, reads, writes):
        need = {}

        def add(sk, v):
            if sk == "pe" and eng == "pe":
                return
            if need.get(sk, 0) < v:
                need[sk] = v
        for k in reads:
            w = self._r(k)["w"]
            if w:
                add(*w)
        for k in writes:
            r = self._r(k)
            if r["w"]:
                add(*r["w"])
            for sk, v in r["r"].items():
                add(sk, v)
        out = []
        wd = self.waited[eng]
        for sk, v in need.items():
            if wd.get(sk, 0) >= v:
                continue
            wd[sk] = v
            out.append((sk, v))
        return out

    def _mark(self, reads, writes, sk, v):
        for k in reads:
            self._r(k)["r"][sk] = v
        for k in writes:
            r = self._r(k)
            r["w"] = (sk, v)
            r["r"] = {}

    alias = {}

    def _x(self, keys):
        out = []
        for k in keys:
            out.extend(self.alias.get(k, (k,)))
        return out

    def op(self, eng, fn, reads=(), writes=()):
        reads, writes = self._x(reads), self._x(writes)
        deps = self._deps(eng, reads, writes)
        self.cnt[eng] += 1
        n = self.cnt[eng]
        self.prog[eng].append((deps, fn, (eng, 1)))
        self._mark(reads, writes, eng, n)

    def dma(self, fn, reads=(), writes=(), q="sp"):
        reads, writes = self._x(reads), self._x(writes)
        pool = self.dpool[q]
        i = pool[self.dnext[q]]
        self.dnext[q] = (self.dnext[q] + 1) % len(pool)
        sk = ("d", i)
        deps = self._deps(q, reads, writes)
        if self.dval[i] > 0 and self.waited[q].get(sk, 0) < self.dval[i]:
            self.waited[q][sk] = self.dval[i]
            deps.append((sk, self.dval[i]))
        self.dval[i] += 16
        self.prog[q].append((deps, fn, (sk, 16)))
        self._mark(reads, writes, sk, self.dval[i])

    def barrier(self):
        for eng in self.ENGS:
            deps = []
            for e in self.ENGS:
                if e != eng and self.cnt[e] and self.waited[eng].get(e, 0) < self.cnt[e]:
                    deps.append((e, self.cnt[e]))
                    self.waited[eng][e] = self.cnt[e]
            for i, v in enumerate(self.dval):
                if v and self.waited[eng].get(("d", i), 0) < v:
                    deps.append((("d", i), v))
                    self.waited[eng][("d", i)] = v
            if deps:
                self.prog[eng].append((deps, None, None))

    def drain(self, eng="sp"):
        deps = []
        for e in self.ENGS:
            if self.cnt[e] and self.waited[eng].get(e, 0) < self.cnt[e] and e != eng:
                deps.append((e, self.cnt[e]))
        for i, v in enumerate(self.dval):
            if v and self.waited[eng].get(("d", i), 0) < v:
                deps.append((("d", i), v))
        self.prog[eng].append((deps, None, None))

    def flush(self, final=False):
        if final:
            self.barrier()
        with self.nc.Block() as block:
            self.emit(block)
        self.prog = {e: [] for e in self.ENGS}

    def _semh(self, sk):
        return self.sem[sk] if isinstance(sk, str) else self.dsem[sk[1]]

    def emit(self, block):
        def run(eng_handle, name):
            for deps, fn, inc in self.prog[name]:
                for sk, v in deps:
                    eng_handle.wait_ge(self._semh(sk), v)
                if fn is not None:
                    ins = fn(eng_handle)
                    ins.then_inc(self._semh(inc[0]), inc[1])

        @block.sync
        def _(e):
            run(e, "sp")

        @block.tensor
        def _(e):
            run(e, "pe")

        @block.vector
        def _(e):
            run(e, "dve")

        @block.scalar
        def _(e):
            run(e, "act")

        @block.gpsimd
        def _(e):
            run(e, "pool")


PW = 6144
NZ = PW // 128
NLORA = 3


def _fm(v, nch):
    return np.ascontiguousarray(np.asarray(v, np.float32).reshape(nch, 128).T)


class Ctx:
    pass


def declare_io(nc, stage):
    g = Ctx()
    dt = nc.dram_tensor
    inp = lambda name, shape, dtype=F32: dt(name, list(shape), dtype, kind="ExternalInput").ap()
    g.x = inp("x", [BPC, SEQ, D])
    g.ctx = inp("ctx", [BPC, CTX, D])
    g.condT = inp("condT", [128, KC, 3])
    g.ada_w = inp("ada_w", [D, 6 * D])
    g.ada_bT = inp("ada_bT", [128, 6 * KC])
    g.g1T = inp("g1T", [128, KC])
    g.g2T = inp("g2T", [128, KC])
    g.in_w = inp("in_w", [D, PW])
    g.lora_w = inp("lora_w", [D, NLORA * 128])
    g.conv_wT = inp("conv_wT", [128, NZ, 3])
    g.conv_bT = inp("conv_bT", [128, NZ])
    g.final_g = inp("final_g", [1, D])
    g.ident_bf = inp("ident_bf", [128, 128], BF16)
    g.ident_f = inp("ident_f", [128, 128], F32)
    g.out = dt("out", [BPC, SEQ, D], F32, kind="ExternalOutput").ap()
    g.ada_brow = inp("ada_brow", [1, 6 * D])
    g.mod_row = (dt("mod_row_scr", [3, 6 * D], F32, kind="ExternalOutput") if stage == "dbgA" else dt("mod_row_scr", [3, 6 * D], F32)).ap()
    g.zT = dt("zT_scr", [BPC, NZ, 128, TB], F32).ap()
    g.loraT = dt("loraT_scr", [BPC, NLORA, 128, TB], F32).ap()
    if stage == "dbgA":
        g.dbg_mod = dt("dbg_mod", [128, 6 * KC, 3], F32, kind="ExternalOutput").ap()
    if stage in ("dbgC",):
        g.dbg_z = dt("dbg_z", [BPC, NZ, 128, TB], F32, kind="ExternalOutput").ap()
        g.dbg_lora = dt("dbg_lora", [BPC, NLORA, 128, TB], F32, kind="ExternalOutput").ap()
    return g


def phase_ABC(nc, g, stage):
    with contextlib.ExitStack() as st:
        S = g.S
        S.barrier()
        sb = lambda name, shape, dtype: st.enter_context(nc.sbuf_tensor(name, shape, dtype))
        ps = [st.enter_context(nc.psum_tensor("ps%d" % i, [128, 512], F32)) for i in range(6)]
        pbf = [st.enter_context(nc.psum_tensor("pbf%d" % i, [128, 1024], BF16)) for i in range(1)]
        pbf_row = st.enter_context(nc.psum_tensor("pbf_row", [128, 512], F32))
        modT = sb("modT", [128, 6 * KC, 3], F32)
        condT = sb("condT_s", [128, KC, 3], F32)
        adab = sb("adab", [128, 6 * KC], F32)
        g1T = sb("g1T_s", [128, KC], F32)
        sc1 = sb("sc1", [128, KC, 3], F32)
        identb = sb("identb", [128, 128], BF16)
        cw = sb("cw", [128, NZ, 3], F32)
        cb = sb("cb", [128, NZ], F32)
        S.dma(lambda e: e.dma_start(out=condT[:], in_=g.condT[:]), writes=["condT"])
        S.dma(lambda e: e.dma_start(out=adab[:], in_=g.ada_bT[:]), writes=["adab"])
        S.dma(lambda e: e.dma_start(out=g1T[:], in_=g.g1T[:]), writes=["g1T"])
        S.dma(lambda e: e.dma_start(out=identb[:], in_=g.ident_bf[:]), writes=["identb"])
        S.dma(lambda e: e.dma_start(out=cw[:], in_=g.conv_wT[:]), writes=["cw"])
        S.dma(lambda e: e.dma_start(out=cb[:], in_=g.conv_bT[:]), writes=["cb"])
        S.op("act", lambda e: e.activation(out=condT[:], in_=condT[:], func=AF.Silu), reads=["condT"], writes=["condT"])
        with contextlib.ExitStack() as stA:
            aw = [stA.enter_context(nc.sbuf_tensor("aw%d" % i, [128, KC, 512], F32)) for i in range(2)]
            mrow = stA.enter_context(nc.sbuf_tensor("mrow", [3, 6 * D], F32))
            S.dma(lambda e: e.dma_start(out=mrow[:], in_=g.ada_brow[0, :].partition_broadcast(3)), writes=["mrow"])
            ada_v = g.ada_w.rearrange("(kc p) f -> p kc f", p=128)
            for sl in range(24):
                p = sl % 2
                S.dma(lambda e, p=p, sl=sl: e.dma_start(out=aw[p][:], in_=ada_v[:, :, sl * 512:(sl + 1) * 512]),
                      writes=["aw%d" % p])
                for kc in range(KC):
                    S.op("pe", lambda e, p=p, kc=kc: e.matmul(pbf_row[0:3, 0:512], lhsT=condT[:, kc, :], rhs=aw[p][:, kc, :],
                                                              start=(kc == 0), stop=(kc == KC - 1)), reads=["aw%d" % p, "condT"], writes=["prow"])
                S.op("dve", lambda e, sl=sl: e.tensor_tensor(out=mrow[:, sl * 512:(sl + 1) * 512], in0=pbf_row[0:3, 0:512], in1=mrow[:, sl * 512:(sl + 1) * 512], op=ALU.add),
                     reads=["prow", "mrow"], writes=["mrow"])
                for j in range(4):
                    ch = sl * 4 + j
                    pb = ch % 6
                    for kc in range(KC):
                        S.op("pe", lambda e, p=p, j=j, kc=kc, pb=pb: e.matmul(
                            ps[pb][:, 0:3], lhsT=aw[p][:, kc, j * 128:(j + 1) * 128], rhs=condT[:, kc, :],
                            start=(kc == 0), stop=(kc == KC - 1)),
                            reads=["aw%d" % p, "condT"], writes=["ps%d" % pb])
                    S.op("dve", lambda e, ch=ch, pb=pb: e.tensor_scalar(
                        out=modT[:, ch, :], in0=ps[pb][:, 0:3], scalar1=adab[:, ch:ch + 1], scalar2=None, op0=ALU.add),
                        reads=["ps%d" % pb, "adab"], writes=["modT"])
            S.dma(lambda e: e.dma_start(out=g.mod_row[:], in_=mrow[:]), reads=["mrow"], writes=["modrow"])
        S.barrier()
        if stage == "dbgA":
            S.dma(lambda e: e.dma_start(out=g.dbg_mod[:], in_=modT[:]), reads=["modT"])
        for n in range(3):
            S.op("dve", lambda e, n=n: e.scalar_tensor_tensor(
                out=sc1[:, :, n], in0=modT[:, KC:2 * KC, n], scalar=1.0, in1=g1T[:], op0=ALU.add, op1=ALU.mult),
                reads=["modT", "g1T"], writes=["sc1"])
        if stage != "dbgA":
            hT = sb("hT", [128, KC, TB], BF16)
            xt = [sb("xt%d" % i, [128, D], F32) for i in range(2)]
            xn = [sb("xn%d" % i, [128, D], BF16) for i in range(2)]
            junk = sb("junk", [128, D], F32)
            ssq = [sb("ssq%d" % i, [128, 1], F32) for i in range(2)]
            wsl = [sb("wsl%d" % i, [128, KC, 256], BF16) for i in range(2)]
            zraw = [sb("zraw%d" % i, [128, TB], F32) for i in range(2)]
            zc = [sb("zc%d" % i, [128, TB], F32) for i in range(2)]
            inw_v = g.in_w.rearrange("(kc p) f -> p kc f", p=128)
            low_v = g.lora_w.rearrange("(kc p) f -> p kc f", p=128)
            it = 0
            for b in range(BPC):
                for t in range(TB // 128):
                    p = it % 2
                    it += 1
                    if t < CTX // 128:
                        src = g.ctx[b, t * 128:(t + 1) * 128, :]
                        n = 2
                    else:
                        src = g.x[b, (t - 2) * 128:(t - 1) * 128, :]
                        n = b
                    X, XN, SS = "xt%d" % p, "xn%d" % p, "ssq%d" % p
                    S.dma(lambda e, p=p, src=src: e.dma_start(out=xt[p][:], in_=src), writes=[X])
                    S.op("act", lambda e, p=p: e.activation(out=junk[:], in_=xt[p][:], func=AF.Square), reads=[X], writes=["junk"])
                    S.op("dve", lambda e, p=p: e.tensor_reduce(out=ssq[p][:], in_=junk[:], axis=AX.X, op=ALU.add), reads=["junk"], writes=[SS])
                    S.op("dve", lambda e, p=p: e.tensor_scalar(out=ssq[p][:], in0=ssq[p][:], scalar1=1.0 / D, scalar2=EPS,
                                                               op0=ALU.mult, op1=ALU.add), reads=[SS], writes=[SS])
                    S.op("act", lambda e, p=p: e.sqrt(out=ssq[p][:], in_=ssq[p][:]), reads=[SS], writes=[SS])
                    S.op("dve", lambda e, p=p: e.reciprocal(out=ssq[p][:], in_=ssq[p][:]), reads=[SS], writes=[SS])
                    S.op("dve", lambda e, p=p: e.tensor_scalar(out=xn[p][:], in0=xt[p][:], scalar1=ssq[p][:], scalar2=None,
                                                               op0=ALU.mult), reads=[X, SS], writes=[XN])
                    for kc in range(KC):
                        q = 0
                        S.op("pe", lambda e, p=p, kc=kc, q=q: e.transpose(
                            pbf[q][:, (kc % 8) * 128:(kc % 8 + 1) * 128], xn[p][:, kc * 128:(kc + 1) * 128], identb[:]),
                            reads=[XN, "identb"], writes=["pbf%d" % q])
                        if kc % 8 == 7:
                            for k2 in range(kc - 7, kc + 1):
                                S.op("act", lambda e, k2=k2, q=q, n=n, t=t: e.activation(
                                    out=hT[:, k2, t * 128:(t + 1) * 128], in_=pbf[q][:, (k2 % 8) * 128:(k2 % 8 + 1) * 128],
                                    func=AF.Identity, scale=sc1[:, k2, n:n + 1], bias=modT[:, k2, n:n + 1]),
                                    reads=["pbf%d" % q, "sc1", "modT"], writes=["hT"])
                S.flush()
                nsl = PW // 256 + 2
                for sl in range(nsl):
                    if sl % 8 == 0 and sl > 0:
                        S.flush()
                    p = sl % 2
                    if sl < PW // 256:
                        srcw = inw_v[:, :, sl * 256:(sl + 1) * 256]
                        ncol = 256
                    elif sl == PW // 256:
                        srcw = low_v[:, :, 0:256]
                        ncol = 256
                    else:
                        srcw = low_v[:, :, 256:384]
                        ncol = 128
                    S.dma(lambda e, p=p, srcw=srcw, ncol=ncol: e.dma_start(out=wsl[p][:, :, 0:ncol], in_=srcw),
                          writes=["wsl%d" % p], q="pool")
                    for j in range(ncol // 128):
                        ch = sl * 2 + j
                        zp = ch % 2
                        Z, ZC = "zraw%d" % zp, "zc%d" % zp
                        toks = [(0, 256)] + [(256 + 512 * i, 512) for i in range(4)]
                        for ti, (t0, tn) in enumerate(toks):
                            pb = (ch * 5 + ti) % 6
                            for kc in range(KC):
                                S.op("pe", lambda e, p=p, j=j, kc=kc, pb=pb, t0=t0, tn=tn: e.matmul(
                                    ps[pb][:, 0:tn], lhsT=wsl[p][:, kc, j * 128:(j + 1) * 128], rhs=hT[:, kc, t0:t0 + tn],
                                    start=(kc == 0), stop=(kc == KC - 1)),
                                    reads=["wsl%d" % p, "hT"], writes=["ps%d" % pb])
                            if ch < NZ:
                                S.op("act", lambda e, zp=zp, pb=pb, t0=t0, tn=tn: e.copy(out=zraw[zp][:, t0:t0 + tn], in_=ps[pb][:, 0:tn]),
                                     reads=["ps%d" % pb], writes=[Z])
                            else:
                                fn = [AF.Tanh, AF.Identity, AF.Sigmoid][ch - NZ]
                                S.op("act", lambda e, zp=zp, pb=pb, t0=t0, tn=tn, fn=fn: e.activation(
                                    out=zc[zp][:, t0:t0 + tn], in_=ps[pb][:, 0:tn], func=fn),
                                    reads=["ps%d" % pb], writes=[ZC])
                        if ch < NZ:
                            S.op("dve", lambda e, zp=zp, ch=ch: e.tensor_scalar(
                                out=zc[zp][:], in0=zraw[zp][:], scalar1=cw[:, ch, 1:2], scalar2=cb[:, ch:ch + 1],
                                op0=ALU.mult, op1=ALU.add), reads=[Z, "cw", "cb"], writes=[ZC])
                            for (s0, s1) in ((0, CTX), (CTX, TB)):
                                S.op("dve", lambda e, zp=zp, ch=ch, s0=s0, s1=s1: e.scalar_tensor_tensor(
                                    out=zc[zp][:, s0 + 1:s1], in0=zraw[zp][:, s0:s1 - 1], scalar=cw[:, ch, 0:1],
                                    in1=zc[zp][:, s0 + 1:s1], op0=ALU.mult, op1=ALU.add), reads=[Z, ZC, "cw"], writes=[ZC])
                                S.op("dve", lambda e, zp=zp, ch=ch, s0=s0, s1=s1: e.scalar_tensor_tensor(
                                    out=zc[zp][:, s0:s1 - 1], in0=zraw[zp][:, s0 + 1:s1], scalar=cw[:, ch, 2:3],
                                    in1=zc[zp][:, s0:s1 - 1], op0=ALU.mult, op1=ALU.add), reads=[Z, ZC, "cw"], writes=[ZC])
                            dst = g.zT[b, ch]
                        else:
                            dst = g.loraT[b, ch - NZ]
                        S.dma(lambda e, zp=zp, dst=dst: e.dma_start(out=dst, in_=zc[zp][:]), reads=[ZC], writes=["scr"])
        S.flush()


def copy_dram(nc, g, pairs):
    with contextlib.ExitStack() as st:
        S = g.S
        S.barrier()
        bufs = [st.enter_context(nc.sbuf_tensor("cpb%d_%d" % (i, id(pairs) % 100000), [128, TB], F32)) for i in range(2)]
        it = 0
        for dst, src in pairs:
            for i in range(dst.shape[0]):
                for j in range(dst.shape[1]):
                    p = it % 2
                    it += 1
                    S.dma(lambda e, p=p, i=i, j=j, src=src: e.dma_start(out=bufs[p][:], in_=src[i, j]), writes=["b%d" % p])
                    S.dma(lambda e, p=p, i=i, j=j, dst=dst: e.dma_start(out=dst[i, j], in_=bufs[p][:]), reads=["b%d" % p])
        S.flush()


RW_NB, RW_NHP = BPC, 8
HY_NB = BPC
F_NEXP, F_NBLK = 32, 4
F_DBG = False


def build_program(stage="final"):
    nc = bass.Bass("TRN2", target_bir_lowering=False)
    g = declare_io(nc, stage)
    gstack = contextlib.ExitStack()
    g.S = Sched(nc, gstack, ndma=48)
    declare_rw(nc, g, stage)
    declare_hy(nc, g, stage)
    if stage in ("final", "simF"):
        declare_F(nc, g, stage)
    if stage not in ("simRW", "simHY", "simF"):
        phase_ABC(nc, g, stage)
    if stage in ("dbgHY", "simHY"):
        phase_HY(nc, g, stage, nb=HY_NB)
    if stage in ("dbgRW", "simRW"):
        phase_RW(nc, g, stage, nb=RW_NB, nhp=RW_NHP)
    if stage in ("final", "dbgMIX"):
        phase_RW(nc, g, stage)
        phase_HY(nc, g, stage)
    if stage == "final":
        phase_F(nc, g, stage, nexp=F_NEXP, nblk=F_NBLK)
    if stage == "simF":
        phase_F(nc, g, stage, nexp=F_NEXP, nblk=F_NBLK)
    if stage == "dbgC":
        copy_dram(nc, g, [(g.dbg_z, g.zT), (g.dbg_lora, g.loraT)])
    g.S.flush(final=True)
    gstack.close()
    return nc


def make_in_maps(inputs, cores=range(NCORES)):
    f = lambda k: np.asarray(inputs[k], np.float32)
    x, ctx, c, c_ctx = f("x"), f("ctx"), f("c"), f("c_ctx")
    lora_w = np.ascontiguousarray(np.concatenate(
        [f("rw_w1")[0, 0], f("rw_w1")[0, 1], f("rw_a1")[0, 0], f("rw_a1")[0, 1], f("rw_g1")[0]], axis=1))
    shared = {
        "ada_w": np.ascontiguousarray(f("ada_w")[0]),
        "ada_bT": _fm(f("ada_b")[0], 6 * KC),
        "g1T": _fm(f("norm1_g")[0], KC),
        "g2T": _fm(f("norm2_g")[0], KC),
        "in_w": np.ascontiguousarray(f("in_w")[0]),
        "lora_w": lora_w,
        "conv_wT": np.ascontiguousarray(f("conv_w")[0].reshape(3, NZ, 128).transpose(2, 1, 0)),
        "conv_bT": _fm(f("conv_b")[0], NZ),
        "final_g": np.ascontiguousarray(f("final_g").reshape(1, D)),
        "ident_bf": np.eye(128, dtype=np.float32).astype(ml_dtypes.bfloat16),
        "ident_f": np.eye(128, dtype=np.float32),
    }
    w2, a2 = f("rw_w2")[0], f("rw_a2")[0]
    w2pad = np.zeros((2, 128, 1024), np.float32)
    a2pad = np.zeros((2, 128, 1024), np.float32)
    for n in range(2):
        w2pad[n, n * 64:(n + 1) * 64] = w2[n]
        a2pad[n, n * 64:(n + 1) * 64] = a2[n]
    rwvec = np.stack([_fm(f("rw_w0")[0, 0], 8), _fm(f("rw_w0")[0, 1], 8), _fm(f("rw_a0")[0, 0], 8), _fm(f("rw_a0")[0, 1], 8),
                      _fm(f("rw_kk")[0], 8), _fm(f("rw_ka")[0], 8), _fm(f("rw_rk")[0].reshape(-1), 8),
                      _fm(f("rw_lnx_g")[0], 8), _fm(f("rw_lnx_b")[0], 8)], axis=1)
    shared.update({"w2pad": w2pad, "a2pad": a2pad, "rw_g2": np.ascontiguousarray(f("rw_g2")[0]),
                   "rwvec": np.ascontiguousarray(rwvec)})
    shared.update(rw_consts())
    shared.update(hy_consts())
    shared.update({"ada_brow": np.ascontiguousarray(f("ada_b")[0].reshape(1, -1)), "out_w": np.ascontiguousarray(f("out_w")[0]),
                   "g2row": np.ascontiguousarray(f("norm2_g")[0].reshape(1, D)), "router_w": np.ascontiguousarray(f("router_w")[0]),
                   "router_b": np.ascontiguousarray(f("router_b")[0].reshape(1, NE))})
    if "ex_w_gate" in inputs:
        bgu = np.stack([f("ex_b_gate")[0].reshape(NE, KC, 128).transpose(0, 2, 1), f("ex_b_up")[0].reshape(NE, KC, 128).transpose(0, 2, 1)], axis=2)
        shared.update({"ex_w_gate": f("ex_w_gate")[0], "ex_w_up": f("ex_w_up")[0], "ex_w_down": f("ex_w_down")[0],
                       "ex_bgu": np.ascontiguousarray(bgu), "ex_b_down": np.ascontiguousarray(f("ex_b_down")[0])})
    shared.update({"hy_w1": np.ascontiguousarray(f("hy_w1")[0]), "hy_w2": np.ascontiguousarray(f("hy_w2")[0]),
                   "hy_w3": np.ascontiguousarray(f("hy_w3")[0]), "hy_w4": np.ascontiguousarray(f("hy_w4")[0]),
                   "hy_fb": np.ascontiguousarray(np.stack([f("hy_freq")[0], f("hy_b1")[0], f("hy_b2")[0], f("hy_b3")[0]], axis=1)),
                   "hy_vec": np.ascontiguousarray(np.stack([_fm(f("hy_bias")[0], 8), _fm(f("hy_norm_g")[0], 8)], axis=1))})
    maps = []
    for cid in cores:
        b0 = cid * BPC
        cond = np.stack([c[b0], c[b0 + 1], c_ctx], axis=0)
        condT = np.ascontiguousarray(cond.reshape(3, KC, 128).transpose(2, 1, 0))
        m = dict(shared)
        m["x"] = np.ascontiguousarray(x[b0:b0 + BPC])
        m["ctx"] = np.ascontiguousarray(ctx[b0:b0 + BPC])
        m["condT"] = condT
        maps.append(m)
    return maps


def kernel(**inputs):
    nc = build_program()
    in_maps = make_in_maps(inputs)
    res = run_bass_kernel_spmd(nc, in_maps, core_ids=list(range(NCORES)))
    return np.concatenate([r["out"] for r in res.results], axis=0)


NCH = TB // 64
C0 = float(np.exp(-0.5))


def rw_consts():
    j = np.arange(128)[:, None]
    t = np.arange(128)[None, :]
    m = {}
    m["rw_mask0"] = np.concatenate([(j < t), (j <= t)], axis=1).astype(np.float32)
    m["rw_mask1"] = np.concatenate([(j > t), (j >= t)], axis=1).astype(np.float32)
    m["rw_maskT0"] = (t < j).astype(np.float32)
    m["rw_maskT1"] = (t > j).astype(np.float32)
    blk = np.zeros((128, 128), np.float32)
    blk[:64, :64] = 1.0
    blk[64:, 64:] = 1.0
    m["rw_blk"] = blk
    rm = np.ones((128, TB), np.float32)
    rm[:, ::64] = 0.0
    m["rw_rm"] = rm
    return m


def declare_rw(nc, g, stage):
    dt = nc.dram_tensor
    inp = lambda name, shape, dtype=F32: dt(name, list(shape), dtype, kind="ExternalInput").ap()
    g.w2pad = inp("w2pad", [2, 128, 1024])
    g.a2pad = inp("a2pad", [2, 128, 1024])
    g.rw_g2 = inp("rw_g2", [128, 1024])
    g.rwvec = inp("rwvec", [128, 9, 8])
    for k in ("rw_mask0", "rw_mask1"):
        setattr(g, k, inp(k, [128, 256]))
    for k in ("rw_maskT0", "rw_maskT1", "rw_blk"):
        setattr(g, k, inp(k, [128, 128]))
    g.rw_rm = inp("rw_rm", [128, TB])
    g.mixT = (dt("mixT_scr", [BPC, KC, 128, SEQ], BF16, kind="ExternalOutput") if stage == "dbgMIX" else dt("mixT_scr", [BPC, KC, 128, SEQ], BF16)).ap()
    if stage in ("dbgRW", "simRW"):
        g.dbg_rwo = dt("dbg_rwo", [BPC, 8, 128, SEQ], BF16, kind="ExternalOutput").ap()


def phase_RW(nc, g, stage, nb=BPC, nhp=8):
    with contextlib.ExitStack() as st:
        S = g.S
        S.barrier()
        sb = lambda name, shape, dtype: st.enter_context(nc.sbuf_tensor("rws_" + name, shape, dtype))
        PB = st.enter_context(nc.psum_tensor("rpbf", [128, 1024], BF16))
        P = [None] + [st.enter_context(nc.psum_tensor("rps%d" % i, [128, 512], F32)) for i in range(1, 8)]
        ROT = [1, 2, 3, 4, 7]
        rot = [0]
        mask = [sb("mask%d" % n, [128, 256], BF16) for n in range(2)]
        maskT = [sb("maskT%d" % n, [128, 128], BF16) for n in range(2)]
        blk = sb("blk", [128, 128], BF16)
        identb = sb("identb", [128, 128], BF16)
        identf = sb("identf", [128, 128], F32)
        rm = sb("rm", [128, TB], BF16)
        w2b = [sb("w2b%d" % n, [128, 128], BF16) for n in range(2)]
        a2b = [sb("a2b%d" % n, [128, 128], BF16) for n in range(2)]
        g2b = sb("g2b", [128, 128], BF16)
        vec = sb("rwvec_s", [128, 9, 8], F32)
        for n in range(2):
            S.dma(lambda e, n=n: e.dma_start(out=mask[n][:], in_=getattr(g, "rw_mask%d" % n)[:]), writes=["mask%d" % n], q="pool")
            S.dma(lambda e, n=n: e.dma_start(out=maskT[n][:], in_=getattr(g, "rw_maskT%d" % n)[:]), writes=["maskT%d" % n], q="pool")
        S.dma(lambda e: e.dma_start(out=blk[:], in_=g.rw_blk[:]), writes=["blk"], q="pool")
        S.dma(lambda e: e.dma_start(out=identb[:], in_=g.ident_bf[:]), writes=["identb"])
        S.dma(lambda e: e.dma_start(out=identf[:], in_=g.ident_f[:]), writes=["identf"])
        S.dma(lambda e: e.dma_start(out=rm[:], in_=g.rw_rm[:]), writes=["rm"], q="pool")
        S.dma(lambda e: e.dma_start(out=vec[:], in_=g.rwvec[:]), writes=["vec"])
        L = [sb("L%d" % i, [128, TB], BF16) for i in range(3)]
        R_, K_, V_ = sb("R_", [128, TB], F32), sb("K_", [128, TB], F32), sb("V_", [128, TB], F32)
        KKN = sb("KKN", [128, TB], F32)
        SG, AN, CUM = sb("SG", [128, TB], F32), sb("AN", [128, TB], F32), sb("CUM", [128, TB], F32)
        E1, E2 = sb("E1", [128, TB], F32), sb("E2", [128, TB], F32)
        KD = SG
        TBF = sb("TBF", [128, TB], BF16)
        TOT = sb("TOT", [128, NCH], F32)
        GAM = [sb("GAM%d" % n, [128, NCH], F32) for n in range(2)]
        arT = [sb("arT%d" % n, [128, NCH, 256], BF16) for n in range(2)]
        bT = [sb("bT%d" % n, [128, NCH, 128], BF16) for n in range(2)]
        kT = [sb("kT%d" % n, [128, NCH, 128], BF16) for n in range(2)]
        vT = sb("vT", [128, NCH, 128], BF16)
        yacc = sb("yacc", [128, 32, 64], F32)
        s1, s2 = sb("s1", [128, 32], F32), sb("s2", [128, 32], F32)
        TM = [sb("TM%d" % n, [128, 512], BF16) for n in range(2)]
        GB = [sb("GB%d" % n, [128, 256], BF16) for n in range(2)]
        GK = [sb("GK%d" % n, [128, 256], BF16) for n in range(2)]
        PW_ = [[sb("PW%d_%d" % (n, i), [128, 256], BF16) for i in range(2)] for n in range(2)]
        XX = [[sb("XX%d_%d" % (n, i), [128, 256], BF16) for i in range(2)] for n in range(2)]
        MT = [sb("MT%d" % n, [128, 128], BF16) for n in range(2)]
        RH = [sb("RH%d" % n, [128, 128], BF16) for n in range(2)]
        HH = [[sb("HH%d_%d" % (n, i), [128, 128], BF16) for i in range(2)] for n in range(2)]
        for n in range(2):
            for tname, tt in (("arT%d" % n, arT[n]), ("bT%d" % n, bT[n]), ("kT%d" % n, kT[n])):
                S.op("pool", lambda e, tt=tt: e.memset(tt[:], 0.0), writes=[tname])
        S.op("pool", lambda e: e.memset(vT[:], 0.0), writes=["vT"])
        ynbd = kT[0]

        toks = [(0, 256)] + [(256 + 512 * i, 512) for i in range(4)]
        v3 = lambda tile: tile[:].rearrange("p (c t) -> p c t", t=64)

        def bd_write(eng, dst, dst_name, col0, in0, in0_name, in1, in1_name, neg=False):
            for h in range(2):
                rs = slice(h * 64, (h + 1) * 64)
                o = dst[rs, :, col0 + h * 64: col0 + (h + 1) * 64]
                a = in0[rs, :].rearrange("p (c t) -> p c t", t=64)
                b = in1[rs, :].rearrange("p (c t) -> p c t", t=64)
                if neg:
                    S.op(eng, lambda e, o=o, a=a, b=b: e.scalar_tensor_tensor(out=o, in0=a, scalar=-1.0, in1=b, op0=ALU.mult, op1=ALU.mult),
                         reads=[in0_name, in1_name], writes=[dst_name])
                else:
                    S.op(eng, lambda e, o=o, a=a, b=b: e.tensor_tensor(out=o, in0=a, in1=b, op=ALU.mult),
                         reads=[in0_name, in1_name], writes=[dst_name])

        for b in range(nb):
            for i in range(3):
                S.dma(lambda e, i=i, b=b: e.dma_start(out=L[i][:], in_=g.loraT[b, i]), reads=["scr"], writes=["L%d" % i], q="pool")
            for hp in range(nhp):
                S.flush()
                S.dma(lambda e, b=b, hp=hp: e.dma_start(out=R_[:], in_=g.zT[b, 24 + hp]), reads=["scr"], writes=["R_"])
                S.dma(lambda e, b=b, hp=hp: e.dma_start(out=K_[:], in_=g.zT[b, 32 + hp]), reads=["scr"], writes=["K_"])
                S.dma(lambda e, b=b, hp=hp: e.dma_start(out=V_[:], in_=g.zT[b, 40 + hp]), reads=["scr"], writes=["V_"])
                hc = slice(hp * 128, (hp + 1) * 128)
                for n in range(2):
                    S.dma(lambda e, n=n, hc=hc: e.dma_start(out=w2b[n][:], in_=g.w2pad[n, :, hc]), writes=["w2b%d" % n], q="pool")
                    S.dma(lambda e, n=n, hc=hc: e.dma_start(out=a2b[n][:], in_=g.a2pad[n, :, hc]), writes=["a2b%d" % n], q="pool")
                S.dma(lambda e, hc=hc: e.dma_start(out=g2b[:], in_=g.rw_g2[:, hc]), writes=["g2b"], q="pool")
                hc = slice(0, 128)
                S.op("dve", lambda e, hp=hp: e.tensor_scalar(out=KKN[:], in0=K_[:], scalar1=vec[:, 4, hp:hp + 1], scalar2=None, op0=ALU.mult),
                     reads=["K_", "vec"], writes=["KKN"])
                S.op("dve", lambda e: e.tensor_tensor(out=TBF[:], in0=KKN[:], in1=KKN[:], op=ALU.mult), reads=["KKN"], writes=["TBF"])
                for ti, (t0, tn) in enumerate(toks):
                    pb = 1 + ti % 4
                    S.op("pe", lambda e, pb=pb, t0=t0, tn=tn: e.matmul(P[pb][:, 0:tn], lhsT=blk[:], rhs=TBF[:, t0:t0 + tn], start=True, stop=True),
                         reads=["blk", "TBF"], writes=["P%d" % pb])
                    S.op("act", lambda e, pb=pb, t0=t0, tn=tn: e.sqrt(out=E1[:, t0:t0 + tn], in_=P[pb][:, 0:tn]), reads=["P%d" % pb], writes=["E1"])
                S.op("dve", lambda e: e.tensor_scalar(out=E1[:], in0=E1[:], scalar1=1e-12, scalar2=None, op0=ALU.max), reads=["E1"], writes=["E1"])
                S.op("dve", lambda e: e.reciprocal(out=E1[:], in_=E1[:]), reads=["E1"], writes=["E1"])
                S.op("dve", lambda e: e.tensor_tensor(out=KKN[:], in0=KKN[:], in1=E1[:], op=ALU.mult), reads=["KKN", "E1"], writes=["KKN"])
                for h in range(2):
                    rs = slice(h * 64, (h + 1) * 64)
                    S.op("pool", lambda e, rs=rs, h=h: e.tensor_copy(out=vT[rs, :, h * 64:(h + 1) * 64], in_=V_[rs, :].rearrange("p (c t) -> p c t", t=64)),
                         reads=["V_"], writes=["vT"])
                for n in range(2):
                    for ti, (t0, tn) in enumerate(toks):
                        pb = 1 + ti % 4
                        S.op("pe", lambda e, pb=pb, t0=t0, tn=tn, n=n, hc=hc: e.matmul(P[pb][:, 0:tn], lhsT=w2b[n][:, hc], rhs=L[0][:, t0:t0 + tn], start=True, stop=True),
                             reads=["w2b%d" % n, "L0"], writes=["P%d" % pb])
                        S.op("act", lambda e, pb=pb, t0=t0, tn=tn, n=n, hp=hp: e.activation(out=SG[:, t0:t0 + tn], in_=P[pb][:, 0:tn], func=AF.Sigmoid, bias=vec[:, n, hp:hp + 1]),
                             reads=["P%d" % pb, "vec"], writes=["SG"])
                        pb2 = 5 + ti % 2
                        S.op("pe", lambda e, pb2=pb2, t0=t0, tn=tn, n=n, hc=hc: e.matmul(P[pb2][:, 0:tn], lhsT=a2b[n][:, hc], rhs=L[1][:, t0:t0 + tn], start=True, stop=True),
                             reads=["a2b%d" % n, "L1"], writes=["P%d" % pb2])
                        S.op("act", lambda e, pb2=pb2, t0=t0, tn=tn, n=n, hp=hp: e.activation(out=AN[:, t0:t0 + tn], in_=P[pb2][:, 0:tn], func=AF.Sigmoid, bias=vec[:, 2 + n, hp:hp + 1]),
                             reads=["P%d" % pb2, "vec"], writes=["AN"])
                    S.op("dve", lambda e: e.tensor_tensor_scan(out=CUM[:], data0=rm[:], data1=SG[:], initial=0.0, op0=ALU.mult, op1=ALU.add),
                         reads=["rm", "SG"], writes=["CUM"])
                    if n == 1:
                        S.op("dve", lambda e: e.tensor_copy(out=TOT[:], in_=v3(CUM)[:, :, 63]), reads=["CUM"], writes=["TOT"])
                        S.op("dve", lambda e: e.tensor_tensor(out=v3(CUM), in0=TOT[:].unsqueeze(2).to_broadcast([128, NCH, 64]), in1=v3(CUM), op=ALU.subtract),
                             reads=["TOT", "CUM"], writes=["CUM"])
                        S.op("dve", lambda e: e.tensor_tensor(out=CUM[:], in0=CUM[:], in1=SG[:], op=ALU.add), reads=["CUM", "SG"], writes=["CUM"])
                    S.op("dve", lambda e: e.tensor_tensor(out=E2[:], in0=CUM[:], in1=SG[:], op=ALU.subtract), reads=["CUM", "SG"], writes=["E2"])
                    S.op("act", lambda e: e.activation(out=E1[:], in_=E2[:], func=AF.Exp, scale=-C0), reads=["E2"], writes=["E1"])
                    bd_write("dve", arT[n], "arT%d" % n, 0, KKN, "KKN", E1, "E1", neg=True)
                    S.op("act", lambda e: e.activation(out=E1[:], in_=CUM[:], func=AF.Exp, scale=-C0), reads=["CUM"], writes=["E1"])
                    bd_write("dve", arT[n], "arT%d" % n, 128, R_, "R_", E1, "E1")
                    S.op("dve", lambda e, n=n: e.tensor_copy(out=GAM[n][:], in_=v3(E1)[:, :, 63 if n == 0 else 0]), reads=["E1"], writes=["GAM%d" % n])
                    S.op("dve", lambda e: e.tensor_tensor(out=E2[:], in0=KKN[:], in1=AN[:], op=ALU.mult), reads=["KKN", "AN"], writes=["E2"])
                    S.op("act", lambda e: e.activation(out=E1[:], in_=CUM[:], func=AF.Exp, scale=C0), reads=["CUM"], writes=["E1"])
                    bd_write("dve", bT[n], "bT%d" % n, 0, E2, "E2", E1, "E1")
                    S.op("dve", lambda e, hp=hp: e.tensor_scalar(out=KD[:], in0=AN[:], scalar1=-1.0, scalar2=vec[:, 5, hp:hp + 1], op0=ALU.add, op1=ALU.mult),
                         reads=["AN", "vec", "SG"], writes=["SG"])
                    S.op("dve", lambda e: e.scalar_tensor_tensor(out=KD[:], in0=KD[:], scalar=1.0, in1=K_[:], op0=ALU.add, op1=ALU.mult),
                         reads=["SG", "K_"], writes=["SG"])
                    bd_write("dve", kT[n], "kT%d" % n, 0, KD, "SG", E1, "E1")
                    if n == 0:
                        S.op("dve", lambda e, hp=hp: e.scalar_tensor_tensor(out=TBF[:], in0=R_[:], scalar=vec[:, 6, hp:hp + 1], in1=KD[:], op0=ALU.mult, op1=ALU.mult),
                             reads=["R_", "vec", "SG"], writes=["TBF"])
                    else:
                        S.op("dve", lambda e, hp=hp: e.scalar_tensor_tensor(out=E2[:], in0=R_[:], scalar=vec[:, 6, hp:hp + 1], in1=KD[:], op0=ALU.mult, op1=ALU.mult),
                             reads=["R_", "vec", "SG"], writes=["E2"])
                        S.op("dve", lambda e: e.tensor_tensor(out=TBF[:], in0=TBF[:], in1=E2[:], op=ALU.add), reads=["TBF", "E2"], writes=["TBF"])
                S.op("pool", lambda e: e.memset(yacc[:], 0.0), writes=["yacc"])
                for n in range(2):
                    S.op("pool", lambda e, n=n: e.memset(HH[n][0][:], 0.0), writes=["HH%d_0" % n])
                order = [list(range(NCH)), [3, 2, 1, 0] + list(range(NCH - 1, 3, -1))]
                for s in range(NCH):
                    for n in range(2):
                        c = order[n][s]
                        lat = c >= 4
                        nm = lambda x: "%s%d" % (x, n)
                        Hold, Hnew = HH[n][s % 2], HH[n][(s + 1) % 2]
                        Hold_n, Hnew_n = "HH%d_%d" % (n, s % 2), "HH%d_%d" % (n, (s + 1) % 2)
                        aTc, rTc = arT[n][:, c, 0:128], arT[n][:, c, 128:256]
                        for qi, (src, sname) in enumerate(((aTc, nm("arT")), (bT[n][:, c, :], nm("bT")), (kT[n][:, c, :], nm("kT")), (vT[:, c, :], "vT"))):
                            S.op("pe", lambda e, qi=qi, src=src: e.transpose(PB[:, qi * 128:(qi + 1) * 128], src, identb[:]),
                                 reads=[sname, "identb"], writes=["PB"])
                        S.op("act", lambda e, n=n: e.copy(out=TM[n][:], in_=PB[:, 0:512]), reads=["PB"], writes=[nm("TM")])
                        a_, b_, k_, v_ = (TM[n][:, i * 128:(i + 1) * 128] for i in range(4))
                        def nbk():
                            rot[0] = (rot[0] + 1) % len(ROT)
                            return ROT[rot[0]]
                        pw = PW_[n]
                        xx = XX[n]
                        k1 = nbk()
                        S.op("pe", lambda e, n=n, c=c, k1=k1: e.matmul(P[k1][:, 0:256], lhsT=bT[n][:, c, :], rhs=arT[n][:, c, :], start=True, stop=True),
                             reads=[nm("bT"), nm("arT")], writes=["P%d" % k1])
                        S.op("dve", lambda e, n=n, k1=k1: e.tensor_tensor(out=GB[n][:], in0=P[k1][:, 0:256], in1=mask[n][:], op=ALU.mult),
                             reads=["P%d" % k1, nm("mask")], writes=[nm("GB")])
                        k2 = nbk()
                        S.op("pe", lambda e, n=n, c=c, k2=k2: e.matmul(P[k2][:, 0:256], lhsT=kT[n][:, c, :], rhs=arT[n][:, c, :], start=True, stop=True),
                             reads=[nm("kT"), nm("arT")], writes=["P%d" % k2])
                        S.op("dve", lambda e, n=n, k2=k2: e.tensor_tensor(out=GK[n][:], in0=P[k2][:, 0:256], in1=mask[n][:], op=ALU.mult),
                             reads=["P%d" % k2, nm("mask")], writes=[nm("GK")])
                        k3 = nbk()
                        S.op("pe", lambda e, n=n, c=c, aTc=aTc, k3=k3: e.matmul(P[k3][:, 0:128], lhsT=aTc, rhs=bT[n][:, c, :], start=True, stop=True),
                             reads=[nm("bT"), nm("arT")], writes=["P%d" % k3])
                        S.op("dve", lambda e, n=n, pw=pw, k3=k3: e.tensor_tensor(out=pw[0][:, 0:128], in0=P[k3][:, 0:128], in1=maskT[n][:], op=ALU.mult),
                             reads=["P%d" % k3, nm("maskT")], writes=[nm("PW") + "_0"])
                        S.op("pool", lambda e, n=n, pw=pw: e.tensor_copy(out=pw[0][:, 128:256], in_=GB[n][:, 0:128]),
                             reads=[nm("GB")], writes=[nm("PW") + "_0"])
                        k4 = nbk()
                        S.op("pe", lambda e, n=n, v_=v_, k4=k4: e.matmul(P[k4][:, 0:128], lhsT=GK[n][:, 0:128], rhs=v_, start=True, stop=True),
                             reads=[nm("GK"), nm("TM")], writes=["P%d" % k4])
                        S.op("act", lambda e, xx=xx, k4=k4: e.copy(out=xx[0][:, 128:256], in_=P[k4][:, 0:128]), reads=["P%d" % k4], writes=[nm("XX") + "_0"])
                        S.op("pool", lambda e, xx=xx, a_=a_: e.tensor_copy(out=xx[0][:, 0:128], in_=a_), reads=[nm("TM")], writes=[nm("XX") + "_0"])
                        for i in range(6):
                            pc, pn = pw[i % 2], pw[(i + 1) % 2]
                            pcn, pnn = nm("PW") + "_%d" % (i % 2), nm("PW") + "_%d" % ((i + 1) % 2)
                            xc, xn_ = xx[i % 2], xx[(i + 1) % 2]
                            xcn, xnn = nm("XX") + "_%d" % (i % 2), nm("XX") + "_%d" % ((i + 1) % 2)
                            bk = nbk()
                            S.op("pe", lambda e, pc=pc, xc=xc, bk=bk: e.matmul(P[bk][:, 0:256], lhsT=pc[:, 128:256], rhs=xc[:], start=True, stop=True),
                                 reads=[pcn, xcn], writes=["P%d" % bk])
                            S.op("dve", lambda e, xc=xc, xn_=xn_, bk=bk: e.tensor_tensor(out=xn_[:], in0=P[bk][:, 0:256], in1=xc[:], op=ALU.add),
                                 reads=["P%d" % bk, xcn], writes=[xnn])
                            if i < 5:
                                bq = nbk()
                                S.op("pe", lambda e, pc=pc, bq=bq: e.matmul(P[bq][:, 0:128], lhsT=pc[:, 128:256], rhs=pc[:, 0:128], start=True, stop=True),
                                     reads=[pcn], writes=["P%d" % bq])
                                S.op("pe", lambda e, pc=pc, bq=bq: e.matmul(P[bq][:, 128:256], lhsT=pc[:, 0:128], rhs=pc[:, 128:256], start=True, stop=True),
                                     reads=[pcn], writes=["P%d" % bq])
                                S.op("act", lambda e, pn=pn, bq=bq: e.copy(out=pn[:], in_=P[bq][:, 0:256]), reads=["P%d" % bq], writes=[pnn])
                        X = xx[0]
                        Xn = nm("XX") + "_0"
                        Ah, U0 = X[:, 0:128], X[:, 128:256]
                        k5 = nbk()
                        S.op("pe", lambda e, Ah=Ah, b_=b_, k5=k5: e.matmul(P[k5][:, 0:128], lhsT=Ah, rhs=b_, start=True, stop=True),
                             reads=[Xn, nm("TM")], writes=["P%d" % k5])
                        S.op("dve", lambda e, n=n, k5=k5: e.tensor_tensor(out=MT[n][:], in0=P[k5][:, 0:128], in1=identf[:], op=ALU.add),
                             reads=["P%d" % k5, "identf"], writes=[nm("MT")])
                        if lat:
                            k6 = nbk()
                            S.op("pe", lambda e, Ah=Ah, n=n, k6=k6: e.matmul(P[k6][:, 0:128], lhsT=Ah, rhs=GB[n][:, 128:256], start=True, stop=True),
                                 reads=[Xn, nm("GB")], writes=["P%d" % k6])
                            S.op("dve", lambda e, n=n, rTc=rTc, k6=k6: e.tensor_tensor(out=RH[n][:], in0=P[k6][:, 0:128], in1=rTc, op=ALU.add),
                                 reads=["P%d" % k6, nm("arT")], writes=[nm("RH")])
                            S.op("pe", lambda e, n=n, U0=U0: e.matmul(P[6][:, 0:128], lhsT=GB[n][:, 128:256], rhs=U0, start=True, stop=False),
                                 reads=[nm("GB"), Xn], writes=["P6"])
                            S.op("pe", lambda e, n=n, v_=v_: e.matmul(P[6][:, 0:128], lhsT=GK[n][:, 128:256], rhs=v_, start=False, stop=False),
                                 reads=[nm("GK"), nm("TM")], writes=["P6"])
                            S.op("pe", lambda e, n=n, Hold=Hold: e.matmul(P[6][:, 0:128], lhsT=RH[n][:], rhs=Hold[:], start=False, stop=True),
                                 reads=[nm("RH"), Hold_n], writes=["P6"])
                            for h in range(2):
                                rs = slice(h * 64, (h + 1) * 64)
                                S.op("dve", lambda e, c=c, rs=rs, h=h: e.tensor_tensor(out=yacc[rs, c - 4, :], in0=P[6][rs, h * 64:(h + 1) * 64], in1=yacc[rs, c - 4, :], op=ALU.add),
                                     reads=["P6", "yacc"], writes=["yacc"])
                        S.op("pe", lambda e, b_=b_, U0=U0: e.matmul(P[5][:, 0:128], lhsT=b_, rhs=U0, start=True, stop=False),
                             reads=[nm("TM"), Xn], writes=["P5"])
                        S.op("pe", lambda e, k_=k_, v_=v_: e.matmul(P[5][:, 0:128], lhsT=k_, rhs=v_, start=False, stop=False),
                             reads=[nm("TM")], writes=["P5"])
                        S.op("pe", lambda e, n=n, Hold=Hold: e.matmul(P[5][:, 0:128], lhsT=MT[n][:], rhs=Hold[:], start=False, stop=True),
                             reads=[nm("MT"), Hold_n], writes=["P5"])
                        S.op("act", lambda e, n=n, c=c, Hnew=Hnew: e.activation(out=Hnew[:], in_=P[5][:, 0:128], func=AF.Copy, scale=GAM[n][:, c:c + 1]),
                             reads=["P5", nm("GAM")], writes=[Hnew_n])
                S.op("dve", lambda e: e.tensor_reduce(out=s1[:], in_=yacc[:], axis=AX.X, op=ALU.add), reads=["yacc"], writes=["s1"])
                ysq = E1[:, 0:32 * 64].rearrange("p (c v) -> p c v", v=64)
                S.op("dve", lambda e: e.tensor_tensor(out=ysq, in0=yacc[:], in1=yacc[:], op=ALU.mult), reads=["yacc"], writes=["E1"])
                S.op("dve", lambda e: e.tensor_reduce(out=s2[:], in_=ysq, axis=AX.X, op=ALU.add), reads=["E1"], writes=["s2"])
                S.op("dve", lambda e: e.tensor_scalar(out=s1[:], in0=s1[:], scalar1=1.0 / 64, scalar2=None, op0=ALU.mult), reads=["s1"], writes=["s1"])
                S.op("dve", lambda e: e.tensor_tensor(out=TOT[:, 0:32], in0=s1[:], in1=s1[:], op=ALU.mult), reads=["s1"], writes=["TOT"])
                S.op("dve", lambda e: e.scalar_tensor_tensor(out=s2[:], in0=s2[:], scalar=1.0 / 64, in1=TOT[:, 0:32], op0=ALU.mult, op1=ALU.subtract),
                     reads=["s2", "TOT"], writes=["s2"])
                S.op("dve", lambda e: e.tensor_scalar(out=s2[:], in0=s2[:], scalar1=64e-5, scalar2=None, op0=ALU.add), reads=["s2"], writes=["s2"])
                S.op("act", lambda e: e.sqrt(out=s2[:], in_=s2[:]), reads=["s2"], writes=["s2"])
                S.op("dve", lambda e: e.reciprocal(out=s2[:], in_=s2[:]), reads=["s2"], writes=["s2"])
                S.op("dve", lambda e: e.tensor_tensor(out=ysq, in0=yacc[:], in1=s1[:].unsqueeze(2).to_broadcast([128, 32, 64]), op=ALU.subtract),
                     reads=["yacc", "s1"], writes=["E1"])
                for h in range(2):
                    rs = slice(h * 64, (h + 1) * 64)
                    cs = slice(h * 64, (h + 1) * 64)
                    S.op("dve", lambda e, rs=rs, cs=cs: e.tensor_tensor(out=ynbd[rs, 0:32, cs], in0=ysq[rs, :, :], in1=s2[rs, :].unsqueeze(2).to_broadcast([64, 32, 64]), op=ALU.mult),
                         reads=["E1", "s2"], writes=["kT0"])
                for c in range(32):
                    q = c % 8
                    S.op("pe", lambda e, c=c, q=q: e.transpose(PB[:, q * 128:(q + 1) * 128], ynbd[:, c, :], identb[:]), reads=["kT0", "identb"], writes=["PB"])
                    for h in range(2):
                        rs = slice(h * 64, (h + 1) * 64)
                        S.op("act", lambda e, c=c, q=q, rs=rs, h=h, hp=hp: e.activation(
                            out=E2[rs, c * 64:(c + 1) * 64], in_=PB[rs, q * 128 + h * 64:q * 128 + (h + 1) * 64], func=AF.Identity,
                            scale=vec[rs, 7, hp:hp + 1], bias=vec[rs, 8, hp:hp + 1]), reads=["PB", "vec"], writes=["E2"])
                BON = SG
                for ti in range(4):
                    pb = 1 + ti % 4
                    t0 = CTX + 512 * ti
                    S.op("pe", lambda e, pb=pb, t0=t0: e.matmul(P[pb][:, 0:512], lhsT=blk[:], rhs=TBF[:, t0:t0 + 512], start=True, stop=True),
                         reads=["blk", "TBF"], writes=["P%d" % pb])
                    S.op("dve", lambda e, pb=pb, t0=t0, ti=ti: e.tensor_tensor(out=BON[:, ti * 512:(ti + 1) * 512], in0=P[pb][:, 0:512], in1=V_[:, t0:t0 + 512], op=ALU.mult),
                         reads=["P%d" % pb, "V_"], writes=["SG"])
                S.op("dve", lambda e: e.tensor_tensor(out=E2[:, 0:SEQ], in0=E2[:, 0:SEQ], in1=BON[:, 0:SEQ], op=ALU.add), reads=["E2", "SG"], writes=["E2"])
                mixo = TBF
                for ti in range(4):
                    pb = 1 + ti % 4
                    t0 = CTX + 512 * ti
                    S.op("pe", lambda e, pb=pb, t0=t0: e.matmul(P[pb][:, 0:512], lhsT=g2b[:], rhs=L[2][:, t0:t0 + 512], start=True, stop=True),
                         reads=["g2b", "L2"], writes=["P%d" % pb])
                    S.op("dve", lambda e, pb=pb, ti=ti: e.tensor_tensor(out=mixo[:, ti * 512:(ti + 1) * 512], in0=P[pb][:, 0:512], in1=E2[:, ti * 512:(ti + 1) * 512], op=ALU.mult),
                         reads=["P%d" % pb, "E2"], writes=["TBF"])
                S.dma(lambda e, b=b, hp=hp: e.dma_start(out=g.mixT[b, 8 + hp], in_=mixo[:, 0:SEQ]), reads=["TBF"], writes=["mixscr"])
                if stage in ("dbgRW", "simRW"):
                    S.dma(lambda e, b=b, hp=hp: e.dma_start(out=g.dbg_rwo[b, hp], in_=mixo[:, 0:SEQ]), reads=["TBF"])
        S.flush()


NF = 4096
TWO_PI = float(2 * np.pi)


def hy_consts():
    L = SEQ
    t = np.linspace(0.0, 1.0, L, dtype=np.float32)[:, None]
    ang = (2.0 * np.pi / L) * np.arange(L, dtype=np.float32)[:, None]
    bands = np.linspace(1e-4, 15, 16, dtype=np.float32)[None, :]
    feats = np.concatenate([t, np.cos(bands * ang), -np.sin(bands * ang)], axis=-1).astype(np.float32)
    deltas = np.abs(np.linspace(np.log(1e-2) / 1.5, np.log(1e-2) / 0.3, 1024, dtype=np.float32))
    win = np.exp(-t * np.tile(deltas, 2)).astype(np.float32)
    tt = np.arange(L, dtype=np.float64)[:, None]
    ff = np.arange(L, dtype=np.float64)[None, :]
    th = 2 * np.pi * ((tt * ff) % NF) / NF
    C = np.cos(th)
    Sn = np.sin(th)
    wt = np.full((L,), 2.0 / NF)
    wt[0] = 1.0 / NF
    bf = ml_dtypes.bfloat16
    tile = lambda M: np.ascontiguousarray(M.reshape(16, 128, 16, 128).transpose(2, 1, 0, 3)).astype(np.float32).astype(bf)
    m = {
        "hy_featsT": np.ascontiguousarray(feats.T),
        "hy_win": np.ascontiguousarray(win.reshape(16, 128, 2048).transpose(1, 0, 2)),
        "hy_Cfw": tile(C), "hy_Sfw": tile(Sn),
        "hy_Cinv": (C * wt[:, None]).astype(np.float32).astype(bf),
        "hy_Sinv": (Sn * wt[:, None]).astype(np.float32).astype(bf),
        "hy_alt": np.ascontiguousarray(((-1.0) ** np.arange(L)).reshape(16, 128).T.astype(np.float32)).astype(bf),
        "hy_altinv": (((-1.0) ** np.arange(L)) / NF).reshape(1, L).astype(np.float32).astype(bf),
    }
    return m


def declare_hy(nc, g, stage):
    dt = nc.dram_tensor
    inp = lambda name, shape, dtype=F32: dt(name, list(shape), dtype, kind="ExternalInput").ap()
    g.hy_featsT = inp("hy_featsT", [33, SEQ])
    g.hy_win = inp("hy_win", [128, 16, 2048])
    g.hy_Cfw = inp("hy_Cfw", [16, 128, 16, 128], BF16)
    g.hy_Sfw = inp("hy_Sfw", [16, 128, 16, 128], BF16)
    g.hy_Cinv = inp("hy_Cinv", [SEQ, SEQ], BF16)
    g.hy_Sinv = inp("hy_Sinv", [SEQ, SEQ], BF16)
    g.hy_alt = inp("hy_alt", [128, 16], BF16)
    g.hy_altinv = inp("hy_altinv", [1, SEQ], BF16)
    g.hy_w1 = inp("hy_w1", [33, 64])
    g.hy_w2 = inp("hy_w2", [64, 64])
    g.hy_w3 = inp("hy_w3", [64, 64])
    g.hy_w4 = inp("hy_w4", [64, 2048])
    g.hy_fb = inp("hy_fb", [64, 4])
    g.hy_vec = inp("hy_vec", [128, 2, 8])
    g.specT = dt("hy_spec_scr", [2, 17, 128, 1024], F32).ap()
    if stage in ("dbgHY", "simHY"):
        g.dbg_hyo = dt("dbg_hyo", [BPC, 8, 128, SEQ], BF16, kind="ExternalOutput").ap()


def phase_HY(nc, g, stage, nb=BPC):
    with contextlib.ExitStack() as st:
        S = g.S
        S.barrier()
        sb = lambda name, shape, dtype: st.enter_context(nc.sbuf_tensor("hys_" + name, shape, dtype))
        PB = st.enter_context(nc.psum_tensor("hpbf", [128, 1024], BF16))
        P = [None] + [st.enter_context(nc.psum_tensor("hps%d" % i, [128, 512], F32)) for i in range(1, 8)]
        identb = sb("identb", [128, 128], BF16)
        blk = sb("blk", [128, 128], BF16)
        alt = sb("alt", [128, 16], BF16)
        altinv = sb("altinv", [1, SEQ], BF16)
        vec = sb("vec", [128, 2, 8], F32)
        S.dma(lambda e: e.dma_start(out=identb[:], in_=g.ident_bf[:]), writes=["identb"])
        S.dma(lambda e: e.dma_start(out=blk[:], in_=g.rw_blk[:]), writes=["blk"], q="pool")
        S.dma(lambda e: e.dma_start(out=alt[:], in_=g.hy_alt[:]), writes=["alt"])
        S.dma(lambda e: e.dma_start(out=altinv[:], in_=g.hy_altinv[:]), writes=["altinv"])
        S.dma(lambda e: e.dma_start(out=vec[:], in_=g.hy_vec[:]), writes=["vec"])
        uTM = sb("uTM", [128, 16, 512], BF16)
        fw = [[sb("fw%d_%d" % (i, j), [128, 16, 128], BF16) for j in range(2)] for i in range(2)]
        Pc = sb("Pc", [128, 1024], F32)
        Ps = sb("Ps", [128, 1024], F32)
        rot = [0]
        ROT = [1, 2, 3, 4, 5, 6, 7]

        def nbk():
            rot[0] = (rot[0] + 1) % len(ROT)
            return ROT[rot[0]]

        def forward_dft(ncols, sink):
            for fc in range(16):
                bufi = fc % 2
                for part, src in ((0, g.hy_Cfw), (1, g.hy_Sfw)):
                    S.dma(lambda e, part=part, bufi=bufi, src=src, fc=fc: e.dma_start(out=fw[part][bufi][:], in_=src[fc]),
                          writes=["fw%d_%d" % (part, bufi)])
                kc_, ks_ = nbk(), nbk()
                for part, bk in ((0, kc_), (1, ks_)):
                    for tb in range(16):
                        S.op("pe", lambda e, part=part, bufi=bufi, tb=tb, bk=bk: e.matmul(
                            P[bk][:, 0:ncols], lhsT=fw[part][bufi][:, tb, :], rhs=uTM[:, tb, 0:ncols], start=(tb == 0), stop=(tb == 15)),
                            reads=["fw%d_%d" % (part, bufi), "uTM"], writes=["P%d" % bk])
                sink(fc, kc_, ks_)
            kn = nbk()
            for tb in range(16):
                S.op("pe", lambda e, tb=tb, kn=kn: e.matmul(P[kn][0:1, 0:ncols], lhsT=alt[:, tb:tb + 1], rhs=uTM[:, tb, 0:ncols],
                                                            start=(tb == 0), stop=(tb == 15)),
                     reads=["alt", "uTM"], writes=["P%d" % kn])
            sink(16, kn, None)

        with contextlib.ExitStack() as stF:
            sbF = lambda name, shape, dtype: stF.enter_context(nc.sbuf_tensor("hyf_" + name, shape, dtype))
            featsT = sbF("featsT", [33, SEQ], F32)
            w1 = sbF("w1", [33, 64], F32)
            w2 = sbF("w2", [64, 64], F32)
            w3 = sbF("w3", [64, 64], F32)
            w4 = sbF("w4", [64, 2048], F32)
            fb = sbF("fb", [64, 4], F32)
            fbc = sbF("fbc", [64, 3], F32)
            hA = sbF("hA", [64, SEQ], F32)
            hB = sbF("hB", [64, SEQ], F32)
            win = sbF("win", [128, 16, 512], F32)
            for tname, tt, src in (("featsT", featsT, g.hy_featsT), ("w1", w1, g.hy_w1), ("w2", w2, g.hy_w2), ("w3", w3, g.hy_w3),
                                   ("w4", w4, g.hy_w4), ("fb", fb, g.hy_fb)):
                S.dma(lambda e, tt=tt, src=src: e.dma_start(out=tt[:], in_=src[:]), writes=[tname])
            for i in range(3):
                S.op("dve", lambda e, i=i: e.tensor_scalar(out=fbc[:, i:i + 1], in0=fb[:, 1 + i:2 + i], scalar1=fb[:, 0:1], scalar2=None,
                                                           op0=ALU.mult), reads=["fb"], writes=["fbc"])
            layers = ((w1, "w1", featsT, "featsT", 33, hA, "hA"), (w2, "w2", hA, "hA", 64, hB, "hB"), (w3, "w3", hB, "hB", 64, hA, "hA"))
            for li, (w, wn, src, sn, kk, dst, dn) in enumerate(layers):
                for ti in range(4):
                    bk = nbk()
                    S.op("pe", lambda e, w=w, src=src, kk=kk, ti=ti, bk=bk: e.matmul(P[bk][0:64, 0:512], lhsT=w[0:kk, :], rhs=src[0:kk, ti * 512:(ti + 1) * 512],
                                                                                   start=True, stop=True), reads=[wn, sn], writes=["P%d" % bk])
                    MAGIC = 12582912.0
                    S.op("dve", lambda e, ti=ti, bk=bk, li=li: e.tensor_scalar(out=win[0:64, 0, :], in0=P[bk][0:64, 0:512], scalar1=fb[:, 0:1], scalar2=fbc[:, li:li + 1],
                                                                               op0=ALU.mult, op1=ALU.add), reads=["P%d" % bk, "fb", "fbc"], writes=["win"])
                    S.op("dve", lambda e: e.tensor_scalar(out=win[0:64, 1, :], in0=win[0:64, 0, :], scalar1=1.0 / TWO_PI, scalar2=MAGIC,
                                                          op0=ALU.mult, op1=ALU.add), reads=["win"], writes=["win"])
                    S.op("dve", lambda e: e.tensor_scalar(out=win[0:64, 1, :], in0=win[0:64, 1, :], scalar1=-MAGIC, scalar2=None,
                                                          op0=ALU.add), reads=["win"], writes=["win"])
                    S.op("dve", lambda e: e.scalar_tensor_tensor(out=win[0:64, 0, :], in0=win[0:64, 1, :], scalar=-TWO_PI, in1=win[0:64, 0, :],
                                                                 op0=ALU.mult, op1=ALU.add), reads=["win"], writes=["win"])
                    S.op("dve", lambda e: e.tensor_scalar(out=win[0:64, 0, :], in0=win[0:64, 0, :], scalar1=-3.1415925, scalar2=3.1415925,
                                                          op0=ALU.max, op1=ALU.min), reads=["win"], writes=["win"])
                    S.op("act", lambda e, dst=dst, ti=ti: e.activation(out=dst[:, ti * 512:(ti + 1) * 512], in_=win[0:64, 0, :], func=AF.Sin),
                         reads=["win"], writes=[dn])
            h3 = hA
            for cg in range(4):
                S.dma(lambda e, cg=cg: e.dma_start(out=win[:], in_=g.hy_win[:, :, cg * 512:(cg + 1) * 512]), writes=["win"])
                for tb in range(16):
                    bk = nbk()
                    S.op("pe", lambda e, tb=tb, cg=cg, bk=bk: e.matmul(P[bk][:, 0:512], lhsT=h3[:, tb * 128:(tb + 1) * 128], rhs=w4[:, cg * 512:(cg + 1) * 512],
                                                                      start=True, stop=True), reads=["hA", "w4"], writes=["P%d" % bk])
                    S.op("dve", lambda e, tb=tb, bk=bk: e.tensor_tensor(out=uTM[:, tb, :], in0=P[bk][:, 0:512], in1=win[:, tb, :], op=ALU.mult),
                         reads=["P%d" % bk, "win"], writes=["uTM"])
                if cg >= 2:
                    S.op("dve", lambda e: e.memset(uTM[0:1, 0, :], 0.0), writes=["uTM"])
                bwd = cg >= 2
                c0 = (cg % 2) * 512

                def sink(fc, kc_, ks_, bwd=bwd, c0=c0):
                    rows = slice(0, 128) if fc < 16 else slice(0, 1)
                    for part, bk, sign in ((0, kc_, 1.0), (1, ks_, -1.0 if not bwd else 1.0)):
                        if bk is None:
                            continue
                        dst = Pc if part == 0 else Ps
                        dn = "Pc" if part == 0 else "Ps"
                        if not bwd:
                            S.op("act", lambda e, dst=dst, bk=bk, sign=sign, rows=rows: e.activation(out=dst[rows, 0:512], in_=P[bk][rows, 0:512], func=AF.Copy, scale=sign),
                                 reads=["P%d" % bk], writes=[dn])
                        else:
                            S.dma(lambda e, dst=dst, part=part, fc=fc, rows=rows, c0=c0: e.dma_start(out=dst[rows, 0:512], in_=g.specT[part, fc, rows, c0:c0 + 512]),
                                  reads=["spec"], writes=[dn])
                            S.op("dve", lambda e, dst=dst, bk=bk, rows=rows: e.tensor_tensor(out=dst[rows, 0:512], in0=dst[rows, 0:512], in1=P[bk][rows, 0:512], op=ALU.add),
                                 reads=["P%d" % bk, dn], writes=[dn])
                        S.dma(lambda e, dst=dst, part=part, fc=fc, rows=rows, c0=c0: e.dma_start(out=g.specT[part, fc, rows, c0:c0 + 512], in_=dst[rows, 0:512]),
                              reads=[dn], writes=["spec"])
                forward_dft(512, sink)

        S.barrier()
        X0 = sb("X0", [128, SEQ], F32)
        X1 = sb("X1", [128, SEQ], F32)
        VV = sb("VV", [128, SEQ], F32)
        UU = [sb("UU%d" % i, [128, SEQ], F32) for i in range(4)]
        Ub = sb("Ub", [128, SEQ], BF16)
        Yr = sb("Yr", [128, 17, 512], BF16)
        Yi = sb("Yi", [128, 17, 512], BF16)
        Tr = sb("Tr", [128, 512], F32)
        Ti = sb("Ti", [128, 512], F32)
        inv = [[sb("inv%d_%d" % (i, j), [128, SEQ], BF16) for j in range(2)] for i in range(2)]
        osb = sb("osb", [128, SEQ], BF16)
        for b in range(nb):
            for cgp in range(2):
                S.flush()
                for j in range(4):
                    ch = cgp * 4 + j
                    S.dma(lambda e, b=b, ch=ch: e.dma_start(out=X1[:], in_=g.zT[b, 8 + ch, :, CTX:TB]), reads=["scr"], writes=["X1"])
                    S.dma(lambda e, b=b, ch=ch: e.dma_start(out=VV[:], in_=g.zT[b, 16 + ch, :, CTX:TB]), reads=["scr"], writes=["VV"])
                    S.op("dve", lambda e, j=j: e.tensor_tensor(out=UU[j][:], in0=VV[:], in1=X1[:], op=ALU.mult), reads=["VV", "X1"], writes=["UU%d" % j])
                    S.op("pool", lambda e, j=j: e.tensor_copy(out=Ub[:], in_=UU[j][:]), reads=["UU%d" % j], writes=["Ub"])
                    for tb in range(16):
                        q = tb % 8
                        S.op("pe", lambda e, tb=tb, q=q: e.transpose(PB[:, q * 128:(q + 1) * 128], Ub[:, tb * 128:(tb + 1) * 128], identb[:]),
                             reads=["Ub", "identb"], writes=["PB"])
                        if q == 7:
                            S.op("act", lambda e, tb=tb, j=j: e.copy(out=uTM[:, tb - 7:tb + 1, j * 128:(j + 1) * 128],
                                                                    in_=PB[:, :].rearrange("p (q f) -> p q f", f=128)), reads=["PB"], writes=["uTM"])

                def sink2(fc, kc_, ks_, cgp=cgp):
                    rows = slice(0, 128) if fc < 16 else slice(0, 1)
                    c0 = cgp * 512
                    S.dma(lambda e, fc=fc, rows=rows, c0=c0: e.dma_start(out=Tr[rows, :], in_=g.specT[0, fc, rows, c0:c0 + 512]), reads=["spec"], writes=["Tr"])
                    if ks_ is not None:
                        S.dma(lambda e, fc=fc, rows=rows, c0=c0: e.dma_start(out=Ti[rows, :], in_=g.specT[1, fc, rows, c0:c0 + 512]), reads=["spec"], writes=["Ti"])
                    if ks_ is None:
                        S.op("dve", lambda e, rows=rows, kc_=kc_: e.tensor_tensor(out=Yr[rows, 16, :], in0=P[kc_][rows, 0:512], in1=Tr[rows, :], op=ALU.mult),
                             reads=["P%d" % kc_, "Tr"], writes=["Yr"])
                        return
                    S.op("act", lambda e, kc_=kc_: e.copy(out=Pc[:, 0:512], in_=P[kc_][:, 0:512]), reads=["P%d" % kc_], writes=["Pc"])
                    S.op("act", lambda e, ks_=ks_: e.copy(out=Ps[:, 0:512], in_=P[ks_][:, 0:512]), reads=["P%d" % ks_], writes=["Ps"])
                    S.op("dve", lambda e: e.tensor_tensor(out=Pc[:, 512:1024], in0=Pc[:, 0:512], in1=Tr[:], op=ALU.mult), reads=["Pc", "Tr"], writes=["Pc"])
                    S.op("pool", lambda e: e.tensor_tensor(out=Ps[:, 512:1024], in0=Ps[:, 0:512], in1=Ti[:], op=ALU.mult), reads=["Ps", "Ti"], writes=["Ps"])
                    S.op("dve", lambda e, fc=fc: e.tensor_tensor(out=Yr[:, fc, :], in0=Pc[:, 512:1024], in1=Ps[:, 512:1024], op=ALU.add), reads=["Pc", "Ps"], writes=["Yr"])
                    S.op("dve", lambda e: e.tensor_tensor(out=Ps[:, 512:1024], in0=Ps[:, 0:512], in1=Tr[:], op=ALU.mult), reads=["Ps", "Tr"], writes=["Ps"])
                    S.op("pool", lambda e: e.tensor_tensor(out=Pc[:, 512:1024], in0=Pc[:, 0:512], in1=Ti[:], op=ALU.mult), reads=["Pc", "Ti"], writes=["Pc"])
                    S.op("dve", lambda e, fc=fc: e.tensor_tensor(out=Yi[:, fc, :], in0=Ps[:, 512:1024], in1=Pc[:, 512:1024], op=ALU.subtract), reads=["Pc", "Ps"], writes=["Yi"])
                forward_dft(512, sink2)
                acc = [[nbk() for _ in range(4)] for _ in range(1)]
                for j in range(4):
                    ch = cgp * 4 + j
                    banks = [1, 2, 3, 4]
                    for fc in range(16):
                        bufi = fc % 2
                        if j == 0 or True:
                            for part, src in ((0, g.hy_Cinv), (1, g.hy_Sinv)):
                                S.dma(lambda e, part=part, bufi=bufi, src=src, fc=fc: e.dma_start(out=inv[part][bufi][:], in_=src[fc * 128:(fc + 1) * 128, :]),
                                      writes=["inv%d_%d" % (part, bufi)])
                        for ti in range(4):
                            bk = banks[ti]
                            S.op("pe", lambda e, fc=fc, j=j, ti=ti, bk=bk, bufi=bufi: e.matmul(
                                P[bk][:, 0:512], lhsT=Yr[:, fc, j * 128:(j + 1) * 128], rhs=inv[0][bufi][:, ti * 512:(ti + 1) * 512], start=(fc == 0), stop=False),
                                reads=["Yr", "inv0_%d" % bufi], writes=["P%d" % bk])
                            S.op("pe", lambda e, fc=fc, j=j, ti=ti, bk=bk, bufi=bufi: e.matmul(
                                P[bk][:, 0:512], lhsT=Yi[:, fc, j * 128:(j + 1) * 128], rhs=inv[1][bufi][:, ti * 512:(ti + 1) * 512], start=False, stop=False),
                                reads=["Yi", "inv1_%d" % bufi], writes=["P%d" % bk])
                    for ti in range(4):
                        bk = banks[ti]
                        S.op("pe", lambda e, j=j, ti=ti, bk=bk: e.matmul(
                            P[bk][:, 0:512], lhsT=Yr[0:1, 16, j * 128:(j + 1) * 128], rhs=altinv[0:1, ti * 512:(ti + 1) * 512], start=False, stop=True),
                            reads=["Yr", "altinv"], writes=["P%d" % bk])
                    S.dma(lambda e, b=b, ch=ch: e.dma_start(out=X0[:], in_=g.zT[b, ch, :, CTX:TB]), reads=["scr"], writes=["X0"])
                    for ti in range(4):
                        bk = banks[ti]
                        ts_ = slice(ti * 512, (ti + 1) * 512)
                        S.op("dve", lambda e, j=j, bk=bk, ts_=ts_, ch=ch: e.scalar_tensor_tensor(out=UU[j][:, ts_], in0=UU[j][:, ts_], scalar=vec[:, 0, ch:ch + 1], in1=P[bk][:, 0:512],
                                                                                           op0=ALU.mult, op1=ALU.add), reads=["UU%d" % j, "vec", "P%d" % bk], writes=["UU%d" % j])
                    S.op("dve", lambda e, j=j: e.tensor_tensor(out=UU[j][:], in0=UU[j][:], in1=X0[:], op=ALU.mult), reads=["UU%d" % j, "X0"], writes=["UU%d" % j])
                    S.op("pool", lambda e, j=j: e.tensor_tensor(out=Ub[:], in0=UU[j][:], in1=UU[j][:], op=ALU.mult), reads=["UU%d" % j], writes=["Ub"])
                    for ti in range(4):
                        bk = 5 + ti % 3
                        ts_ = slice(ti * 512, (ti + 1) * 512)
                        S.op("pe", lambda e, bk=bk, ts_=ts_: e.matmul(P[bk][:, 0:512], lhsT=blk[:], rhs=Ub[:, ts_], start=True, stop=True), reads=["blk", "Ub"], writes=["P%d" % bk])
                        S.op("dve", lambda e, bk=bk, ts_=ts_: e.tensor_scalar(out=X0[:, ts_], in0=P[bk][:, 0:512], scalar1=1.0 / 64, scalar2=EPS, op0=ALU.mult, op1=ALU.add),
                             reads=["P%d" % bk], writes=["X0"])
                    S.op("act", lambda e: e.sqrt(out=X0[:], in_=X0[:]), reads=["X0"], writes=["X0"])
                    S.op("dve", lambda e: e.reciprocal(out=X0[:], in_=X0[:]), reads=["X0"], writes=["X0"])
                    S.op("dve", lambda e, j=j, ch=ch: e.scalar_tensor_tensor(out=osb[:], in0=UU[j][:], scalar=vec[:, 1, ch:ch + 1], in1=X0[:], op0=ALU.mult, op1=ALU.mult),
                         reads=["UU%d" % j, "vec", "X0"], writes=["osb"])
                    S.dma(lambda e, b=b, ch=ch: e.dma_start(out=g.mixT[b, ch], in_=osb[:]), reads=["osb"], writes=["mixscr"])
                    if stage in ("dbgHY", "simHY"):
                        S.dma(lambda e, b=b, ch=ch: e.dma_start(out=g.dbg_hyo[b, ch], in_=osb[:]), reads=["osb"])
        S.flush()


NE = 32
TBK = 1024
NTOK = BPC * SEQ


def declare_F(nc, g, stage):
    dt = nc.dram_tensor
    inp = lambda name, shape, dtype=F32: dt(name, list(shape), dtype, kind="ExternalInput").ap()
    g.out_w = inp("out_w", [D, D])
    g.g2row = inp("g2row", [1, D])
    g.router_w = inp("router_w", [D, NE])
    g.router_b = inp("router_b", [1, NE])
    g.ex_wg = inp("ex_w_gate", [NE, D, D])
    g.ex_wu = inp("ex_w_up", [NE, D, D])
    g.ex_wd = inp("ex_w_down", [NE, D, D])
    g.ex_bgu = inp("ex_bgu", [NE, 128, 2, KC])
    g.ex_bd = inp("ex_b_down", [NE, D])
    kd = {"kind": "ExternalOutput"} if F_DBG else {}
    g.x1_scr = dt("x1_scr", [NTOK, D], F32, **kd).ap()
    g.h2T_scr = dt("h2T_scr", [KC, 128, NTOK], BF16, **kd).ap()
    if stage == "simF" or F_DBG:
        g.dbg_gates = dt("dbg_gates", [128, NTOK // 128, NE], F32, kind="ExternalOutput").ap()


def phase_F(nc, g, stage, nexp=NE, nblk=NTOK // TBK):
    with contextlib.ExitStack() as st:
        S = g.S
        S.barrier()
        sb = lambda name, shape, dtype: st.enter_context(nc.sbuf_tensor("fs_" + name, shape, dtype))
        PB = st.enter_context(nc.psum_tensor("fpbf", [128, 1024], BF16))
        P = [None] + [st.enter_context(nc.psum_tensor("fps%d" % i, [128, 512], F32)) for i in range(1, 8)]
        rot = [0]
        ROT = [1, 2, 3, 4, 5, 6, 7]

        def nbk():
            rot[0] = (rot[0] + 1) % len(ROT)
            return ROT[rot[0]]
        gates = sb("gates", [128, NTOK // 128, NE], F32)
        identf = sb("identf", [128, 128], F32)
        fg = sb("fg", [128, D], F32)
        m5t = sb("m5t", [128, D], F32)
        S.dma(lambda e: e.dma_start(out=identf[:], in_=g.ident_f[:]), writes=["identf"])
        S.dma(lambda e: e.dma_start(out=fg[:], in_=g.final_g[0, :].partition_broadcast(128)), writes=["fg"])
        with contextlib.ExitStack() as s1:
            sb1 = lambda name, shape, dtype: s1.enter_context(nc.sbuf_tensor("f1_" + name, shape, dtype))
            ow = sb1("ow", [128, KC, D], BF16)
            for kc in range(KC):
                S.dma(lambda e, kc=kc: e.dma_start(out=ow[:, kc, :], in_=g.out_w[kc * 128:(kc + 1) * 128, :]), writes=["ow"], q="pool")
            rw_ = sb1("rw", [128, KC, NE], F32)
            S.dma(lambda e: e.dma_start(out=rw_[:], in_=g.router_w.rearrange("(kc p) e -> p kc e", p=128)), writes=["rw"])
            rb = sb1("rb", [128, NE], F32)
            S.dma(lambda e: e.dma_start(out=rb[:], in_=g.router_b[0, :].partition_broadcast(128)), writes=["rb"])
            m2 = sb1("m2", [128, D], F32)
            A3 = sb1("A3", [128, D], F32)
            B3 = sb1("B3", [128, D], F32)
            mixt = [sb1("mixt%d" % i, [128, KC, 128], BF16) for i in range(2)]
            xt = [sb1("xt%d" % i, [128, D], F32) for i in range(2)]
            h2 = sb1("h2", [128, D], F32)
            junk = sb1("junk", [128, D], F32)
            h2Tf = sb1("h2Tf", [128, KC, 128], F32)
            h2Tb = sb1("h2Tb", [128, KC, 128], BF16)
            sm = sb1("sm", [128, 16], F32)
            lg = sb1("lg", [128, NE], F32)
            ex = sb1("ex", [128, NE], F32)
            it = 0
            for b in range(BPC):
                S.dma(lambda e, b=b: e.dma_start(out=m2[:], in_=g.mod_row[b, 2 * D:3 * D].partition_broadcast(128)), reads=["modrow"], writes=["m2"])
                S.dma(lambda e, b=b: e.dma_start(out=A3[:], in_=g.mod_row[b, 4 * D:5 * D].partition_broadcast(128)), reads=["modrow"], writes=["A3"])
                S.dma(lambda e, b=b: e.dma_start(out=B3[:], in_=g.mod_row[b, 3 * D:4 * D].partition_broadcast(128)), reads=["modrow"], writes=["B3"])
                S.dma(lambda e: e.dma_start(out=junk[:], in_=g.g2row[0, :].partition_broadcast(128)), writes=["junk"])
                S.op("dve", lambda e: e.scalar_tensor_tensor(out=A3[:], in0=A3[:], scalar=1.0, in1=junk[:], op0=ALU.add, op1=ALU.mult), reads=["A3", "junk"], writes=["A3"])
                for t in range(SEQ // 128):
                    if t % 8 == 0:
                        S.flush()
                    p = it % 2
                    it += 1
                    gt = b * (SEQ // 128) + t
                    X, MX = "xt%d" % p, "mixt%d" % p
                    S.dma(lambda e, p=p, b=b, t=t: e.dma_start(out=xt[p][:], in_=g.x[b, t * 128:(t + 1) * 128, :]), writes=[X])
                    S.dma(lambda e, p=p, b=b, t=t: e.dma_start(out=mixt[p][:], in_=g.mixT[b, :, :, t * 128:(t + 1) * 128].rearrange("k p t -> p k t")),
                          reads=["mixscr"], writes=[MX])
                    for dj in range(4):
                        bk = nbk()
                        for kc in range(KC):
                            S.op("pe", lambda e, p=p, kc=kc, dj=dj, bk=bk: e.matmul(P[bk][:, 0:512], lhsT=mixt[p][:, kc, :], rhs=ow[:, kc, dj * 512:(dj + 1) * 512],
                                                                                   start=(kc == 0), stop=(kc == KC - 1)), reads=[MX, "ow"], writes=["P%d" % bk])
                        ds_ = slice(dj * 512, (dj + 1) * 512)
                        S.op("dve", lambda e, bk=bk, ds_=ds_: e.tensor_tensor(out=h2[:, ds_], in0=P[bk][:, 0:512], in1=m2[:, ds_], op=ALU.mult), reads=["P%d" % bk, "m2"], writes=["h2"])
                    S.op("dve", lambda e, p=p: e.tensor_tensor(out=xt[p][:], in0=xt[p][:], in1=h2[:], op=ALU.add), reads=[X, "h2"], writes=[X])
                    S.dma(lambda e, p=p, gt=gt: e.dma_start(out=g.x1_scr[gt * 128:(gt + 1) * 128, :], in_=xt[p][:]), reads=[X], writes=["x1scr"])
                    S.op("act", lambda e, p=p: e.activation(out=junk[:], in_=xt[p][:], func=AF.Square), reads=[X], writes=["junk"])
                    S.op("dve", lambda e: e.tensor_reduce(out=sm[:, 0:1], in_=junk[:], axis=AX.X, op=ALU.add), reads=["junk"], writes=["sm"])
                    S.op("dve", lambda e: e.tensor_scalar(out=sm[:, 0:1], in0=sm[:, 0:1], scalar1=1.0 / D, scalar2=EPS, op0=ALU.mult, op1=ALU.add), reads=["sm"], writes=["sm"])
                    S.op("act", lambda e: e.sqrt(out=sm[:, 0:1], in_=sm[:, 0:1]), reads=["sm"], writes=["sm"])
                    S.op("dve", lambda e: e.reciprocal(out=sm[:, 0:1], in_=sm[:, 0:1]), reads=["sm"], writes=["sm"])
                    S.op("dve", lambda e, p=p: e.scalar_tensor_tensor(out=h2[:], in0=xt[p][:], scalar=sm[:, 0:1], in1=A3[:], op0=ALU.mult, op1=ALU.mult),
                         reads=[X, "sm", "A3"], writes=["h2"])
                    S.op("dve", lambda e: e.tensor_tensor(out=h2[:], in0=h2[:], in1=B3[:], op=ALU.add), reads=["h2", "B3"], writes=["h2"])
                    for kc in range(KC):
                        bk = nbk()
                        S.op("pe", lambda e, kc=kc, bk=bk: e.transpose(P[bk][:, 0:128], h2[:, kc * 128:(kc + 1) * 128], identf[:]), reads=["h2", "identf"], writes=["P%d" % bk])
                        S.op("act", lambda e, kc=kc, bk=bk: e.copy(out=h2Tf[:, kc, :], in_=P[bk][:, 0:128]), reads=["P%d" % bk], writes=["h2Tf"])
                    S.op("pool", lambda e: e.tensor_copy(out=h2Tb[:], in_=h2Tf[:]), reads=["h2Tf"], writes=["h2Tb"])
                    S.dma(lambda e, gt=gt: e.dma_start(out=g.h2T_scr[:, :, gt * 128:(gt + 1) * 128].rearrange("k p t -> p k t"), in_=h2Tb[:]), reads=["h2Tb"], writes=["h2scr"])
                    bk = nbk()
                    for kc in range(KC):
                        S.op("pe", lambda e, kc=kc, bk=bk: e.matmul(P[bk][:, 0:NE], lhsT=h2Tf[:, kc, :], rhs=rw_[:, kc, :], start=(kc == 0), stop=(kc == KC - 1)),
                             reads=["h2Tf", "rw"], writes=["P%d" % bk])
                    S.op("dve", lambda e, bk=bk: e.tensor_tensor(out=lg[:], in0=P[bk][:, 0:NE], in1=rb[:], op=ALU.add), reads=["P%d" % bk, "rb"], writes=["lg"])
                    S.op("dve", lambda e: e.max(out=sm[:, 8:16], in_=lg[:]), reads=["lg"], writes=["sm"])
                    S.op("dve", lambda e: e.tensor_scalar(out=sm[:, 1:2], in0=sm[:, 8:9], scalar1=-1.0, scalar2=None, op0=ALU.mult), reads=["sm"], writes=["sm"])
                    S.op("act", lambda e: e.activation(out=ex[:], in_=lg[:], func=AF.Exp, bias=sm[:, 1:2]), reads=["lg", "sm"], writes=["ex"])
                    S.op("dve", lambda e: e.scalar_tensor_tensor(out=ex[:], in0=lg[:], scalar=sm[:, 11:12], in1=ex[:], op0=ALU.is_ge, op1=ALU.mult),
                         reads=["lg", "sm", "ex"], writes=["ex"])
                    S.op("dve", lambda e: e.tensor_reduce(out=sm[:, 2:3], in_=ex[:], axis=AX.X, op=ALU.add), reads=["ex"], writes=["sm"])
                    S.op("dve", lambda e: e.reciprocal(out=sm[:, 2:3], in_=sm[:, 2:3]), reads=["sm"], writes=["sm"])
                    S.op("dve", lambda e, gt=gt: e.tensor_scalar(out=gates[:, gt, :], in0=ex[:], scalar1=sm[:, 2:3], scalar2=None, op0=ALU.mult), reads=["ex", "sm"], writes=["gates"])
        if stage == "simF" or F_DBG:
            S.dma(lambda e: e.dma_start(out=g.dbg_gates[:], in_=gates[:]), reads=["gates"])
        S.barrier()
        NT = TBK // 128
        hb = sb("hb", [128, KC, TBK], BF16)
        actT = sb("actT", [128, KC, TBK], BF16)
        acc = sb("acc", [128, NT, D], F32)
        wg = [sb("wg%d" % i, [128, KC, 128], BF16) for i in range(2)]
        wu = [sb("wu%d" % i, [128, KC, 128], BF16) for i in range(2)]
        wd = [sb("wd%d" % i, [128, KC, 256], BF16) for i in range(2)]
        bgu = sb("bgu", [128, 2, KC], F32)
        bdr = sb("bdr", [1, D], BF16)
        ones = sb("ones", [1, 128], BF16)
        Gt, Ut, St = sb("Gt", [128, 512], F32), sb("Ut", [128, 512], F32), sb("St", [128, 512], F32)
        S.op("pool", lambda e: e.memset(ones[:], 1.0), writes=["ones"])
        acc_x = sb("acc_x", [128, D], F32)
        fsm = sb("fsm", [128, 2], F32)
        wv = lambda w, e_: w[e_].rearrange("(kc p) f -> p kc f", p=128)
        wi = 0
        for blk_i in range(nblk):
            t0 = blk_i * TBK
            S.dma(lambda e, t0=t0: e.dma_start(out=hb[:], in_=g.h2T_scr[:, :, t0:t0 + TBK].rearrange("k p t -> p k t")), reads=["h2scr"], writes=["hb"])
            S.op("pool", lambda e: e.memset(acc[:], 0.0), writes=["acc"])
            bb = t0 // SEQ
            S.dma(lambda e, bb=bb: e.dma_start(out=m5t[:], in_=g.mod_row[bb, 5 * D:6 * D].partition_broadcast(128)), reads=["modrow"], writes=["m5t"])
            for ex_i in range(nexp):
                if ex_i % 4 == 0:
                    S.flush()
                S.dma(lambda e, ex_i=ex_i: e.dma_start(out=bgu[:], in_=g.ex_bgu[ex_i]), writes=["bgu"])
                S.dma(lambda e, ex_i=ex_i: e.dma_start(out=bdr[:], in_=g.ex_bd[ex_i:ex_i + 1, :]), writes=["bdr"], q="pool")
                for fs in range(D // 128):
                    p = wi % 2
                    wi += 1
                    S.dma(lambda e, p=p, ex_i=ex_i, fs=fs: e.dma_start(out=wg[p][:], in_=wv(g.ex_wg, ex_i)[:, :, fs * 128:(fs + 1) * 128]), writes=["wg%d" % p], q="pool")
                    S.dma(lambda e, p=p, ex_i=ex_i, fs=fs: e.dma_start(out=wu[p][:], in_=wv(g.ex_wu, ex_i)[:, :, fs * 128:(fs + 1) * 128]), writes=["wu%d" % p], q="pool")
                    for j in range(1):
                        fc = fs
                        for tg in range(TBK // 512):
                            ts_ = slice(tg * 512, (tg + 1) * 512)
                            kg, ku = nbk(), nbk()
                            for kc in range(KC):
                                S.op("pe", lambda e, p=p, j=j, kc=kc, ts_=ts_, kg=kg: e.matmul(P[kg][:, 0:512], lhsT=wg[p][:, kc, j * 128:(j + 1) * 128], rhs=hb[:, kc, ts_],
                                                                                              start=(kc == 0), stop=(kc == KC - 1)), reads=["wg%d" % p, "hb"], writes=["P%d" % kg])
                            for kc in range(KC):
                                S.op("pe", lambda e, p=p, j=j, kc=kc, ts_=ts_, ku=ku: e.matmul(P[ku][:, 0:512], lhsT=wu[p][:, kc, j * 128:(j + 1) * 128], rhs=hb[:, kc, ts_],
                                                                                              start=(kc == 0), stop=(kc == KC - 1)), reads=["wu%d" % p, "hb"], writes=["P%d" % ku])
                            S.op("dve", lambda e, kg=kg, fc=fc: e.tensor_scalar(out=Gt[:], in0=P[kg][:, 0:512], scalar1=bgu[:, 0, fc:fc + 1], scalar2=7.0, op0=ALU.add, op1=ALU.min),
                                 reads=["P%d" % kg, "bgu"], writes=["Gt"])
                            S.op("act", lambda e: e.activation(out=St[:], in_=Gt[:], func=AF.Sigmoid, scale=1.702), reads=["Gt"], writes=["St"])
                            S.op("dve", lambda e, ku=ku, fc=fc: e.tensor_scalar(out=Ut[:], in0=P[ku][:, 0:512], scalar1=bgu[:, 1, fc:fc + 1], scalar2=7.0, op0=ALU.add, op1=ALU.min),
                                 reads=["P%d" % ku, "bgu"], writes=["Ut"])
                            S.op("dve", lambda e: e.tensor_scalar(out=Ut[:], in0=Ut[:], scalar1=-7.0, scalar2=1.0, op0=ALU.max, op1=ALU.add), reads=["Ut"], writes=["Ut"])
                            S.op("dve", lambda e: e.tensor_tensor(out=Gt[:], in0=Gt[:], in1=St[:], op=ALU.mult), reads=["Gt", "St"], writes=["Gt"])
                            S.op("dve", lambda e, fc=fc, ts_=ts_: e.tensor_tensor(out=actT[:, fc, ts_], in0=Ut[:], in1=Gt[:], op=ALU.mult), reads=["Ut", "Gt"], writes=["actT"])
                for dj in range(8):
                    p = dj % 2
                    S.dma(lambda e, p=p, ex_i=ex_i, dj=dj: e.dma_start(out=wd[p][:], in_=wv(g.ex_wd, ex_i)[:, :, dj * 256:(dj + 1) * 256]), writes=["wd%d" % p], q="pool")
                    ds_ = slice(dj * 256, (dj + 1) * 256)
                    for tt in range(NT):
                        bk = nbk()
                        gt = blk_i * NT + tt
                        for fc in range(KC):
                            S.op("pe", lambda e, p=p, fc=fc, tt=tt, bk=bk: e.matmul(P[bk][:, 0:256], lhsT=actT[:, fc, tt * 128:(tt + 1) * 128], rhs=wd[p][:, fc, :],
                                                                                   start=(fc == 0), stop=False), reads=["actT", "wd%d" % p], writes=["P%d" % bk])
                        S.op("pe", lambda e, bk=bk, ds_=ds_: e.matmul(P[bk][:, 0:256], lhsT=ones[0:1, :], rhs=bdr[0:1, ds_], start=False, stop=True),
                             reads=["ones", "bdr"], writes=["P%d" % bk])
                        S.op("dve", lambda e, bk=bk, tt=tt, ds_=ds_, gt=gt, ex_i=ex_i: e.scalar_tensor_tensor(
                            out=acc[:, tt, ds_], in0=P[bk][:, 0:256], scalar=gates[:, gt, ex_i:ex_i + 1], in1=acc[:, tt, ds_], op0=ALU.mult, op1=ALU.add),
                            reads=["P%d" % bk, "gates", "acc"], writes=["acc"])
            S.flush()
            for tt in range(NT):
                gt = blk_i * NT + tt
                b = gt // (SEQ // 128)
                S.dma(lambda e, gt=gt: e.dma_start(out=acc_x[:], in_=g.x1_scr[gt * 128:(gt + 1) * 128, :]), reads=["x1scr"], writes=["accx"])
                S.op("dve", lambda e, tt=tt, b=b: e.tensor_tensor(out=acc[:, tt, :], in0=acc[:, tt, :], in1=m5t[:], op=ALU.mult), reads=["acc", "m5t"], writes=["acc"])
                S.op("dve", lambda e, tt=tt: e.tensor_tensor(out=acc[:, tt, :], in0=acc[:, tt, :], in1=acc_x[:], op=ALU.add), reads=["acc", "accx"], writes=["acc"])
                S.op("act", lambda e, tt=tt: e.activation(out=acc_x[:], in_=acc[:, tt, :], func=AF.Square), reads=["acc"], writes=["accx"])
                S.op("dve", lambda e: e.tensor_reduce(out=fsm[:, 0:1], in_=acc_x[:], axis=AX.X, op=ALU.add), reads=["accx"], writes=["fsm"])
                S.op("dve", lambda e: e.tensor_scalar(out=fsm[:, 0:1], in0=fsm[:, 0:1], scalar1=1.0 / D, scalar2=EPS, op0=ALU.mult, op1=ALU.add), reads=["fsm"], writes=["fsm"])
                S.op("act", lambda e: e.sqrt(out=fsm[:, 0:1], in_=fsm[:, 0:1]), reads=["fsm"], writes=["fsm"])
                S.op("dve", lambda e: e.reciprocal(out=fsm[:, 0:1], in_=fsm[:, 0:1]), reads=["fsm"], writes=["fsm"])
                S.op("dve", lambda e, tt=tt: e.scalar_tensor_tensor(out=acc[:, tt, :], in0=acc[:, tt, :], scalar=fsm[:, 0:1], in1=fg[:], op0=ALU.mult, op1=ALU.mult),
                     reads=["acc", "fsm", "fg"], writes=["acc"])
                t_in_b = gt % (SEQ // 128)
                S.dma(lambda e, tt=tt, b=b, t_in_b=t_in_b: e.dma_start(out=g.out[b, t_in_b * 128:(t_in_b + 1) * 128, :], in_=acc[:, tt, :]), reads=["acc"])
        S.flush()
```

```python
import contextlib
import numpy as np
import ml_dtypes
import concourse.bass as bass
import concourse.mybir as mybir
from concourse.bass_utils import run_bass_kernel_spmd

F32 = mybir.dt.float32
BF16 = mybir.dt.bfloat16
I32 = mybir.dt.int32
ALU = mybir.AluOpType
AF = mybir.ActivationFunctionType
AX = mybir.AxisListType

NCORES = 8
D = 2048
KC = 16
SEQ = 2048
CTX = 256
TB = SEQ + CTX
BPC = 2
EPS = 1e-6


class Sched:
    ENGS = ("pe", "act", "dve", "pool", "sp")

    _uid = [0]

    def __init__(self, nc, stack, ndma=24):
        self.nc = nc
        Sched._uid[0] += 1
        u = "s%d_" % Sched._uid[0]
        self.sem = {e: stack.enter_context(nc.semaphore(u + "pg_" + e)) for e in self.ENGS}
        self.dsem = [stack.enter_context(nc.semaphore(u + "dq%d" % i)) for i in range(ndma)]
        self.dval = [0] * ndma
        self.dnext = {"sp": 0, "pool": 0}
        self.dpool = {"sp": list(range(0, ndma // 2)), "pool": list(range(ndma // 2, ndma))}
        self.cnt = {e: 0 for e in self.ENGS}
        self.prog = {e: [] for e in self.ENGS}
        self.waited = {e: {} for e in self.ENGS}
        self.res = {}

    def _r(self, k):
        r = self.res.get(k)
        if r is None:
            r = self.res[k] = {"w": None, "r": {}}
        return r

    def _deps(self, eng, reads, writes):
        need = {}

        def add(sk, v):
            if sk == "pe" and eng == "pe":
                return
            if need.get(sk, 0) < v:
                need[sk] = v
        for k in reads:
            w = self._r(k)["w"]
            if w:
                add(*w)
        for k in writes:
            r = self._r(k)
            if r["w"]:
                add(*r["w"])
            for sk, v in r["r"].items():
                add(sk, v)
        out = []
        wd = self.waited[eng]
        for sk, v in need.items():
            if wd.get(sk, 0) >= v:
                continue
            wd[sk] = v
            out.append((sk, v))
        return out

    def _mark(self, reads, writes, sk, v):
        for k in reads:
            self._r(k)["r"][sk] = v
        for k in writes:
            r = self._r(k)
            r["w"] = (sk, v)
            r["r"] = {}

    alias = {}

    def _x(self, keys):
        out = []
        for k in keys:
            out.extend(self.alias.get(k, (k,)))
        return out

    def op(self, eng, fn, reads=(), writes=()):
        reads, writes = self._x(reads), self._x(writes)
        deps = self._deps(eng, reads, writes)
        self.cnt[eng] += 1
        n = self.cnt[eng]
        self.prog[eng].append((deps, fn, (eng, 1)))
        self._mark(reads, writes, eng, n)

    def dma(self, fn, reads=(), writes=(), q="sp"):
        reads, writes = self._x(reads), self._x(writes)
        pool = self.dpool[q]
        i = pool[self.dnext[q]]
        self.dnext[q] = (self.dnext[q] + 1) % len(pool)
        sk = ("d", i)
        deps = self._deps(q, reads, writes)
        if self.dval[i] > 0 and self.waited[q].get(sk, 0) < self.dval[i]:
            self.waited[q][sk] = self.dval[i]
            deps.append((sk, self.dval[i]))
        self.dval[i] += 16
        self.prog[q].append((deps, fn, (sk, 16)))
        self._mark(reads, writes, sk, self.dval[i])

    def barrier(self):
        for eng in self.ENGS:
            deps = []
            for e in self.ENGS:
                if e != eng and self.cnt[e] and self.waited[eng].get(e, 0) < self.cnt[e]:
                    deps.append((e, self.cnt[e]))
                    self.waited[eng][e] = self.cnt[e]
            for i, v in enumerate(self.dval):
                if v and self.waited[eng].get(("d", i), 0) < v:
                    deps.append((("d", i), v))
                    self.waited[eng][("d", i)] = v
            if deps:
                self.prog[eng].append((deps, None, None))

    def drain(self, eng="sp"):
        deps = []
        for e in self.ENGS:
            if self.cnt[e] and self.waited[eng].get(e, 0) < self.cnt[e] and e != eng:
                deps.append((e, self.cnt[e]))
        for i, v in enumerate(self.dval):
            if v and self.waited[eng].get(("d", i), 0) < v:
                deps.append((("d", i), v))
        self.prog[eng].append((deps, None, None))

    def flush(self, final=False):
        if final:
            self.barrier()
        with self.nc.Block() as block:
            self.emit(block)
        self.prog = {e: [] for e in self.ENGS}

    def _semh(self, sk):
        return self.sem[sk] if isinstance(sk, str) else self.dsem[sk[1]]

    def emit(self, block):
        def run(eng_handle, name):
            for deps, fn, inc in self.prog[name]:
                for sk, v in deps:
                    eng_handle.wait_ge(self._semh(sk), v)
                if fn is not None:
                    ins = fn(eng_handle)
                    ins.then_inc(self._semh(inc[0]), inc[1])

        @block.sync
        def _(e):
            run(e, "sp")

        @block.tensor
        def _(e):
            run(e, "pe")

        @block.vector
        def _(e):
            run(e, "dve")

        @block.scalar
        def _(e):
            run(e, "act")

        @block.gpsimd
        def _(e):
            run(e, "pool")


PW = 6144
NZ = PW // 128
NLORA = 3


def _fm(v, nch):
    return np.ascontiguousarray(np.asarray(v, np.float32).reshape(nch, 128).T)


class Ctx:
    pass


def declare_io(nc, stage):
    g = Ctx()
    dt = nc.dram_tensor
    inp = lambda name, shape, dtype=F32: dt(name, list(shape), dtype, kind="ExternalInput").ap()
    g.x = inp("x", [BPC, SEQ, D])
    g.ctx = inp("ctx", [BPC, CTX, D])
    g.condT = inp("condT", [128, KC, 3])
    g.ada_w = inp("ada_w", [D, 6 * D])
    g.ada_bT = inp("ada_bT", [128, 6 * KC])
    g.g1T = inp("g1T", [128, KC])
    g.g2T = inp("g2T", [128, KC])
    g.in_w = inp("in_w", [D, PW])
    g.lora_w = inp("lora_w", [D, NLORA * 128])
    g.conv_wT = inp("conv_wT", [128, NZ, 3])
    g.conv_bT = inp("conv_bT", [128, NZ])
    g.final_g = inp("final_g", [1, D])
    g.ident_bf = inp("ident_bf", [128, 128], BF16)
    g.ident_f = inp("ident_f", [128, 128], F32)
    g.out = dt("out", [BPC, SEQ, D], F32, kind="ExternalOutput").ap()
    g.ada_brow = inp("ada_brow", [1, 6 * D])
    g.mod_row = (dt("mod_row_scr", [3, 6 * D], F32, kind="ExternalOutput") if stage == "dbgA" else dt("mod_row_scr", [3, 6 * D], F32)).ap()
    g.zT = dt("zT_scr", [BPC, NZ, 128, TB], F32).ap()
    g.loraT = dt("loraT_scr", [BPC, NLORA, 128, TB], F32).ap()
    if stage == "dbgA":
        g.dbg_mod = dt("dbg_mod", [128, 6 * KC, 3], F32, kind="ExternalOutput").ap()
    if stage in ("dbgC",):
        g.dbg_z = dt("dbg_z", [BPC, NZ, 128, TB], F32, kind="ExternalOutput").ap()
        g.dbg_lora = dt("dbg_lora", [BPC, NLORA, 128, TB], F32, kind="ExternalOutput").ap()
    return g


def phase_ABC(nc, g, stage):
    with contextlib.ExitStack() as st:
        S = g.S
        S.barrier()
        sb = lambda name, shape, dtype: st.enter_context(nc.sbuf_tensor(name, shape, dtype))
        ps = [st.enter_context(nc.psum_tensor("ps%d" % i, [128, 512], F32)) for i in range(6)]
        pbf = [st.enter_context(nc.psum_tensor("pbf%d" % i, [128, 1024], BF16)) for i in range(1)]
        pbf_row = st.enter_context(nc.psum_tensor("pbf_row", [128, 512], F32))
        modT = sb("modT", [128, 6 * KC, 3], F32)
        condT = sb("condT_s", [128, KC, 3], F32)
        adab = sb("adab", [128, 6 * KC], F32)
        g1T = sb("g1T_s", [128, KC], F32)
        sc1 = sb("sc1", [128, KC, 3], F32)
        identb = sb("identb", [128, 128], BF16)
        cw = sb("cw", [128, NZ, 3], F32)
        cb = sb("cb", [128, NZ], F32)
        S.dma(lambda e: e.dma_start(out=condT[:], in_=g.condT[:]), writes=["condT"])
        S.dma(lambda e: e.dma_start(out=adab[:], in_=g.ada_bT[:]), writes=["adab"])
        S.dma(lambda e: e.dma_start(out=g1T[:], in_=g.g1T[:]), writes=["g1T"])
        S.dma(lambda e: e.dma_start(out=identb[:], in_=g.ident_bf[:]), writes=["identb"])
        S.dma(lambda e: e.dma_start(out=cw[:], in_=g.conv_wT[:]), writes=["cw"])
        S.dma(lambda e: e.dma_start(out=cb[:], in_=g.conv_bT[:]), writes=["cb"])
        S.op("act", lambda e: e.activation(out=condT[:], in_=condT[:], func=AF.Silu), reads=["condT"], writes=["condT"])
        with contextlib.ExitStack() as stA:
            aw = [stA.enter_context(nc.sbuf_tensor("aw%d" % i, [128, KC, 512], F32)) for i in range(2)]
            mrow = stA.enter_context(nc.sbuf_tensor("mrow", [3, 6 * D], F32))
            S.dma(lambda e: e.dma_start(out=mrow[:], in_=g.ada_brow[0, :].partition_broadcast(3)), writes=["mrow"])
            ada_v = g.ada_w.rearrange("(kc p) f -> p kc f", p=128)
            for sl in range(24):
                p = sl % 2
                S.dma(lambda e, p=p, sl=sl: e.dma_start(out=aw[p][:], in_=ada_v[:, :, sl * 512:(sl + 1) * 512]),
                      writes=["aw%d" % p])
                for kc in range(KC):
                    S.op("pe", lambda e, p=p, kc=kc: e.matmul(pbf_row[0:3, 0:512], lhsT=condT[:, kc, :], rhs=aw[p][:, kc, :],
                                                              start=(kc == 0), stop=(kc == KC - 1)), reads=["aw%d" % p, "condT"], writes=["prow"])
                S.op("dve", lambda e, sl=sl: e.tensor_tensor(out=mrow[:, sl * 512:(sl + 1) * 512], in0=pbf_row[0:3, 0:512], in1=mrow[:, sl * 512:(sl + 1) * 512], op=ALU.add),
                     reads=["prow", "mrow"], writes=["mrow"])
                for j in range(4):
                    ch = sl * 4 + j
                    pb = ch % 6
                    for kc in range(KC):
                        S.op("pe", lambda e, p=p, j=j, kc=kc, pb=pb: e.matmul(
                            ps[pb][:, 0:3], lhsT=aw[p][:, kc, j * 128:(j + 1) * 128], rhs=condT[:, kc, :],
                            start=(kc == 0), stop=(kc == KC - 1)),
                            reads=["aw%d" % p, "condT"], writes=["ps%d" % pb])
                    S.op("dve", lambda e, ch=ch, pb=pb: e.tensor_scalar(
                        out=modT[:, ch, :], in0=ps[pb][:, 0:3], scalar1=adab[:, ch:ch + 1], scalar2=None, op0=ALU.add),
                        reads=["ps%d" % pb, "adab"], writes=["modT"])
            S.dma(lambda e: e.dma_start(out=g.mod_row[:], in_=mrow[:]), reads=["mrow"], writes=["modrow"])
        S.barrier()
        if stage == "dbgA":
            S.dma(lambda e: e.dma_start(out=g.dbg_mod[:], in_=modT[:]), reads=["modT"])
        for n in range(3):
            S.op("dve", lambda e, n=n: e.scalar_tensor_tensor(
                out=sc1[:, :, n], in0=modT[:, KC:2 * KC, n], scalar=1.0, in1=g1T[:], op0=ALU.add, op1=ALU.mult),
                reads=["modT", "g1T"], writes=["sc1"])
        if stage != "dbgA":
            hT = sb("hT", [128, KC, TB], BF16)
            xt = [sb("xt%d" % i, [128, D], F32) for i in range(2)]
            xn = [sb("xn%d" % i, [128, D], BF16) for i in range(2)]
            junk = sb("junk", [128, D], F32)
            ssq = [sb("ssq%d" % i, [128, 1], F32) for i in range(2)]
            wsl = [sb("wsl%d" % i, [128, KC, 256], BF16) for i in range(2)]
            zraw = [sb("zraw%d" % i, [128, TB], F32) for i in range(2)]
            zc = [sb("zc%d" % i, [128, TB], F32) for i in range(2)]
            inw_v = g.in_w.rearrange("(kc p) f -> p kc f", p=128)
            low_v = g.lora_w.rearrange("(kc p) f -> p kc f", p=128)
            it = 0
            for b in range(BPC):
                for t in range(TB // 128):
                    p = it % 2
                    it += 1
                    if t < CTX // 128:
                        src = g.ctx[b, t * 128:(t + 1) * 128, :]
                        n = 2
                    else:
                        src = g.x[b, (t - 2) * 128:(t - 1) * 128, :]
                        n = b
                    X, XN, SS = "xt%d" % p, "xn%d" % p, "ssq%d" % p
                    S.dma(lambda e, p=p, src=src: e.dma_start(out=xt[p][:], in_=src), writes=[X])
                    S.op("act", lambda e, p=p: e.activation(out=junk[:], in_=xt[p][:], func=AF.Square), reads=[X], writes=["junk"])
                    S.op("dve", lambda e, p=p: e.tensor_reduce(out=ssq[p][:], in_=junk[:], axis=AX.X, op=ALU.add), reads=["junk"], writes=[SS])
                    S.op("dve", lambda e, p=p: e.tensor_scalar(out=ssq[p][:], in0=ssq[p][:], scalar1=1.0 / D, scalar2=EPS,
                                                               op0=ALU.mult, op1=ALU.add), reads=[SS], writes=[SS])
                    S.op("act", lambda e, p=p: e.sqrt(out=ssq[p][:], in_=ssq[p][:]), reads=[SS], writes=[SS])
                    S.op("dve", lambda e, p=p: e.reciprocal(out=ssq[p][:], in_=ssq[p][:]), reads=[SS], writes=[SS])
                    S.op("dve", lambda e, p=p: e.tensor_scalar(out=xn[p][:], in0=xt[p][:], scalar1=ssq[p][:], scalar2=None,
                                                               op0=ALU.mult), reads=[X, SS], writes=[XN])
                    for kc in range(KC):
                        q = 0
                        S.op("pe", lambda e, p=p, kc=kc, q=q: e.transpose(
                            pbf[q][:, (kc % 8) * 128:(kc % 8 + 1) * 128], xn[p][:, kc * 128:(kc + 1) * 128], identb[:]),
                            reads=[XN, "identb"], writes=["pbf%d" % q])
                        if kc % 8 == 7:
                            for k2 in range(kc - 7, kc + 1):
                                S.op("act", lambda e, k2=k2, q=q, n=n, t=t: e.activation(
                                    out=hT[:, k2, t * 128:(t + 1) * 128], in_=pbf[q][:, (k2 % 8) * 128:(k2 % 8 + 1) * 128],
                                    func=AF.Identity, scale=sc1[:, k2, n:n + 1], bias=modT[:, k2, n:n + 1]),
                                    reads=["pbf%d" % q, "sc1", "modT"], writes=["hT"])
                S.flush()
                nsl = PW // 256 + 2
                for sl in range(nsl):
                    if sl % 8 == 0 and sl > 0:
                        S.flush()
                    p = sl % 2
                    if sl < PW // 256:
                        srcw = inw_v[:, :, sl * 256:(sl + 1) * 256]
                        ncol = 256
                    elif sl == PW // 256:
                        srcw = low_v[:, :, 0:256]
                        ncol = 256
                    else:
                        srcw = low_v[:, :, 256:384]
                        ncol = 128
                    S.dma(lambda e, p=p, srcw=srcw, ncol=ncol: e.dma_start(out=wsl[p][:, :, 0:ncol], in_=srcw),
                          writes=["wsl%d" % p], q="pool")
                    for j in range(ncol // 128):
                        ch = sl * 2 + j
                        zp = ch % 2
                        Z, ZC = "zraw%d" % zp, "zc%d" % zp
                        toks = [(0, 256)] + [(256 + 512 * i, 512) for i in range(4)]
                        for ti, (t0, tn) in enumerate(toks):
                            pb = (ch * 5 + ti) % 6
                            for kc in range(KC):
                                S.op("pe", lambda e, p=p, j=j, kc=kc, pb=pb, t0=t0, tn=tn: e.matmul(
                                    ps[pb][:, 0:tn], lhsT=wsl[p][:, kc, j * 128:(j + 1) * 128], rhs=hT[:, kc, t0:t0 + tn],
                                    start=(kc == 0), stop=(kc == KC - 1)),
                                    reads=["wsl%d" % p, "hT"], writes=["ps%d" % pb])
                            if ch < NZ:
                                S.op("act", lambda e, zp=zp, pb=pb, t0=t0, tn=tn: e.copy(out=zraw[zp][:, t0:t0 + tn], in_=ps[pb][:, 0:tn]),
                                     reads=["ps%d" % pb], writes=[Z])
                            else:
                                fn = [AF.Tanh, AF.Identity, AF.Sigmoid][ch - NZ]
                                S.op("act", lambda e, zp=zp, pb=pb, t0=t0, tn=tn, fn=fn: e.activation(
                                    out=zc[zp][:, t0:t0 + tn], in_=ps[pb][:, 0:tn], func=fn),
                                    reads=["ps%d" % pb], writes=[ZC])
                        if ch < NZ:
                            S.op("dve", lambda e, zp=zp, ch=ch: e.tensor_scalar(
                                out=zc[zp][:], in0=zraw[zp][:], scalar1=cw[:, ch, 1:2], scalar2=cb[:, ch:ch + 1],
                                op0=ALU.mult, op1=ALU.add), reads=[Z, "cw", "cb"], writes=[ZC])
                            for (s0, s1) in ((0, CTX), (CTX, TB)):
                                S.op("dve", lambda e, zp=zp, ch=ch, s0=s0, s1=s1: e.scalar_tensor_tensor(
                                    out=zc[zp][:, s0 + 1:s1], in0=zraw[zp][:, s0:s1 - 1], scalar=cw[:, ch, 0:1],
                                    in1=zc[zp][:, s0 + 1:s1], op0=ALU.mult, op1=ALU.add), reads=[Z, ZC, "cw"], writes=[ZC])
                                S.op("dve", lambda e, zp=zp, ch=ch, s0=s0, s1=s1: e.scalar_tensor_tensor(
                                    out=zc[zp][:, s0:s1 - 1], in0=zraw[zp][:, s0 + 1:s1], scalar=cw[:, ch, 2:3],
                                    in1=zc[zp][:, s0:s1 - 1], op0=ALU.mult, op1=ALU.add), reads=[Z, ZC, "cw"], writes=[ZC])
                            dst = g.zT[b, ch]
                        else:
                            dst = g.loraT[b, ch - NZ]
                        S.dma(lambda e, zp=zp, dst=dst: e.dma_start(out=dst, in_=zc[zp][:]), reads=[ZC], writes=["scr"])
        S.flush()


def copy_dram(nc, g, pairs):
    with contextlib.ExitStack() as st:
        S = g.S
        S.barrier()
        bufs = [st.enter_context(nc.sbuf_tensor("cpb%d_%d" % (i, id(pairs) % 100000), [128, TB], F32)) for i in range(2)]
        it = 0
        for dst, src in pairs:
            for i in range(dst.shape[0]):
                for j in range(dst.shape[1]):
                    p = it % 2
                    it += 1
                    S.dma(lambda e, p=p, i=i, j=j, src=src: e.dma_start(out=bufs[p][:], in_=src[i, j]), writes=["b%d" % p])
                    S.dma(lambda e, p=p, i=i, j=j, dst=dst: e.dma_start(out=dst[i, j], in_=bufs[p][:]), reads=["b%d" % p])
        S.flush()


RW_NB, RW_NHP = BPC, 8
HY_NB = BPC
F_NEXP, F_NBLK = 32, 4
F_DBG = False


def build_program(stage="final"):
    nc = bass.Bass("TRN2", target_bir_lowering=False)
    g = declare_io(nc, stage)
    gstack = contextlib.ExitStack()
    g.S = Sched(nc, gstack, ndma=48)
    declare_rw(nc, g, stage)
    declare_hy(nc, g, stage)
    if stage in ("final", "simF"):
        declare_F(nc, g, stage)
    if stage not in ("simRW", "simHY", "simF"):
        phase_ABC(nc, g, stage)
    if stage in ("dbgHY", "simHY"):
        phase_HY(nc, g, stage, nb=HY_NB)
    if stage in ("dbgRW", "simRW"):
        phase_RW(nc, g, stage, nb=RW_NB, nhp=RW_NHP)
    if stage in ("final", "dbgMIX"):
        phase_RW(nc, g, stage)
        phase_HY(nc, g, stage)
    if stage == "final":
        phase_F(nc, g, stage, nexp=F_NEXP, nblk=F_NBLK)
    if stage == "simF":
        phase_F(nc, g, stage, nexp=F_NEXP, nblk=F_NBLK)
    if stage == "dbgC":
        copy_dram(nc, g, [(g.dbg_z, g.zT), (g.dbg_lora, g.loraT)])
    g.S.flush(final=True)
    gstack.close()
    return nc


def make_in_maps(inputs, cores=range(NCORES)):
    f = lambda k: np.asarray(inputs[k], np.float32)
    x, ctx, c, c_ctx = f("x"), f("ctx"), f("c"), f("c_ctx")
    lora_w = np.ascontiguousarray(np.concatenate(
        [f("rw_w1")[0, 0], f("rw_w1")[0, 1], f("rw_a1")[0, 0], f("rw_a1")[0, 1], f("rw_g1")[0]], axis=1))
    shared = {
        "ada_w": np.ascontiguousarray(f("ada_w")[0]),
        "ada_bT": _fm(f("ada_b")[0], 6 * KC),
        "g1T": _fm(f("norm1_g")[0], KC),
        "g2T": _fm(f("norm2_g")[0], KC),
        "in_w": np.ascontiguousarray(f("in_w")[0]),
        "lora_w": lora_w,
        "conv_wT": np.ascontiguousarray(f("conv_w")[0].reshape(3, NZ, 128).transpose(2, 1, 0)),
        "conv_bT": _fm(f("conv_b")[0], NZ),
        "final_g": np.ascontiguousarray(f("final_g").reshape(1, D)),
        "ident_bf": np.eye(128, dtype=np.float32).astype(ml_dtypes.bfloat16),
        "ident_f": np.eye(128, dtype=np.float32),
    }
    w2, a2 = f("rw_w2")[0], f("rw_a2")[0]
    w2pad = np.zeros((2, 128, 1024), np.float32)
    a2pad = np.zeros((2, 128, 1024), np.float32)
    for n in range(2):
        w2pad[n, n * 64:(n + 1) * 64] = w2[n]
        a2pad[n, n * 64:(n + 1) * 64] = a2[n]
    rwvec = np.stack([_fm(f("rw_w0")[0, 0], 8), _fm(f("rw_w0")[0, 1], 8), _fm(f("rw_a0")[0, 0], 8), _fm(f("rw_a0")[0, 1], 8),
                      _fm(f("rw_kk")[0], 8), _fm(f("rw_ka")[0], 8), _fm(f("rw_rk")[0].reshape(-1), 8),
                      _fm(f("rw_lnx_g")[0], 8), _fm(f("rw_lnx_b")[0], 8)], axis=1)
    shared.update({"w2pad": w2pad, "a2pad": a2pad, "rw_g2": np.ascontiguousarray(f("rw_g2")[0]),
                   "rwvec": np.ascontiguousarray(rwvec)})
    shared.update(rw_consts())
    shared.update(hy_consts())
    shared.update({"ada_brow": np.ascontiguousarray(f("ada_b")[0].reshape(1, -1)), "out_w": np.ascontiguousarray(f("out_w")[0]),
                   "g2row": np.ascontiguousarray(f("norm2_g")[0].reshape(1, D)), "router_w": np.ascontiguousarray(f("router_w")[0]),
                   "router_b": np.ascontiguousarray(f("router_b")[0].reshape(1, NE))})
    pcol = np.arange(128, dtype=np.float32)[:, None]
    tri = (np.arange(128)[:, None] < np.arange(128)[None, :]).astype(np.float32)
    base4 = 4.0 * (np.arange(KC, dtype=np.float32)[None, None, :] * 128 + pcol[:, :, None]) + np.arange(4, dtype=np.float32)[None, :, None]
    shared.update({"sp_tri": tri, "sp_ones": np.ones((128, 128), np.float32),
                   "sp_sidx": np.ascontiguousarray(np.broadcast_to(np.arange(NSB, dtype=np.float32)[None, :], (128, NSB))),
                   "sp_base4": np.ascontiguousarray(base4.astype(np.float32)), "sp_piota": np.ascontiguousarray(pcol)})
    if "ex_w_gate" in inputs:
        bgu = np.stack([f("ex_b_gate")[0].reshape(NE, KC, 128).transpose(0, 2, 1), f("ex_b_up")[0].reshape(NE, KC, 128).transpose(0, 2, 1)], axis=2)
        shared.update({"ex_w_gate": f("ex_w_gate")[0], "ex_w_up": f("ex_w_up")[0], "ex_w_down": f("ex_w_down")[0],
                       "ex_bgu": np.ascontiguousarray(bgu), "ex_b_down": np.ascontiguousarray(f("ex_b_down")[0])})
    shared.update({"hy_w1": np.ascontiguousarray(f("hy_w1")[0]), "hy_w2": np.ascontiguousarray(f("hy_w2")[0]),
                   "hy_w3": np.ascontiguousarray(f("hy_w3")[0]), "hy_w4": np.ascontiguousarray(f("hy_w4")[0]),
                   "hy_fb": np.ascontiguousarray(np.stack([f("hy_freq")[0], f("hy_b1")[0], f("hy_b2")[0], f("hy_b3")[0]], axis=1)),
                   "hy_vec": np.ascontiguousarray(np.stack([_fm(f("hy_bias")[0], 8), _fm(f("hy_norm_g")[0], 8)], axis=1))})
    maps = []
    for cid in cores:
        b0 = cid * BPC
        cond = np.stack([c[b0], c[b0 + 1], c_ctx], axis=0)
        condT = np.ascontiguousarray(cond.reshape(3, KC, 128).transpose(2, 1, 0))
        m = dict(shared)
        m["x"] = np.ascontiguousarray(x[b0:b0 + BPC])
        m["ctx"] = np.ascontiguousarray(ctx[b0:b0 + BPC])
        m["condT"] = condT
        maps.append(m)
    return maps


def kernel(**inputs):
    nc = build_program()
    in_maps = make_in_maps(inputs)
    res = run_bass_kernel_spmd(nc, in_maps, core_ids=list(range(NCORES)))
    return np.concatenate([r["out"] for r in res.results], axis=0)


NCH = TB // 64
C0 = float(np.exp(-0.5))


def rw_consts():
    j = np.arange(128)[:, None]
    t = np.arange(128)[None, :]
    m = {}
    m["rw_mask0"] = np.concatenate([(j < t), (j <= t)], axis=1).astype(np.float32)
    m["rw_mask1"] = np.concatenate([(j > t), (j >= t)], axis=1).astype(np.float32)
    m["rw_maskT0"] = (t < j).astype(np.float32)
    m["rw_maskT1"] = (t > j).astype(np.float32)
    blk = np.zeros((128, 128), np.float32)
    blk[:64, :64] = 1.0
    blk[64:, 64:] = 1.0
    m["rw_blk"] = blk
    rm = np.ones((128, TB), np.float32)
    rm[:, ::64] = 0.0
    m["rw_rm"] = rm
    return m


def declare_rw(nc, g, stage):
    dt = nc.dram_tensor
    inp = lambda name, shape, dtype=F32: dt(name, list(shape), dtype, kind="ExternalInput").ap()
    g.w2pad = inp("w2pad", [2, 128, 1024])
    g.a2pad = inp("a2pad", [2, 128, 1024])
    g.rw_g2 = inp("rw_g2", [128, 1024])
    g.rwvec = inp("rwvec", [128, 9, 8])
    for k in ("rw_mask0", "rw_mask1"):
        setattr(g, k, inp(k, [128, 256]))
    for k in ("rw_maskT0", "rw_maskT1", "rw_blk"):
        setattr(g, k, inp(k, [128, 128]))
    g.rw_rm = inp("rw_rm", [128, TB])
    g.mixT = (dt("mixT_scr", [BPC, KC, 128, SEQ], BF16, kind="ExternalOutput") if stage == "dbgMIX" else dt("mixT_scr", [BPC, KC, 128, SEQ], BF16)).ap()
    if stage in ("dbgRW", "simRW"):
        g.dbg_rwo = dt("dbg_rwo", [BPC, 8, 128, SEQ], BF16, kind="ExternalOutput").ap()


def phase_RW(nc, g, stage, nb=BPC, nhp=8):
    with contextlib.ExitStack() as st:
        S = g.S
        S.barrier()
        sb = lambda name, shape, dtype: st.enter_context(nc.sbuf_tensor("rws_" + name, shape, dtype))
        PB = st.enter_context(nc.psum_tensor("rpbf", [128, 1024], BF16))
        P = [None] + [st.enter_context(nc.psum_tensor("rps%d" % i, [128, 512], F32)) for i in range(1, 8)]
        ROT = [1, 2, 3, 4, 7]
        rot = [0]
        mask = [sb("mask%d" % n, [128, 256], BF16) for n in range(2)]
        maskT = [sb("maskT%d" % n, [128, 128], BF16) for n in range(2)]
        blk = sb("blk", [128, 128], BF16)
        identb = sb("identb", [128, 128], BF16)
        identf = sb("identf", [128, 128], F32)
        rm = sb("rm", [128, TB], BF16)
        w2b = [sb("w2b%d" % n, [128, 128], BF16) for n in range(2)]
        a2b = [sb("a2b%d" % n, [128, 128], BF16) for n in range(2)]
        g2b = sb("g2b", [128, 128], BF16)
        vec = sb("rwvec_s", [128, 9, 8], F32)
        for n in range(2):
            S.dma(lambda e, n=n: e.dma_start(out=mask[n][:], in_=getattr(g, "rw_mask%d" % n)[:]), writes=["mask%d" % n], q="pool")
            S.dma(lambda e, n=n: e.dma_start(out=maskT[n][:], in_=getattr(g, "rw_maskT%d" % n)[:]), writes=["maskT%d" % n], q="pool")
        S.dma(lambda e: e.dma_start(out=blk[:], in_=g.rw_blk[:]), writes=["blk"], q="pool")
        S.dma(lambda e: e.dma_start(out=identb[:], in_=g.ident_bf[:]), writes=["identb"])
        S.dma(lambda e: e.dma_start(out=identf[:], in_=g.ident_f[:]), writes=["identf"])
        S.dma(lambda e: e.dma_start(out=rm[:], in_=g.rw_rm[:]), writes=["rm"], q="pool")
        S.dma(lambda e: e.dma_start(out=vec[:], in_=g.rwvec[:]), writes=["vec"])
        L = [sb("L%d" % i, [128, TB], BF16) for i in range(3)]
        R_, K_, V_ = sb("R_", [128, TB], F32), sb("K_", [128, TB], F32), sb("V_", [128, TB], F32)
        KKN = sb("KKN", [128, TB], F32)
        SG, AN, CUM = sb("SG", [128, TB], F32), sb("AN", [128, TB], F32), sb("CUM", [128, TB], F32)
        E1, E2 = sb("E1", [128, TB], F32), sb("E2", [128, TB], F32)
        KD = SG
        TBF = sb("TBF", [128, TB], BF16)
        TOT = sb("TOT", [128, NCH], F32)
        GAM = [sb("GAM%d" % n, [128, NCH], F32) for n in range(2)]
        arT = [sb("arT%d" % n, [128, NCH, 256], BF16) for n in range(2)]
        bT = [sb("bT%d" % n, [128, NCH, 128], BF16) for n in range(2)]
        kT = [sb("kT%d" % n, [128, NCH, 128], BF16) for n in range(2)]
        vT = sb("vT", [128, NCH, 128], BF16)
        yacc = sb("yacc", [128, 32, 64], F32)
        s1, s2 = sb("s1", [128, 32], F32), sb("s2", [128, 32], F32)
        TM = [sb("TM%d" % n, [128, 512], BF16) for n in range(2)]
        GB = [sb("GB%d" % n, [128, 256], BF16) for n in range(2)]
        GK = [sb("GK%d" % n, [128, 256], BF16) for n in range(2)]
        PW_ = [[sb("PW%d_%d" % (n, i), [128, 256], BF16) for i in range(2)] for n in range(2)]
        XX = [[sb("XX%d_%d" % (n, i), [128, 256], BF16) for i in range(2)] for n in range(2)]
        MT = [sb("MT%d" % n, [128, 128], BF16) for n in range(2)]
        RH = [sb("RH%d" % n, [128, 128], BF16) for n in range(2)]
        HH = [[sb("HH%d_%d" % (n, i), [128, 128], BF16) for i in range(2)] for n in range(2)]
        for n in range(2):
            for tname, tt in (("arT%d" % n, arT[n]), ("bT%d" % n, bT[n]), ("kT%d" % n, kT[n])):
                S.op("pool", lambda e, tt=tt: e.memset(tt[:], 0.0), writes=[tname])
        S.op("pool", lambda e: e.memset(vT[:], 0.0), writes=["vT"])
        ynbd = kT[0]

        toks = [(0, 256)] + [(256 + 512 * i, 512) for i in range(4)]
        v3 = lambda tile: tile[:].rearrange("p (c t) -> p c t", t=64)

        def bd_write(eng, dst, dst_name, col0, in0, in0_name, in1, in1_name, neg=False):
            for h in range(2):
                rs = slice(h * 64, (h + 1) * 64)
                o = dst[rs, :, col0 + h * 64: col0 + (h + 1) * 64]
                a = in0[rs, :].rearrange("p (c t) -> p c t", t=64)
                b = in1[rs, :].rearrange("p (c t) -> p c t", t=64)
                if neg:
                    S.op(eng, lambda e, o=o, a=a, b=b: e.scalar_tensor_tensor(out=o, in0=a, scalar=-1.0, in1=b, op0=ALU.mult, op1=ALU.mult),
                         reads=[in0_name, in1_name], writes=[dst_name])
                else:
                    S.op(eng, lambda e, o=o, a=a, b=b: e.tensor_tensor(out=o, in0=a, in1=b, op=ALU.mult),
                         reads=[in0_name, in1_name], writes=[dst_name])

        for b in range(nb):
            for i in range(3):
                S.dma(lambda e, i=i, b=b: e.dma_start(out=L[i][:], in_=g.loraT[b, i]), reads=["scr"], writes=["L%d" % i], q="pool")
            for hp in range(nhp):
                S.flush()
                S.dma(lambda e, b=b, hp=hp: e.dma_start(out=R_[:], in_=g.zT[b, 24 + hp]), reads=["scr"], writes=["R_"])
                S.dma(lambda e, b=b, hp=hp: e.dma_start(out=K_[:], in_=g.zT[b, 32 + hp]), reads=["scr"], writes=["K_"])
                S.dma(lambda e, b=b, hp=hp: e.dma_start(out=V_[:], in_=g.zT[b, 40 + hp]), reads=["scr"], writes=["V_"])
                hc = slice(hp * 128, (hp + 1) * 128)
                for n in range(2):
                    S.dma(lambda e, n=n, hc=hc: e.dma_start(out=w2b[n][:], in_=g.w2pad[n, :, hc]), writes=["w2b%d" % n], q="pool")
                    S.dma(lambda e, n=n, hc=hc: e.dma_start(out=a2b[n][:], in_=g.a2pad[n, :, hc]), writes=["a2b%d" % n], q="pool")
                S.dma(lambda e, hc=hc: e.dma_start(out=g2b[:], in_=g.rw_g2[:, hc]), writes=["g2b"], q="pool")
                hc = slice(0, 128)
                S.op("dve", lambda e, hp=hp: e.tensor_scalar(out=KKN[:], in0=K_[:], scalar1=vec[:, 4, hp:hp + 1], scalar2=None, op0=ALU.mult),
                     reads=["K_", "vec"], writes=["KKN"])
                S.op("dve", lambda e: e.tensor_tensor(out=TBF[:], in0=KKN[:], in1=KKN[:], op=ALU.mult), reads=["KKN"], writes=["TBF"])
                for ti, (t0, tn) in enumerate(toks):
                    pb = 1 + ti % 4
                    S.op("pe", lambda e, pb=pb, t0=t0, tn=tn: e.matmul(P[pb][:, 0:tn], lhsT=blk[:], rhs=TBF[:, t0:t0 + tn], start=True, stop=True),
                         reads=["blk", "TBF"], writes=["P%d" % pb])
                    S.op("act", lambda e, pb=pb, t0=t0, tn=tn: e.sqrt(out=E1[:, t0:t0 + tn], in_=P[pb][:, 0:tn]), reads=["P%d" % pb], writes=["E1"])
                S.op("dve", lambda e: e.tensor_scalar(out=E1[:], in0=E1[:], scalar1=1e-12, scalar2=None, op0=ALU.max), reads=["E1"], writes=["E1"])
                S.op("dve", lambda e: e.reciprocal(out=E1[:], in_=E1[:]), reads=["E1"], writes=["E1"])
                S.op("dve", lambda e: e.tensor_tensor(out=KKN[:], in0=KKN[:], in1=E1[:], op=ALU.mult), reads=["KKN", "E1"], writes=["KKN"])
                for h in range(2):
                    rs = slice(h * 64, (h + 1) * 64)
                    S.op("pool", lambda e, rs=rs, h=h: e.tensor_copy(out=vT[rs, :, h * 64:(h + 1) * 64], in_=V_[rs, :].rearrange("p (c t) -> p c t", t=64)),
                         reads=["V_"], writes=["vT"])
                for n in range(2):
                    for ti, (t0, tn) in enumerate(toks):
                        pb = 1 + ti % 4
                        S.op("pe", lambda e, pb=pb, t0=t0, tn=tn, n=n, hc=hc: e.matmul(P[pb][:, 0:tn], lhsT=w2b[n][:, hc], rhs=L[0][:, t0:t0 + tn], start=True, stop=True),
                             reads=["w2b%d" % n, "L0"], writes=["P%d" % pb])
                        S.op("act", lambda e, pb=pb, t0=t0, tn=tn, n=n, hp=hp: e.activation(out=SG[:, t0:t0 + tn], in_=P[pb][:, 0:tn], func=AF.Sigmoid, bias=vec[:, n, hp:hp + 1]),
                             reads=["P%d" % pb, "vec"], writes=["SG"])
                        pb2 = 5 + ti % 2
                        S.op("pe", lambda e, pb2=pb2, t0=t0, tn=tn, n=n, hc=hc: e.matmul(P[pb2][:, 0:tn], lhsT=a2b[n][:, hc], rhs=L[1][:, t0:t0 + tn], start=True, stop=True),
                             reads=["a2b%d" % n, "L1"], writes=["P%d" % pb2])
                        S.op("act", lambda e, pb2=pb2, t0=t0, tn=tn, n=n, hp=hp: e.activation(out=AN[:, t0:t0 + tn], in_=P[pb2][:, 0:tn], func=AF.Sigmoid, bias=vec[:, 2 + n, hp:hp + 1]),
                             reads=["P%d" % pb2, "vec"], writes=["AN"])
                    S.op("dve", lambda e: e.tensor_tensor_scan(out=CUM[:], data0=rm[:], data1=SG[:], initial=0.0, op0=ALU.mult, op1=ALU.add),
                         reads=["rm", "SG"], writes=["CUM"])
                    if n == 1:
                        S.op("dve", lambda e: e.tensor_copy(out=TOT[:], in_=v3(CUM)[:, :, 63]), reads=["CUM"], writes=["TOT"])
                        S.op("dve", lambda e: e.tensor_tensor(out=v3(CUM), in0=TOT[:].unsqueeze(2).to_broadcast([128, NCH, 64]), in1=v3(CUM), op=ALU.subtract),
                             reads=["TOT", "CUM"], writes=["CUM"])
                        S.op("dve", lambda e: e.tensor_tensor(out=CUM[:], in0=CUM[:], in1=SG[:], op=ALU.add), reads=["CUM", "SG"], writes=["CUM"])
                    S.op("dve", lambda e: e.tensor_tensor(out=E2[:], in0=CUM[:], in1=SG[:], op=ALU.subtract), reads=["CUM", "SG"], writes=["E2"])
                    S.op("act", lambda e: e.activation(out=E1[:], in_=E2[:], func=AF.Exp, scale=-C0), reads=["E2"], writes=["E1"])
                    bd_write("dve", arT[n], "arT%d" % n, 0, KKN, "KKN", E1, "E1", neg=True)
                    S.op("act", lambda e: e.activation(out=E1[:], in_=CUM[:], func=AF.Exp, scale=-C0), reads=["CUM"], writes=["E1"])
                    bd_write("dve", arT[n], "arT%d" % n, 128, R_, "R_", E1, "E1")
                    S.op("dve", lambda e, n=n: e.tensor_copy(out=GAM[n][:], in_=v3(E1)[:, :, 63 if n == 0 else 0]), reads=["E1"], writes=["GAM%d" % n])
                    S.op("dve", lambda e: e.tensor_tensor(out=E2[:], in0=KKN[:], in1=AN[:], op=ALU.mult), reads=["KKN", "AN"], writes=["E2"])
                    S.op("act", lambda e: e.activation(out=E1[:], in_=CUM[:], func=AF.Exp, scale=C0), reads=["CUM"], writes=["E1"])
                    bd_write("dve", bT[n], "bT%d" % n, 0, E2, "E2", E1, "E1")
                    S.op("dve", lambda e, hp=hp: e.tensor_scalar(out=KD[:], in0=AN[:], scalar1=-1.0, scalar2=vec[:, 5, hp:hp + 1], op0=ALU.add, op1=ALU.mult),
                         reads=["AN", "vec", "SG"], writes=["SG"])
                    S.op("dve", lambda e: e.scalar_tensor_tensor(out=KD[:], in0=KD[:], scalar=1.0, in1=K_[:], op0=ALU.add, op1=ALU.mult),
                         reads=["SG", "K_"], writes=["SG"])
                    bd_write("dve", kT[n], "kT%d" % n, 0, KD, "SG", E1, "E1")
                    if n == 0:
                        S.op("dve", lambda e, hp=hp: e.scalar_tensor_tensor(out=TBF[:], in0=R_[:], scalar=vec[:, 6, hp:hp + 1], in1=KD[:], op0=ALU.mult, op1=ALU.mult),
                             reads=["R_", "vec", "SG"], writes=["TBF"])
                    else:
                        S.op("dve", lambda e, hp=hp: e.scalar_tensor_tensor(out=E2[:], in0=R_[:], scalar=vec[:, 6, hp:hp + 1], in1=KD[:], op0=ALU.mult, op1=ALU.mult),
                             reads=["R_", "vec", "SG"], writes=["E2"])
                        S.op("dve", lambda e: e.tensor_tensor(out=TBF[:], in0=TBF[:], in1=E2[:], op=ALU.add), reads=["TBF", "E2"], writes=["TBF"])
                S.op("pool", lambda e: e.memset(yacc[:], 0.0), writes=["yacc"])
                for n in range(2):
                    S.op("pool", lambda e, n=n: e.memset(HH[n][0][:], 0.0), writes=["HH%d_0" % n])
                order = [list(range(NCH)), [3, 2, 1, 0] + list(range(NCH - 1, 3, -1))]
                for s in range(NCH):
                    for n in range(2):
                        c = order[n][s]
                        lat = c >= 4
                        nm = lambda x: "%s%d" % (x, n)
                        Hold, Hnew = HH[n][s % 2], HH[n][(s + 1) % 2]
                        Hold_n, Hnew_n = "HH%d_%d" % (n, s % 2), "HH%d_%d" % (n, (s + 1) % 2)
                        aTc, rTc = arT[n][:, c, 0:128], arT[n][:, c, 128:256]
                        for qi, (src, sname) in enumerate(((aTc, nm("arT")), (bT[n][:, c, :], nm("bT")), (kT[n][:, c, :], nm("kT")), (vT[:, c, :], "vT"))):
                            S.op("pe", lambda e, qi=qi, src=src: e.transpose(PB[:, qi * 128:(qi + 1) * 128], src, identb[:]),
                                 reads=[sname, "identb"], writes=["PB"])
                        S.op("act", lambda e, n=n: e.copy(out=TM[n][:], in_=PB[:, 0:512]), reads=["PB"], writes=[nm("TM")])
                        a_, b_, k_, v_ = (TM[n][:, i * 128:(i + 1) * 128] for i in range(4))
                        def nbk():
                            rot[0] = (rot[0] + 1) % len(ROT)
                            return ROT[rot[0]]
                        pw = PW_[n]
                        xx = XX[n]
                        k1 = nbk()
                        S.op("pe", lambda e, n=n, c=c, k1=k1: e.matmul(P[k1][:, 0:256], lhsT=bT[n][:, c, :], rhs=arT[n][:, c, :], start=True, stop=True),
                             reads=[nm("bT"), nm("arT")], writes=["P%d" % k1])
                        S.op("dve", lambda e, n=n, k1=k1: e.tensor_tensor(out=GB[n][:], in0=P[k1][:, 0:256], in1=mask[n][:], op=ALU.mult),
                             reads=["P%d" % k1, nm("mask")], writes=[nm("GB")])
                        k2 = nbk()
                        S.op("pe", lambda e, n=n, c=c, k2=k2: e.matmul(P[k2][:, 0:256], lhsT=kT[n][:, c, :], rhs=arT[n][:, c, :], start=True, stop=True),
                             reads=[nm("kT"), nm("arT")], writes=["P%d" % k2])
                        S.op("dve", lambda e, n=n, k2=k2: e.tensor_tensor(out=GK[n][:], in0=P[k2][:, 0:256], in1=mask[n][:], op=ALU.mult),
                             reads=["P%d" % k2, nm("mask")], writes=[nm("GK")])
                        k3 = nbk()
                        S.op("pe", lambda e, n=n, c=c, aTc=aTc, k3=k3: e.matmul(P[k3][:, 0:128], lhsT=aTc, rhs=bT[n][:, c, :], start=True, stop=True),
                             reads=[nm("bT"), nm("arT")], writes=["P%d" % k3])
                        S.op("dve", lambda e, n=n, pw=pw, k3=k3: e.tensor_tensor(out=pw[0][:, 0:128], in0=P[k3][:, 0:128], in1=maskT[n][:], op=ALU.mult),
                             reads=["P%d" % k3, nm("maskT")], writes=[nm("PW") + "_0"])
                        S.op("pool", lambda e, n=n, pw=pw: e.tensor_copy(out=pw[0][:, 128:256], in_=GB[n][:, 0:128]),
                             reads=[nm("GB")], writes=[nm("PW") + "_0"])
                        k4 = nbk()
                        S.op("pe", lambda e, n=n, v_=v_, k4=k4: e.matmul(P[k4][:, 0:128], lhsT=GK[n][:, 0:128], rhs=v_, start=True, stop=True),
                             reads=[nm("GK"), nm("TM")], writes=["P%d" % k4])
                        S.op("act", lambda e, xx=xx, k4=k4: e.copy(out=xx[0][:, 128:256], in_=P[k4][:, 0:128]), reads=["P%d" % k4], writes=[nm("XX") + "_0"])
                        S.op("pool", lambda e, xx=xx, a_=a_: e.tensor_copy(out=xx[0][:, 0:128], in_=a_), reads=[nm("TM")], writes=[nm("XX") + "_0"])
                        for i in range(6):
                            pc, pn = pw[i % 2], pw[(i + 1) % 2]
                            pcn, pnn = nm("PW") + "_%d" % (i % 2), nm("PW") + "_%d" % ((i + 1) % 2)
                            xc, xn_ = xx[i % 2], xx[(i + 1) % 2]
                            xcn, xnn = nm("XX") + "_%d" % (i % 2), nm("XX") + "_%d" % ((i + 1) % 2)
                            bk = nbk()
                            S.op("pe", lambda e, pc=pc, xc=xc, bk=bk: e.matmul(P[bk][:, 0:256], lhsT=pc[:, 128:256], rhs=xc[:], start=True, stop=True),
                                 reads=[pcn, xcn], writes=["P%d" % bk])
                            S.op("dve", lambda e, xc=xc, xn_=xn_, bk=bk: e.tensor_tensor(out=xn_[:], in0=P[bk][:, 0:256], in1=xc[:], op=ALU.add),
                                 reads=["P%d" % bk, xcn], writes=[xnn])
                            if i < 5:
                                bq = nbk()
                                S.op("pe", lambda e, pc=pc, bq=bq: e.matmul(P[bq][:, 0:128], lhsT=pc[:, 128:256], rhs=pc[:, 0:128], start=True, stop=True),
                                     reads=[pcn], writes=["P%d" % bq])
                                S.op("pe", lambda e, pc=pc, bq=bq: e.matmul(P[bq][:, 128:256], lhsT=pc[:, 0:128], rhs=pc[:, 128:256], start=True, stop=True),
                                     reads=[pcn], writes=["P%d" % bq])
                                S.op("act", lambda e, pn=pn, bq=bq: e.copy(out=pn[:], in_=P[bq][:, 0:256]), reads=["P%d" % bq], writes=[pnn])
                        X = xx[0]
                        Xn = nm("XX") + "_0"
                        Ah, U0 = X[:, 0:128], X[:, 128:256]
                        k5 = nbk()
                        S.op("pe", lambda e, Ah=Ah, b_=b_, k5=k5: e.matmul(P[k5][:, 0:128], lhsT=Ah, rhs=b_, start=True, stop=True),
                             reads=[Xn, nm("TM")], writes=["P%d" % k5])
                        S.op("dve", lambda e, n=n, k5=k5: e.tensor_tensor(out=MT[n][:], in0=P[k5][:, 0:128], in1=identf[:], op=ALU.add),
                             reads=["P%d" % k5, "identf"], writes=[nm("MT")])
                        if lat:
                            k6 = nbk()
                            S.op("pe", lambda e, Ah=Ah, n=n, k6=k6: e.matmul(P[k6][:, 0:128], lhsT=Ah, rhs=GB[n][:, 128:256], start=True, stop=True),
                                 reads=[Xn, nm("GB")], writes=["P%d" % k6])
                            S.op("dve", lambda e, n=n, rTc=rTc, k6=k6: e.tensor_tensor(out=RH[n][:], in0=P[k6][:, 0:128], in1=rTc, op=ALU.add),
                                 reads=["P%d" % k6, nm("arT")], writes=[nm("RH")])
                            S.op("pe", lambda e, n=n, U0=U0: e.matmul(P[6][:, 0:128], lhsT=GB[n][:, 128:256], rhs=U0, start=True, stop=False),
                                 reads=[nm("GB"), Xn], writes=["P6"])
                            S.op("pe", lambda e, n=n, v_=v_: e.matmul(P[6][:, 0:128], lhsT=GK[n][:, 128:256], rhs=v_, start=False, stop=False),
                                 reads=[nm("GK"), nm("TM")], writes=["P6"])
                            S.op("pe", lambda e, n=n, Hold=Hold: e.matmul(P[6][:, 0:128], lhsT=RH[n][:], rhs=Hold[:], start=False, stop=True),
                                 reads=[nm("RH"), Hold_n], writes=["P6"])
                            for h in range(2):
                                rs = slice(h * 64, (h + 1) * 64)
                                S.op("dve", lambda e, c=c, rs=rs, h=h: e.tensor_tensor(out=yacc[rs, c - 4, :], in0=P[6][rs, h * 64:(h + 1) * 64], in1=yacc[rs, c - 4, :], op=ALU.add),
                                     reads=["P6", "yacc"], writes=["yacc"])
                        S.op("pe", lambda e, b_=b_, U0=U0: e.matmul(P[5][:, 0:128], lhsT=b_, rhs=U0, start=True, stop=False),
                             reads=[nm("TM"), Xn], writes=["P5"])
                        S.op("pe", lambda e, k_=k_, v_=v_: e.matmul(P[5][:, 0:128], lhsT=k_, rhs=v_, start=False, stop=False),
                             reads=[nm("TM")], writes=["P5"])
                        S.op("pe", lambda e, n=n, Hold=Hold: e.matmul(P[5][:, 0:128], lhsT=MT[n][:], rhs=Hold[:], start=False, stop=True),
                             reads=[nm("MT"), Hold_n], writes=["P5"])
                        S.op("act", lambda e, n=n, c=c, Hnew=Hnew: e.activation(out=Hnew[:], in_=P[5][:, 0:128], func=AF.Copy, scale=GAM[n][:, c:c + 1]),
                             reads=["P5", nm("GAM")], writes=[Hnew_n])
                S.op("dve", lambda e: e.tensor_reduce(out=s1[:], in_=yacc[:], axis=AX.X, op=ALU.add), reads=["yacc"], writes=["s1"])
                ysq = E1[:, 0:32 * 64].rearrange("p (c v) -> p c v", v=64)
                S.op("dve", lambda e: e.tensor_tensor(out=ysq, in0=yacc[:], in1=yacc[:], op=ALU.mult), reads=["yacc"], writes=["E1"])
                S.op("dve", lambda e: e.tensor_reduce(out=s2[:], in_=ysq, axis=AX.X, op=ALU.add), reads=["E1"], writes=["s2"])
                S.op("dve", lambda e: e.tensor_scalar(out=s1[:], in0=s1[:], scalar1=1.0 / 64, scalar2=None, op0=ALU.mult), reads=["s1"], writes=["s1"])
                S.op("dve", lambda e: e.tensor_tensor(out=TOT[:, 0:32], in0=s1[:], in1=s1[:], op=ALU.mult), reads=["s1"], writes=["TOT"])
                S.op("dve", lambda e: e.scalar_tensor_tensor(out=s2[:], in0=s2[:], scalar=1.0 / 64, in1=TOT[:, 0:32], op0=ALU.mult, op1=ALU.subtract),
                     reads=["s2", "TOT"], writes=["s2"])
                S.op("dve", lambda e: e.tensor_scalar(out=s2[:], in0=s2[:], scalar1=64e-5, scalar2=None, op0=ALU.add), reads=["s2"], writes=["s2"])
                S.op("act", lambda e: e.sqrt(out=s2[:], in_=s2[:]), reads=["s2"], writes=["s2"])
                S.op("dve", lambda e: e.reciprocal(out=s2[:], in_=s2[:]), reads=["s2"], writes=["s2"])
                S.op("dve", lambda e: e.tensor_tensor(out=ysq, in0=yacc[:], in1=s1[:].unsqueeze(2).to_broadcast([128, 32, 64]), op=ALU.subtract),
                     reads=["yacc", "s1"], writes=["E1"])
                for h in range(2):
                    rs = slice(h * 64, (h + 1) * 64)
                    cs = slice(h * 64, (h + 1) * 64)
                    S.op("dve", lambda e, rs=rs, cs=cs: e.tensor_tensor(out=ynbd[rs, 0:32, cs], in0=ysq[rs, :, :], in1=s2[rs, :].unsqueeze(2).to_broadcast([64, 32, 64]), op=ALU.mult),
                         reads=["E1", "s2"], writes=["kT0"])
                for c in range(32):
                    q = c % 8
                    S.op("pe", lambda e, c=c, q=q: e.transpose(PB[:, q * 128:(q + 1) * 128], ynbd[:, c, :], identb[:]), reads=["kT0", "identb"], writes=["PB"])
                    for h in range(2):
                        rs = slice(h * 64, (h + 1) * 64)
                        S.op("act", lambda e, c=c, q=q, rs=rs, h=h, hp=hp: e.activation(
                            out=E2[rs, c * 64:(c + 1) * 64], in_=PB[rs, q * 128 + h * 64:q * 128 + (h + 1) * 64], func=AF.Identity,
                            scale=vec[rs, 7, hp:hp + 1], bias=vec[rs, 8, hp:hp + 1]), reads=["PB", "vec"], writes=["E2"])
                BON = SG
                for ti in range(4):
                    pb = 1 + ti % 4
                    t0 = CTX + 512 * ti
                    S.op("pe", lambda e, pb=pb, t0=t0: e.matmul(P[pb][:, 0:512], lhsT=blk[:], rhs=TBF[:, t0:t0 + 512], start=True, stop=True),
                         reads=["blk", "TBF"], writes=["P%d" % pb])
                    S.op("dve", lambda e, pb=pb, t0=t0, ti=ti: e.tensor_tensor(out=BON[:, ti * 512:(ti + 1) * 512], in0=P[pb][:, 0:512], in1=V_[:, t0:t0 + 512], op=ALU.mult),
                         reads=["P%d" % pb, "V_"], writes=["SG"])
                S.op("dve", lambda e: e.tensor_tensor(out=E2[:, 0:SEQ], in0=E2[:, 0:SEQ], in1=BON[:, 0:SEQ], op=ALU.add), reads=["E2", "SG"], writes=["E2"])
                mixo = TBF
                for ti in range(4):
                    pb = 1 + ti % 4
                    t0 = CTX + 512 * ti
                    S.op("pe", lambda e, pb=pb, t0=t0: e.matmul(P[pb][:, 0:512], lhsT=g2b[:], rhs=L[2][:, t0:t0 + 512], start=True, stop=True),
                         reads=["g2b", "L2"], writes=["P%d" % pb])
                    S.op("dve", lambda e, pb=pb, ti=ti: e.tensor_tensor(out=mixo[:, ti * 512:(ti + 1) * 512], in0=P[pb][:, 0:512], in1=E2[:, ti * 512:(ti + 1) * 512], op=ALU.mult),
                         reads=["P%d" % pb, "E2"], writes=["TBF"])
                S.dma(lambda e, b=b, hp=hp: e.dma_start(out=g.mixT[b, 8 + hp], in_=mixo[:, 0:SEQ]), reads=["TBF"], writes=["mixscr"])
                if stage in ("dbgRW", "simRW"):
                    S.dma(lambda e, b=b, hp=hp: e.dma_start(out=g.dbg_rwo[b, hp], in_=mixo[:, 0:SEQ]), reads=["TBF"])
        S.flush()


NF = 4096
TWO_PI = float(2 * np.pi)


def hy_consts():
    L = SEQ
    t = np.linspace(0.0, 1.0, L, dtype=np.float32)[:, None]
    ang = (2.0 * np.pi / L) * np.arange(L, dtype=np.float32)[:, None]
    bands = np.linspace(1e-4, 15, 16, dtype=np.float32)[None, :]
    feats = np.concatenate([t, np.cos(bands * ang), -np.sin(bands * ang)], axis=-1).astype(np.float32)
    deltas = np.abs(np.linspace(np.log(1e-2) / 1.5, np.log(1e-2) / 0.3, 1024, dtype=np.float32))
    win = np.exp(-t * np.tile(deltas, 2)).astype(np.float32)
    tt = np.arange(L, dtype=np.float64)[:, None]
    ff = np.arange(L, dtype=np.float64)[None, :]
    th = 2 * np.pi * ((tt * ff) % NF) / NF
    C = np.cos(th)
    Sn = np.sin(th)
    wt = np.full((L,), 2.0 / NF)
    wt[0] = 1.0 / NF
    bf = ml_dtypes.bfloat16
    tile = lambda M: np.ascontiguousarray(M.reshape(16, 128, 16, 128).transpose(2, 1, 0, 3)).astype(np.float32).astype(bf)
    m = {
        "hy_featsT": np.ascontiguousarray(feats.T),
        "hy_win": np.ascontiguousarray(win.reshape(16, 128, 2048).transpose(1, 0, 2)),
        "hy_Cfw": tile(C), "hy_Sfw": tile(Sn),
        "hy_Cinv": (C * wt[:, None]).astype(np.float32).astype(bf),
        "hy_Sinv": (Sn * wt[:, None]).astype(np.float32).astype(bf),
        "hy_alt": np.ascontiguousarray(((-1.0) ** np.arange(L)).reshape(16, 128).T.astype(np.float32)).astype(bf),
        "hy_altinv": (((-1.0) ** np.arange(L)) / NF).reshape(1, L).astype(np.float32).astype(bf),
    }
    return m


def declare_hy(nc, g, stage):
    dt = nc.dram_tensor
    inp = lambda name, shape, dtype=F32: dt(name, list(shape), dtype, kind="ExternalInput").ap()
    g.hy_featsT = inp("hy_featsT", [33, SEQ])
    g.hy_win = inp("hy_win", [128, 16, 2048])
    g.hy_Cfw = inp("hy_Cfw", [16, 128, 16, 128], BF16)
    g.hy_Sfw = inp("hy_Sfw", [16, 128, 16, 128], BF16)
    g.hy_Cinv = inp("hy_Cinv", [SEQ, SEQ], BF16)
    g.hy_Sinv = inp("hy_Sinv", [SEQ, SEQ], BF16)
    g.hy_alt = inp("hy_alt", [128, 16], BF16)
    g.hy_altinv = inp("hy_altinv", [1, SEQ], BF16)
    g.hy_w1 = inp("hy_w1", [33, 64])
    g.hy_w2 = inp("hy_w2", [64, 64])
    g.hy_w3 = inp("hy_w3", [64, 64])
    g.hy_w4 = inp("hy_w4", [64, 2048])
    g.hy_fb = inp("hy_fb", [64, 4])
    g.hy_vec = inp("hy_vec", [128, 2, 8])
    g.specT = dt("hy_spec_scr", [2, 17, 128, 1024], F32).ap()
    if stage in ("dbgHY", "simHY"):
        g.dbg_hyo = dt("dbg_hyo", [BPC, 8, 128, SEQ], BF16, kind="ExternalOutput").ap()


def phase_HY(nc, g, stage, nb=BPC):
    with contextlib.ExitStack() as st:
        S = g.S
        S.barrier()
        sb = lambda name, shape, dtype: st.enter_context(nc.sbuf_tensor("hys_" + name, shape, dtype))
        PB = st.enter_context(nc.psum_tensor("hpbf", [128, 1024], BF16))
        P = [None] + [st.enter_context(nc.psum_tensor("hps%d" % i, [128, 512], F32)) for i in range(1, 8)]
        identb = sb("identb", [128, 128], BF16)
        blk = sb("blk", [128, 128], BF16)
        alt = sb("alt", [128, 16], BF16)
        altinv = sb("altinv", [1, SEQ], BF16)
        vec = sb("vec", [128, 2, 8], F32)
        S.dma(lambda e: e.dma_start(out=identb[:], in_=g.ident_bf[:]), writes=["identb"])
        S.dma(lambda e: e.dma_start(out=blk[:], in_=g.rw_blk[:]), writes=["blk"], q="pool")
        S.dma(lambda e: e.dma_start(out=alt[:], in_=g.hy_alt[:]), writes=["alt"])
        S.dma(lambda e: e.dma_start(out=altinv[:], in_=g.hy_altinv[:]), writes=["altinv"])
        S.dma(lambda e: e.dma_start(out=vec[:], in_=g.hy_vec[:]), writes=["vec"])
        uTM = sb("uTM", [128, 16, 512], BF16)
        fw = [[sb("fw%d_%d" % (i, j), [128, 16, 128], BF16) for j in range(2)] for i in range(2)]
        Pc = sb("Pc", [128, 1024], F32)
        Ps = sb("Ps", [128, 1024], F32)
        rot = [0]
        ROT = [1, 2, 3, 4, 5, 6, 7]

        def nbk():
            rot[0] = (rot[0] + 1) % len(ROT)
            return ROT[rot[0]]

        def forward_dft(ncols, sink):
            for fc in range(16):
                bufi = fc % 2
                for part, src in ((0, g.hy_Cfw), (1, g.hy_Sfw)):
                    S.dma(lambda e, part=part, bufi=bufi, src=src, fc=fc: e.dma_start(out=fw[part][bufi][:], in_=src[fc]),
                          writes=["fw%d_%d" % (part, bufi)])
                kc_, ks_ = nbk(), nbk()
                for part, bk in ((0, kc_), (1, ks_)):
                    for tb in range(16):
                        S.op("pe", lambda e, part=part, bufi=bufi, tb=tb, bk=bk: e.matmul(
                            P[bk][:, 0:ncols], lhsT=fw[part][bufi][:, tb, :], rhs=uTM[:, tb, 0:ncols], start=(tb == 0), stop=(tb == 15)),
                            reads=["fw%d_%d" % (part, bufi), "uTM"], writes=["P%d" % bk])
                sink(fc, kc_, ks_)
            kn = nbk()
            for tb in range(16):
                S.op("pe", lambda e, tb=tb, kn=kn: e.matmul(P[kn][0:1, 0:ncols], lhsT=alt[:, tb:tb + 1], rhs=uTM[:, tb, 0:ncols],
                                                            start=(tb == 0), stop=(tb == 15)),
                     reads=["alt", "uTM"], writes=["P%d" % kn])
            sink(16, kn, None)

        with contextlib.ExitStack() as stF:
            sbF = lambda name, shape, dtype: stF.enter_context(nc.sbuf_tensor("hyf_" + name, shape, dtype))
            featsT = sbF("featsT", [33, SEQ], F32)
            w1 = sbF("w1", [33, 64], F32)
            w2 = sbF("w2", [64, 64], F32)
            w3 = sbF("w3", [64, 64], F32)
            w4 = sbF("w4", [64, 2048], F32)
            fb = sbF("fb", [64, 4], F32)
            fbc = sbF("fbc", [64, 3], F32)
            hA = sbF("hA", [64, SEQ], F32)
            hB = sbF("hB", [64, SEQ], F32)
            win = sbF("win", [128, 16, 512], F32)
            for tname, tt, src in (("featsT", featsT, g.hy_featsT), ("w1", w1, g.hy_w1), ("w2", w2, g.hy_w2), ("w3", w3, g.hy_w3),
                                   ("w4", w4, g.hy_w4), ("fb", fb, g.hy_fb)):
                S.dma(lambda e, tt=tt, src=src: e.dma_start(out=tt[:], in_=src[:]), writes=[tname])
            for i in range(3):
                S.op("dve", lambda e, i=i: e.tensor_scalar(out=fbc[:, i:i + 1], in0=fb[:, 1 + i:2 + i], scalar1=fb[:, 0:1], scalar2=None,
                                                           op0=ALU.mult), reads=["fb"], writes=["fbc"])
            layers = ((w1, "w1", featsT, "featsT", 33, hA, "hA"), (w2, "w2", hA, "hA", 64, hB, "hB"), (w3, "w3", hB, "hB", 64, hA, "hA"))
            for li, (w, wn, src, sn, kk, dst, dn) in enumerate(layers):
                for ti in range(4):
                    bk = nbk()
                    S.op("pe", lambda e, w=w, src=src, kk=kk, ti=ti, bk=bk: e.matmul(P[bk][0:64, 0:512], lhsT=w[0:kk, :], rhs=src[0:kk, ti * 512:(ti + 1) * 512],
                                                                                   start=True, stop=True), reads=[wn, sn], writes=["P%d" % bk])
                    MAGIC = 12582912.0
                    S.op("dve", lambda e, ti=ti, bk=bk, li=li: e.tensor_scalar(out=win[0:64, 0, :], in0=P[bk][0:64, 0:512], scalar1=fb[:, 0:1], scalar2=fbc[:, li:li + 1],
                                                                               op0=ALU.mult, op1=ALU.add), reads=["P%d" % bk, "fb", "fbc"], writes=["win"])
                    S.op("dve", lambda e: e.tensor_scalar(out=win[0:64, 1, :], in0=win[0:64, 0, :], scalar1=1.0 / TWO_PI, scalar2=MAGIC,
                                                          op0=ALU.mult, op1=ALU.add), reads=["win"], writes=["win"])
                    S.op("dve", lambda e: e.tensor_scalar(out=win[0:64, 1, :], in0=win[0:64, 1, :], scalar1=-MAGIC, scalar2=None,
                                                          op0=ALU.add), reads=["win"], writes=["win"])
                    S.op("dve", lambda e: e.scalar_tensor_tensor(out=win[0:64, 0, :], in0=win[0:64, 1, :], scalar=-TWO_PI, in1=win[0:64, 0, :],
                                                                 op0=ALU.mult, op1=ALU.add), reads=["win"], writes=["win"])
                    S.op("dve", lambda e: e.tensor_scalar(out=win[0:64, 0, :], in0=win[0:64, 0, :], scalar1=-3.1415925, scalar2=3.1415925,
                                                          op0=ALU.max, op1=ALU.min), reads=["win"], writes=["win"])
                    S.op("act", lambda e, dst=dst, ti=ti: e.activation(out=dst[:, ti * 512:(ti + 1) * 512], in_=win[0:64, 0, :], func=AF.Sin),
                         reads=["win"], writes=[dn])
            h3 = hA
            for cg in range(4):
                S.dma(lambda e, cg=cg: e.dma_start(out=win[:], in_=g.hy_win[:, :, cg * 512:(cg + 1) * 512]), writes=["win"])
                for tb in range(16):
                    bk = nbk()
                    S.op("pe", lambda e, tb=tb, cg=cg, bk=bk: e.matmul(P[bk][:, 0:512], lhsT=h3[:, tb * 128:(tb + 1) * 128], rhs=w4[:, cg * 512:(cg + 1) * 512],
                                                                      start=True, stop=True), reads=["hA", "w4"], writes=["P%d" % bk])
                    S.op("dve", lambda e, tb=tb, bk=bk: e.tensor_tensor(out=uTM[:, tb, :], in0=P[bk][:, 0:512], in1=win[:, tb, :], op=ALU.mult),
                         reads=["P%d" % bk, "win"], writes=["uTM"])
                if cg >= 2:
                    S.op("dve", lambda e: e.memset(uTM[0:1, 0, :], 0.0), writes=["uTM"])
                bwd = cg >= 2
                c0 = (cg % 2) * 512

                def sink(fc, kc_, ks_, bwd=bwd, c0=c0):
                    rows = slice(0, 128) if fc < 16 else slice(0, 1)
                    for part, bk, sign in ((0, kc_, 1.0), (1, ks_, -1.0 if not bwd else 1.0)):
                        if bk is None:
                            continue
                        dst = Pc if part == 0 else Ps
                        dn = "Pc" if part == 0 else "Ps"
                        if not bwd:
                            S.op("act", lambda e, dst=dst, bk=bk, sign=sign, rows=rows: e.activation(out=dst[rows, 0:512], in_=P[bk][rows, 0:512], func=AF.Copy, scale=sign),
                                 reads=["P%d" % bk], writes=[dn])
                        else:
                            S.dma(lambda e, dst=dst, part=part, fc=fc, rows=rows, c0=c0: e.dma_start(out=dst[rows, 0:512], in_=g.specT[part, fc, rows, c0:c0 + 512]),
                                  reads=["spec"], writes=[dn])
                            S.op("dve", lambda e, dst=dst, bk=bk, rows=rows: e.tensor_tensor(out=dst[rows, 0:512], in0=dst[rows, 0:512], in1=P[bk][rows, 0:512], op=ALU.add),
                                 reads=["P%d" % bk, dn], writes=[dn])
                        S.dma(lambda e, dst=dst, part=part, fc=fc, rows=rows, c0=c0: e.dma_start(out=g.specT[part, fc, rows, c0:c0 + 512], in_=dst[rows, 0:512]),
                              reads=[dn], writes=["spec"])
                forward_dft(512, sink)

        S.barrier()
        X0 = sb("X0", [128, SEQ], F32)
        X1 = sb("X1", [128, SEQ], F32)
        VV = sb("VV", [128, SEQ], F32)
        UU = [sb("UU%d" % i, [128, SEQ], F32) for i in range(4)]
        Ub = sb("Ub", [128, SEQ], BF16)
        Yr = sb("Yr", [128, 17, 512], BF16)
        Yi = sb("Yi", [128, 17, 512], BF16)
        Tr = sb("Tr", [128, 512], F32)
        Ti = sb("Ti", [128, 512], F32)
        inv = [[sb("inv%d_%d" % (i, j), [128, SEQ], BF16) for j in range(2)] for i in range(2)]
        osb = sb("osb", [128, SEQ], BF16)
        for b in range(nb):
            for cgp in range(2):
                S.flush()
                for j in range(4):
                    ch = cgp * 4 + j
                    S.dma(lambda e, b=b, ch=ch: e.dma_start(out=X1[:], in_=g.zT[b, 8 + ch, :, CTX:TB]), reads=["scr"], writes=["X1"])
                    S.dma(lambda e, b=b, ch=ch: e.dma_start(out=VV[:], in_=g.zT[b, 16 + ch, :, CTX:TB]), reads=["scr"], writes=["VV"])
                    S.op("dve", lambda e, j=j: e.tensor_tensor(out=UU[j][:], in0=VV[:], in1=X1[:], op=ALU.mult), reads=["VV", "X1"], writes=["UU%d" % j])
                    S.op("pool", lambda e, j=j: e.tensor_copy(out=Ub[:], in_=UU[j][:]), reads=["UU%d" % j], writes=["Ub"])
                    for tb in range(16):
                        q = tb % 8
                        S.op("pe", lambda e, tb=tb, q=q: e.transpose(PB[:, q * 128:(q + 1) * 128], Ub[:, tb * 128:(tb + 1) * 128], identb[:]),
                             reads=["Ub", "identb"], writes=["PB"])
                        if q == 7:
                            S.op("act", lambda e, tb=tb, j=j: e.copy(out=uTM[:, tb - 7:tb + 1, j * 128:(j + 1) * 128],
                                                                    in_=PB[:, :].rearrange("p (q f) -> p q f", f=128)), reads=["PB"], writes=["uTM"])

                def sink2(fc, kc_, ks_, cgp=cgp):
                    rows = slice(0, 128) if fc < 16 else slice(0, 1)
                    c0 = cgp * 512
                    S.dma(lambda e, fc=fc, rows=rows, c0=c0: e.dma_start(out=Tr[rows, :], in_=g.specT[0, fc, rows, c0:c0 + 512]), reads=["spec"], writes=["Tr"])
                    if ks_ is not None:
                        S.dma(lambda e, fc=fc, rows=rows, c0=c0: e.dma_start(out=Ti[rows, :], in_=g.specT[1, fc, rows, c0:c0 + 512]), reads=["spec"], writes=["Ti"])
                    if ks_ is None:
                        S.op("dve", lambda e, rows=rows, kc_=kc_: e.tensor_tensor(out=Yr[rows, 16, :], in0=P[kc_][rows, 0:512], in1=Tr[rows, :], op=ALU.mult),
                             reads=["P%d" % kc_, "Tr"], writes=["Yr"])
                        return
                    S.op("act", lambda e, kc_=kc_: e.copy(out=Pc[:, 0:512], in_=P[kc_][:, 0:512]), reads=["P%d" % kc_], writes=["Pc"])
                    S.op("act", lambda e, ks_=ks_: e.copy(out=Ps[:, 0:512], in_=P[ks_][:, 0:512]), reads=["P%d" % ks_], writes=["Ps"])
                    S.op("dve", lambda e: e.tensor_tensor(out=Pc[:, 512:1024], in0=Pc[:, 0:512], in1=Tr[:], op=ALU.mult), reads=["Pc", "Tr"], writes=["Pc"])
                    S.op("pool", lambda e: e.tensor_tensor(out=Ps[:, 512:1024], in0=Ps[:, 0:512], in1=Ti[:], op=ALU.mult), reads=["Ps", "Ti"], writes=["Ps"])
                    S.op("dve", lambda e, fc=fc: e.tensor_tensor(out=Yr[:, fc, :], in0=Pc[:, 512:1024], in1=Ps[:, 512:1024], op=ALU.add), reads=["Pc", "Ps"], writes=["Yr"])
                    S.op("dve", lambda e: e.tensor_tensor(out=Ps[:, 512:1024], in0=Ps[:, 0:512], in1=Tr[:], op=ALU.mult), reads=["Ps", "Tr"], writes=["Ps"])
                    S.op("pool", lambda e: e.tensor_tensor(out=Pc[:, 512:1024], in0=Pc[:, 0:512], in1=Ti[:], op=ALU.mult), reads=["Pc", "Ti"], writes=["Pc"])
                    S.op("dve", lambda e, fc=fc: e.tensor_tensor(out=Yi[:, fc, :], in0=Ps[:, 512:1024], in1=Pc[:, 512:1024], op=ALU.subtract), reads=["Pc", "Ps"], writes=["Yi"])
                forward_dft(512, sink2)
                acc = [[nbk() for _ in range(4)] for _ in range(1)]
                for j in range(4):
                    ch = cgp * 4 + j
                    banks = [1, 2, 3, 4]
                    for fc in range(16):
                        bufi = fc % 2
                        if j == 0 or True:
                            for part, src in ((0, g.hy_Cinv), (1, g.hy_Sinv)):
                                S.dma(lambda e, part=part, bufi=bufi, src=src, fc=fc: e.dma_start(out=inv[part][bufi][:], in_=src[fc * 128:(fc + 1) * 128, :]),
                                      writes=["inv%d_%d" % (part, bufi)])
                        for ti in range(4):
                            bk = banks[ti]
                            S.op("pe", lambda e, fc=fc, j=j, ti=ti, bk=bk, bufi=bufi: e.matmul(
                                P[bk][:, 0:512], lhsT=Yr[:, fc, j * 128:(j + 1) * 128], rhs=inv[0][bufi][:, ti * 512:(ti + 1) * 512], start=(fc == 0), stop=False),
                                reads=["Yr", "inv0_%d" % bufi], writes=["P%d" % bk])
                            S.op("pe", lambda e, fc=fc, j=j, ti=ti, bk=bk, bufi=bufi: e.matmul(
                                P[bk][:, 0:512], lhsT=Yi[:, fc, j * 128:(j + 1) * 128], rhs=inv[1][bufi][:, ti * 512:(ti + 1) * 512], start=False, stop=False),
                                reads=["Yi", "inv1_%d" % bufi], writes=["P%d" % bk])
                    for ti in range(4):
                        bk = banks[ti]
                        S.op("pe", lambda e, j=j, ti=ti, bk=bk: e.matmul(
                            P[bk][:, 0:512], lhsT=Yr[0:1, 16, j * 128:(j + 1) * 128], rhs=altinv[0:1, ti * 512:(ti + 1) * 512], start=False, stop=True),
                            reads=["Yr", "altinv"], writes=["P%d" % bk])
                    S.dma(lambda e, b=b, ch=ch: e.dma_start(out=X0[:], in_=g.zT[b, ch, :, CTX:TB]), reads=["scr"], writes=["X0"])
                    for ti in range(4):
                        bk = banks[ti]
                        ts_ = slice(ti * 512, (ti + 1) * 512)
                        S.op("dve", lambda e, j=j, bk=bk, ts_=ts_, ch=ch: e.scalar_tensor_tensor(out=UU[j][:, ts_], in0=UU[j][:, ts_], scalar=vec[:, 0, ch:ch + 1], in1=P[bk][:, 0:512],
                                                                                           op0=ALU.mult, op1=ALU.add), reads=["UU%d" % j, "vec", "P%d" % bk], writes=["UU%d" % j])
                    S.op("dve", lambda e, j=j: e.tensor_tensor(out=UU[j][:], in0=UU[j][:], in1=X0[:], op=ALU.mult), reads=["UU%d" % j, "X0"], writes=["UU%d" % j])
                    S.op("pool", lambda e, j=j: e.tensor_tensor(out=Ub[:], in0=UU[j][:], in1=UU[j][:], op=ALU.mult), reads=["UU%d" % j], writes=["Ub"])
                    for ti in range(4):
                        bk = 5 + ti % 3
                        ts_ = slice(ti * 512, (ti + 1) * 512)
                        S.op("pe", lambda e, bk=bk, ts_=ts_: e.matmul(P[bk][:, 0:512], lhsT=blk[:], rhs=Ub[:, ts_], start=True, stop=True), reads=["blk", "Ub"], writes=["P%d" % bk])
                        S.op("dve", lambda e, bk=bk, ts_=ts_: e.tensor_scalar(out=X0[:, ts_], in0=P[bk][:, 0:512], scalar1=1.0 / 64, scalar2=EPS, op0=ALU.mult, op1=ALU.add),
                             reads=["P%d" % bk], writes=["X0"])
                    S.op("act", lambda e: e.sqrt(out=X0[:], in_=X0[:]), reads=["X0"], writes=["X0"])
                    S.op("dve", lambda e: e.reciprocal(out=X0[:], in_=X0[:]), reads=["X0"], writes=["X0"])
                    S.op("dve", lambda e, j=j, ch=ch: e.scalar_tensor_tensor(out=osb[:], in0=UU[j][:], scalar=vec[:, 1, ch:ch + 1], in1=X0[:], op0=ALU.mult, op1=ALU.mult),
                         reads=["UU%d" % j, "vec", "X0"], writes=["osb"])
                    S.dma(lambda e, b=b, ch=ch: e.dma_start(out=g.mixT[b, ch], in_=osb[:]), reads=["osb"], writes=["mixscr"])
                    if stage in ("dbgHY", "simHY"):
                        S.dma(lambda e, b=b, ch=ch: e.dma_start(out=g.dbg_hyo[b, ch], in_=osb[:]), reads=["osb"])
        S.flush()


NE = 32
SPARSE = True
SBR = 512
NSB = (NTOK_ := BPC * SEQ) * 4 // SBR + 32
TBK = 1024
NTOK = BPC * SEQ


def declare_F(nc, g, stage):
    dt = nc.dram_tensor
    inp = lambda name, shape, dtype=F32: dt(name, list(shape), dtype, kind="ExternalInput").ap()
    g.out_w = inp("out_w", [D, D])
    g.g2row = inp("g2row", [1, D])
    g.router_w = inp("router_w", [D, NE])
    g.router_b = inp("router_b", [1, NE])
    g.ex_wg = inp("ex_w_gate", [NE, D, D])
    g.ex_wu = inp("ex_w_up", [NE, D, D])
    g.ex_wd = inp("ex_w_down", [NE, D, D])
    g.ex_bgu = inp("ex_bgu", [NE, 128, 2, KC])
    g.ex_bd = inp("ex_b_down", [NE, D])
    g.sp_tri = inp("sp_tri", [128, 128])
    g.sp_ones = inp("sp_ones", [128, 128])
    g.sp_sidx = inp("sp_sidx", [128, NSB])
    g.sp_base4 = inp("sp_base4", [128, 4, KC])
    g.sp_piota = inp("sp_piota", [128, 1])
    g.h2tm_scr = dt("h2tm_scr", [NTOK, D], BF16).ap()
    g.xs_scr = dt("xs_scr", [NSB * SBR, D], BF16).ap()
    g.ys_scr = dt("ys_scr", [NSB * SBR, D], F32).ap()
    kd = {"kind": "ExternalOutput"} if F_DBG else {}
    g.x1_scr = dt("x1_scr", [NTOK, D], F32, **kd).ap()
    g.h2T_scr = dt("h2T_scr", [KC, 128, NTOK], BF16, **kd).ap()
    if stage == "simF" or F_DBG:
        g.dbg_gates = dt("dbg_gates", [128, NTOK // 128, NE], F32, kind="ExternalOutput").ap()


def phase_F(nc, g, stage, nexp=NE, nblk=NTOK // TBK):
    with contextlib.ExitStack() as st:
        S = g.S
        S.barrier()
        sb = lambda name, shape, dtype: st.enter_context(nc.sbuf_tensor("fs_" + name, shape, dtype))
        PB = st.enter_context(nc.psum_tensor("fpbf", [128, 1024], BF16))
        P = [None] + [st.enter_context(nc.psum_tensor("fps%d" % i, [128, 512], F32)) for i in range(1, 8)]
        rot = [0]
        ROT = [1, 2, 3, 4, 5, 6, 7]

        def nbk():
            rot[0] = (rot[0] + 1) % len(ROT)
            return ROT[rot[0]]
        gates = sb("gates", [128, NTOK // 128, NE], F32)
        NTT = NTOK // 128
        if SPARSE:
            LG = sb("LG", [128, NTT, NE], F32)
            MK = sb("MK", [128, NTT, NE], F32)
            TOP8 = sb("TOP8", [128, NTT, 8], F32)
        identf = sb("identf", [128, 128], F32)
        fg = sb("fg", [128, D], F32)
        m5t = sb("m5t", [128, D], F32)
        S.dma(lambda e: e.dma_start(out=identf[:], in_=g.ident_f[:]), writes=["identf"])
        S.dma(lambda e: e.dma_start(out=fg[:], in_=g.final_g[0, :].partition_broadcast(128)), writes=["fg"])
        with contextlib.ExitStack() as s1:
            sb1 = lambda name, shape, dtype: s1.enter_context(nc.sbuf_tensor("f1_" + name, shape, dtype))
            ow = sb1("ow", [128, KC, D], BF16)
            for kc in range(KC):
                S.dma(lambda e, kc=kc: e.dma_start(out=ow[:, kc, :], in_=g.out_w[kc * 128:(kc + 1) * 128, :]), writes=["ow"], q="pool")
            rw_ = sb1("rw", [128, KC, NE], F32)
            S.dma(lambda e: e.dma_start(out=rw_[:], in_=g.router_w.rearrange("(kc p) e -> p kc e", p=128)), writes=["rw"])
            rb = sb1("rb", [128, NE], F32)
            S.dma(lambda e: e.dma_start(out=rb[:], in_=g.router_b[0, :].partition_broadcast(128)), writes=["rb"])
            m2 = sb1("m2", [128, D], F32)
            A3 = sb1("A3", [128, D], F32)
            B3 = sb1("B3", [128, D], F32)
            mixt = [sb1("mixt%d" % i, [128, KC, 128], BF16) for i in range(2)]
            xt = [sb1("xt%d" % i, [128, D], F32) for i in range(2)]
            h2 = sb1("h2", [128, D], F32)
            junk = sb1("junk", [128, D], F32)
            h2Tf = sb1("h2Tf", [128, KC, 128], F32)
            h2Tb = sb1("h2Tb", [128, KC, 128], BF16)
            sm = sb1("sm", [128, 16], F32)
            lg = sb1("lg", [128, NE], F32)
            ex = sb1("ex", [128, NE], F32)
            it = 0
            for b in range(BPC):
                S.dma(lambda e, b=b: e.dma_start(out=m2[:], in_=g.mod_row[b, 2 * D:3 * D].partition_broadcast(128)), reads=["modrow"], writes=["m2"])
                S.dma(lambda e, b=b: e.dma_start(out=A3[:], in_=g.mod_row[b, 4 * D:5 * D].partition_broadcast(128)), reads=["modrow"], writes=["A3"])
                S.dma(lambda e, b=b: e.dma_start(out=B3[:], in_=g.mod_row[b, 3 * D:4 * D].partition_broadcast(128)), reads=["modrow"], writes=["B3"])
                S.dma(lambda e: e.dma_start(out=junk[:], in_=g.g2row[0, :].partition_broadcast(128)), writes=["junk"])
                S.op("dve", lambda e: e.scalar_tensor_tensor(out=A3[:], in0=A3[:], scalar=1.0, in1=junk[:], op0=ALU.add, op1=ALU.mult), reads=["A3", "junk"], writes=["A3"])
                for t in range(SEQ // 128):
                    if t % 8 == 0:
                        S.flush()
                    p = it % 2
                    it += 1
                    gt = b * (SEQ // 128) + t
                    X, MX = "xt%d" % p, "mixt%d" % p
                    S.dma(lambda e, p=p, b=b, t=t: e.dma_start(out=xt[p][:], in_=g.x[b, t * 128:(t + 1) * 128, :]), writes=[X])
                    S.dma(lambda e, p=p, b=b, t=t: e.dma_start(out=mixt[p][:], in_=g.mixT[b, :, :, t * 128:(t + 1) * 128].rearrange("k p t -> p k t")),
                          reads=["mixscr"], writes=[MX])
                    for dj in range(4):
                        bk = nbk()
                        for kc in range(KC):
                            S.op("pe", lambda e, p=p, kc=kc, dj=dj, bk=bk: e.matmul(P[bk][:, 0:512], lhsT=mixt[p][:, kc, :], rhs=ow[:, kc, dj * 512:(dj + 1) * 512],
                                                                                   start=(kc == 0), stop=(kc == KC - 1)), reads=[MX, "ow"], writes=["P%d" % bk])
                        ds_ = slice(dj * 512, (dj + 1) * 512)
                        S.op("dve", lambda e, bk=bk, ds_=ds_: e.tensor_tensor(out=h2[:, ds_], in0=P[bk][:, 0:512], in1=m2[:, ds_], op=ALU.mult), reads=["P%d" % bk, "m2"], writes=["h2"])
                    S.op("dve", lambda e, p=p: e.tensor_tensor(out=xt[p][:], in0=xt[p][:], in1=h2[:], op=ALU.add), reads=[X, "h2"], writes=[X])
                    S.dma(lambda e, p=p, gt=gt: e.dma_start(out=g.x1_scr[gt * 128:(gt + 1) * 128, :], in_=xt[p][:]), reads=[X], writes=["x1scr"])
                    S.op("act", lambda e, p=p: e.activation(out=junk[:], in_=xt[p][:], func=AF.Square), reads=[X], writes=["junk"])
                    S.op("dve", lambda e: e.tensor_reduce(out=sm[:, 0:1], in_=junk[:], axis=AX.X, op=ALU.add), reads=["junk"], writes=["sm"])
                    S.op("dve", lambda e: e.tensor_scalar(out=sm[:, 0:1], in0=sm[:, 0:1], scalar1=1.0 / D, scalar2=EPS, op0=ALU.mult, op1=ALU.add), reads=["sm"], writes=["sm"])
                    S.op("act", lambda e: e.sqrt(out=sm[:, 0:1], in_=sm[:, 0:1]), reads=["sm"], writes=["sm"])
                    S.op("dve", lambda e: e.reciprocal(out=sm[:, 0:1], in_=sm[:, 0:1]), reads=["sm"], writes=["sm"])
                    S.op("dve", lambda e, p=p: e.scalar_tensor_tensor(out=h2[:], in0=xt[p][:], scalar=sm[:, 0:1], in1=A3[:], op0=ALU.mult, op1=ALU.mult),
                         reads=[X, "sm", "A3"], writes=["h2"])
                    S.op("dve", lambda e: e.tensor_tensor(out=h2[:], in0=h2[:], in1=B3[:], op=ALU.add), reads=["h2", "B3"], writes=["h2"])
                    for kc in range(KC):
                        bk = nbk()
                        S.op("pe", lambda e, kc=kc, bk=bk: e.transpose(P[bk][:, 0:128], h2[:, kc * 128:(kc + 1) * 128], identf[:]), reads=["h2", "identf"], writes=["P%d" % bk])
                        S.op("act", lambda e, kc=kc, bk=bk: e.copy(out=h2Tf[:, kc, :], in_=P[bk][:, 0:128]), reads=["P%d" % bk], writes=["h2Tf"])
                    S.op("pool", lambda e: e.tensor_copy(out=h2Tb[:], in_=h2Tf[:]), reads=["h2Tf"], writes=["h2Tb"])
                    S.dma(lambda e, gt=gt: e.dma_start(out=g.h2T_scr[:, :, gt * 128:(gt + 1) * 128].rearrange("k p t -> p k t"), in_=h2Tb[:]), reads=["h2Tb"], writes=["h2scr"])
                    bk = nbk()
                    for kc in range(KC):
                        S.op("pe", lambda e, kc=kc, bk=bk: e.matmul(P[bk][:, 0:NE], lhsT=h2Tf[:, kc, :], rhs=rw_[:, kc, :], start=(kc == 0), stop=(kc == KC - 1)),
                             reads=["h2Tf", "rw"], writes=["P%d" % bk])
                    S.op("dve", lambda e, bk=bk: e.tensor_tensor(out=lg[:], in0=P[bk][:, 0:NE], in1=rb[:], op=ALU.add), reads=["P%d" % bk, "rb"], writes=["lg"])
                    S.op("dve", lambda e: e.max(out=sm[:, 8:16], in_=lg[:]), reads=["lg"], writes=["sm"])
                    S.op("dve", lambda e: e.tensor_scalar(out=sm[:, 1:2], in0=sm[:, 8:9], scalar1=-1.0, scalar2=None, op0=ALU.mult), reads=["sm"], writes=["sm"])
                    S.op("act", lambda e: e.activation(out=ex[:], in_=lg[:], func=AF.Exp, bias=sm[:, 1:2]), reads=["lg", "sm"], writes=["ex"])
                    S.op("dve", lambda e: e.scalar_tensor_tensor(out=ex[:], in0=lg[:], scalar=sm[:, 11:12], in1=ex[:], op0=ALU.is_ge, op1=ALU.mult),
                         reads=["lg", "sm", "ex"], writes=["ex"])
                    S.op("dve", lambda e: e.tensor_reduce(out=sm[:, 2:3], in_=ex[:], axis=AX.X, op=ALU.add), reads=["ex"], writes=["sm"])
                    S.op("dve", lambda e: e.reciprocal(out=sm[:, 2:3], in_=sm[:, 2:3]), reads=["sm"], writes=["sm"])
                    S.op("dve", lambda e, gt=gt: e.tensor_scalar(out=gates[:, gt, :], in0=ex[:], scalar1=sm[:, 2:3], scalar2=None, op0=ALU.mult), reads=["ex", "sm"], writes=["gates"])
                    if SPARSE:
                        S.op("pool", lambda e, gt=gt: e.tensor_copy(out=LG[:, gt, :], in_=lg[:]), reads=["lg"], writes=["LG"])
                        S.op("pool", lambda e, gt=gt: e.tensor_copy(out=TOP8[:, gt, :], in_=sm[:, 8:16]), reads=["sm"], writes=["TOP8"])
                        S.op("dve", lambda e, gt=gt: e.tensor_scalar(out=MK[:, gt, :], in0=lg[:], scalar1=sm[:, 11:12], scalar2=None, op0=ALU.is_ge), reads=["lg", "sm"], writes=["MK"])
                        S.op("pool", lambda e: e.tensor_copy(out=junk[:].bitcast(BF16)[:, 0:D], in_=h2[:]), reads=["h2"], writes=["junk"])
                        S.dma(lambda e, gt=gt: e.dma_start(out=g.h2tm_scr[gt * 128:(gt + 1) * 128, :], in_=junk[:].bitcast(BF16)[:, 0:D]), reads=["junk"], writes=["h2tm"])
        if stage == "simF" or F_DBG:
            S.dma(lambda e: e.dma_start(out=g.dbg_gates[:], in_=gates[:]), reads=["gates"])
        if SPARSE:
            moe_sparse(nc, g, S, sb, P, PB, nbk, gates, LG, MK, TOP8, identf, fg)
            return
        S.barrier()
        NT = TBK // 128
        hb = sb("hb", [128, KC, TBK], BF16)
        actT = sb("actT", [128, KC, TBK], BF16)
        acc = sb("acc", [128, NT, D], F32)
        wg = [sb("wg%d" % i, [128, KC, 128], BF16) for i in range(2)]
        wu = [sb("wu%d" % i, [128, KC, 128], BF16) for i in range(2)]
        wd = [sb("wd%d" % i, [128, KC, 256], BF16) for i in range(2)]
        bgu = sb("bgu", [128, 2, KC], F32)
        bdr = sb("bdr", [1, D], BF16)
        ones = sb("ones", [1, 128], BF16)
        Gt, Ut, St = sb("Gt", [128, 512], F32), sb("Ut", [128, 512], F32), sb("St", [128, 512], F32)
        S.op("pool", lambda e: e.memset(ones[:], 1.0), writes=["ones"])
        acc_x = sb("acc_x", [128, D], F32)
        fsm = sb("fsm", [128, 2], F32)
        wv = lambda w, e_: w[e_].rearrange("(kc p) f -> p kc f", p=128)
        wi = 0
        for blk_i in range(nblk):
            t0 = blk_i * TBK
            S.dma(lambda e, t0=t0: e.dma_start(out=hb[:], in_=g.h2T_scr[:, :, t0:t0 + TBK].rearrange("k p t -> p k t")), reads=["h2scr"], writes=["hb"])
            S.op("pool", lambda e: e.memset(acc[:], 0.0), writes=["acc"])
            bb = t0 // SEQ
            S.dma(lambda e, bb=bb: e.dma_start(out=m5t[:], in_=g.mod_row[bb, 5 * D:6 * D].partition_broadcast(128)), reads=["modrow"], writes=["m5t"])
            for ex_i in range(nexp):
                if ex_i % 4 == 0:
                    S.flush()
                S.dma(lambda e, ex_i=ex_i: e.dma_start(out=bgu[:], in_=g.ex_bgu[ex_i]), writes=["bgu"])
                S.dma(lambda e, ex_i=ex_i: e.dma_start(out=bdr[:], in_=g.ex_bd[ex_i:ex_i + 1, :]), writes=["bdr"], q="pool")
                for fs in range(D // 128):
                    p = wi % 2
                    wi += 1
                    S.dma(lambda e, p=p, ex_i=ex_i, fs=fs: e.dma_start(out=wg[p][:], in_=wv(g.ex_wg, ex_i)[:, :, fs * 128:(fs + 1) * 128]), writes=["wg%d" % p], q="pool")
                    S.dma(lambda e, p=p, ex_i=ex_i, fs=fs: e.dma_start(out=wu[p][:], in_=wv(g.ex_wu, ex_i)[:, :, fs * 128:(fs + 1) * 128]), writes=["wu%d" % p], q="pool")
                    for j in range(1):
                        fc = fs
                        for tg in range(TBK // 512):
                            ts_ = slice(tg * 512, (tg + 1) * 512)
                            kg, ku = nbk(), nbk()
                            for kc in range(KC):
                                S.op("pe", lambda e, p=p, j=j, kc=kc, ts_=ts_, kg=kg: e.matmul(P[kg][:, 0:512], lhsT=wg[p][:, kc, j * 128:(j + 1) * 128], rhs=hb[:, kc, ts_],
                                                                                              start=(kc == 0), stop=(kc == KC - 1)), reads=["wg%d" % p, "hb"], writes=["P%d" % kg])
                            for kc in range(KC):
                                S.op("pe", lambda e, p=p, j=j, kc=kc, ts_=ts_, ku=ku: e.matmul(P[ku][:, 0:512], lhsT=wu[p][:, kc, j * 128:(j + 1) * 128], rhs=hb[:, kc, ts_],
                                                                                              start=(kc == 0), stop=(kc == KC - 1)), reads=["wu%d" % p, "hb"], writes=["P%d" % ku])
                            S.op("dve", lambda e, kg=kg, fc=fc: e.tensor_scalar(out=Gt[:], in0=P[kg][:, 0:512], scalar1=bgu[:, 0, fc:fc + 1], scalar2=7.0, op0=ALU.add, op1=ALU.min),
                                 reads=["P%d" % kg, "bgu"], writes=["Gt"])
                            S.op("act", lambda e: e.activation(out=St[:], in_=Gt[:], func=AF.Sigmoid, scale=1.702), reads=["Gt"], writes=["St"])
                            S.op("dve", lambda e, ku=ku, fc=fc: e.tensor_scalar(out=Ut[:], in0=P[ku][:, 0:512], scalar1=bgu[:, 1, fc:fc + 1], scalar2=7.0, op0=ALU.add, op1=ALU.min),
                                 reads=["P%d" % ku, "bgu"], writes=["Ut"])
                            S.op("dve", lambda e: e.tensor_scalar(out=Ut[:], in0=Ut[:], scalar1=-7.0, scalar2=1.0, op0=ALU.max, op1=ALU.add), reads=["Ut"], writes=["Ut"])
                            S.op("dve", lambda e: e.tensor_tensor(out=Gt[:], in0=Gt[:], in1=St[:], op=ALU.mult), reads=["Gt", "St"], writes=["Gt"])
                            S.op("dve", lambda e, fc=fc, ts_=ts_: e.tensor_tensor(out=actT[:, fc, ts_], in0=Ut[:], in1=Gt[:], op=ALU.mult), reads=["Ut", "Gt"], writes=["actT"])
                for dj in range(8):
                    p = dj % 2
                    S.dma(lambda e, p=p, ex_i=ex_i, dj=dj: e.dma_start(out=wd[p][:], in_=wv(g.ex_wd, ex_i)[:, :, dj * 256:(dj + 1) * 256]), writes=["wd%d" % p], q="pool")
                    ds_ = slice(dj * 256, (dj + 1) * 256)
                    for tt in range(NT):
                        bk = nbk()
                        gt = blk_i * NT + tt
                        for fc in range(KC):
                            S.op("pe", lambda e, p=p, fc=fc, tt=tt, bk=bk: e.matmul(P[bk][:, 0:256], lhsT=actT[:, fc, tt * 128:(tt + 1) * 128], rhs=wd[p][:, fc, :],
                                                                                   start=(fc == 0), stop=False), reads=["actT", "wd%d" % p], writes=["P%d" % bk])
                        S.op("pe", lambda e, bk=bk, ds_=ds_: e.matmul(P[bk][:, 0:256], lhsT=ones[0:1, :], rhs=bdr[0:1, ds_], start=False, stop=True),
                             reads=["ones", "bdr"], writes=["P%d" % bk])
                        S.op("dve", lambda e, bk=bk, tt=tt, ds_=ds_, gt=gt, ex_i=ex_i: e.scalar_tensor_tensor(
                            out=acc[:, tt, ds_], in0=P[bk][:, 0:256], scalar=gates[:, gt, ex_i:ex_i + 1], in1=acc[:, tt, ds_], op0=ALU.mult, op1=ALU.add),
                            reads=["P%d" % bk, "gates", "acc"], writes=["acc"])
            S.flush()
            for tt in range(NT):
                gt = blk_i * NT + tt
                b = gt // (SEQ // 128)
                S.dma(lambda e, gt=gt: e.dma_start(out=acc_x[:], in_=g.x1_scr[gt * 128:(gt + 1) * 128, :]), reads=["x1scr"], writes=["accx"])
                S.op("dve", lambda e, tt=tt, b=b: e.tensor_tensor(out=acc[:, tt, :], in0=acc[:, tt, :], in1=m5t[:], op=ALU.mult), reads=["acc", "m5t"], writes=["acc"])
                S.op("dve", lambda e, tt=tt: e.tensor_tensor(out=acc[:, tt, :], in0=acc[:, tt, :], in1=acc_x[:], op=ALU.add), reads=["acc", "accx"], writes=["acc"])
                S.op("act", lambda e, tt=tt: e.activation(out=acc_x[:], in_=acc[:, tt, :], func=AF.Square), reads=["acc"], writes=["accx"])
                S.op("dve", lambda e: e.tensor_reduce(out=fsm[:, 0:1], in_=acc_x[:], axis=AX.X, op=ALU.add), reads=["accx"], writes=["fsm"])
                S.op("dve", lambda e: e.tensor_scalar(out=fsm[:, 0:1], in0=fsm[:, 0:1], scalar1=1.0 / D, scalar2=EPS, op0=ALU.mult, op1=ALU.add), reads=["fsm"], writes=["fsm"])
                S.op("act", lambda e: e.sqrt(out=fsm[:, 0:1], in_=fsm[:, 0:1]), reads=["fsm"], writes=["fsm"])
                S.op("dve", lambda e: e.reciprocal(out=fsm[:, 0:1], in_=fsm[:, 0:1]), reads=["fsm"], writes=["fsm"])
                S.op("dve", lambda e, tt=tt: e.scalar_tensor_tensor(out=acc[:, tt, :], in0=acc[:, tt, :], scalar=fsm[:, 0:1], in1=fg[:], op0=ALU.mult, op1=ALU.mult),
                     reads=["acc", "fsm", "fg"], writes=["acc"])
                t_in_b = gt % (SEQ // 128)
                S.dma(lambda e, tt=tt, b=b, t_in_b=t_in_b: e.dma_start(out=g.out[b, t_in_b * 128:(t_in_b + 1) * 128, :], in_=acc[:, tt, :]), reads=["acc"])
        S.flush()


def moe_sparse(nc, g, S, sb, P, PB, nbk, gates, LG, MK, TOP8, identf, fg):
    U32 = mybir.dt.uint32
    MAGIC = 12582912.0
    NTT = NTOK // 128
    S.barrier()
    stA = contextlib.ExitStack()
    sbp = sb
    sb = lambda name, shape, dtype: stA.enter_context(nc.sbuf_tensor("spa_" + name, shape, dtype))
    base4, piota = sbp("base4", [128, 4, KC], F32), sbp("piota", [128, 1], F32)
    exs = sbp("exs", [128, NSB], F32)
    identb = sbp("sidentb", [128, 128], BF16)
    D4i, GS4 = sbp("D4i", [128, NTOK // 128, 4], I32), sbp("GS4", [128, NTOK // 128, 4], F32)
    tri, onesf = sb("tri", [128, 128], F32), sb("onesf", [128, 128], F32)
    S.dma(lambda e: e.dma_start(out=identb[:], in_=g.ident_bf[:]), writes=["identb"])
    sidx = sb("sidx", [128, NSB], F32)
    for tname, tt, src in (("tri", tri, g.sp_tri), ("onesf", onesf, g.sp_ones), ("sidx", sidx, g.sp_sidx), ("base4", base4, g.sp_base4), ("piota", piota, g.sp_piota)):
        S.dma(lambda e, tt=tt, src=src: e.dma_start(out=tt[:], in_=src[:]), writes=[tname])
    cnt, nbt, pend, pstart = sb("cnt", [128, NE], F32), sb("nbt", [128, NE], F32), sb("pend", [128, NE], F32), sb("pstart", [128, NE], F32)
    t3 = sb("t3", [128, NSB, NE], F32)
    D4f = sb("D4f", [128, NTT, 4], F32)
    pos, oh = sb("pos", [128, NE], F32), sb("oh", [128, NE], F32)
    zt = sb("zt", [128, D], BF16)
    S.op("pool", lambda e: e.memset(zt[:], 0.0), writes=["zt"])
    for i in range(NSB * SBR // 128):
        S.dma(lambda e, i=i: e.dma_start(out=g.xs_scr[i * 128:(i + 1) * 128, :], in_=zt[:]), reads=["zt"], writes=["xs"])
    bk = nbk()
    for g2 in range(NTT):
        S.op("pe", lambda e, g2=g2, bk=bk: e.matmul(P[bk][:, 0:NE], lhsT=onesf[:], rhs=MK[:, g2, :], start=(g2 == 0), stop=(g2 == NTT - 1)), reads=["onesf", "MK"], writes=["P%d" % bk])
    S.op("dve", lambda e, bk=bk: e.tensor_copy(out=cnt[:], in_=P[bk][:, 0:NE]), reads=["P%d" % bk], writes=["cnt"])
    S.op("dve", lambda e: e.tensor_scalar(out=nbt[:], in0=cnt[:], scalar1=1.0 / SBR, scalar2=(SBR - 1.0) / SBR - 0.5 + 0.5 / SBR, op0=ALU.mult, op1=ALU.add), reads=["cnt"], writes=["nbt"])
    S.op("dve", lambda e: e.tensor_scalar(out=nbt[:], in0=nbt[:], scalar1=MAGIC, scalar2=None, op0=ALU.add), reads=["nbt"], writes=["nbt"])
    S.op("dve", lambda e: e.tensor_scalar(out=nbt[:], in0=nbt[:], scalar1=-MAGIC, scalar2=None, op0=ALU.add), reads=["nbt"], writes=["nbt"])
    S.op("dve", lambda e: e.tensor_tensor_scan(out=pend[:], data0=onesf[:, 0:NE], data1=nbt[:], initial=0.0, op0=ALU.mult, op1=ALU.add), reads=["onesf", "nbt"], writes=["pend"])
    S.op("dve", lambda e: e.tensor_tensor(out=pstart[:], in0=pend[:], in1=nbt[:], op=ALU.subtract), reads=["pend", "nbt"], writes=["pstart"])
    S.op("dve", lambda e: e.tensor_scalar(out=pstart[:], in0=pstart[:], scalar1=float(SBR), scalar2=None, op0=ALU.mult), reads=["pstart"], writes=["pstart"])
    S.op("dve", lambda e: e.tensor_tensor(out=t3[:], in0=pend[:].unsqueeze(1).to_broadcast([128, NSB, NE]), in1=sidx[:].unsqueeze(2).to_broadcast([128, NSB, NE]), op=ALU.is_le),
         reads=["pend", "sidx"], writes=["t3"])
    S.op("dve", lambda e: e.tensor_reduce(out=exs[:], in_=t3[:], axis=AX.X, op=ALU.add), reads=["t3"], writes=["exs"])
    S.op("dve", lambda e: e.tensor_scalar(out=exs[:], in0=exs[:], scalar1=float(NE - 1), scalar2=None, op0=ALU.min), reads=["exs"], writes=["exs"])
    for gt in range(NTT):
        bk = nbk()
        S.op("pe", lambda e, gt=gt, bk=bk: e.matmul(P[bk][:, 0:NE], lhsT=tri[:], rhs=MK[:, gt, :], start=True, stop=(gt == 0)), reads=["tri", "MK"], writes=["P%d" % bk])
        for g2 in range(gt):
            S.op("pe", lambda e, g2=g2, bk=bk, gt=gt: e.matmul(P[bk][:, 0:NE], lhsT=onesf[:], rhs=MK[:, g2, :], start=False, stop=(g2 == gt - 1)), reads=["onesf", "MK"], writes=["P%d" % bk])
        S.op("dve", lambda e, bk=bk: e.tensor_tensor(out=pos[:], in0=P[bk][:, 0:NE], in1=pstart[:], op=ALU.add), reads=["P%d" % bk, "pstart"], writes=["pos"])
        for k in range(4):
            S.op("dve", lambda e, gt=gt, k=k: e.tensor_scalar(out=oh[:], in0=LG[:, gt, :], scalar1=TOP8[:, gt, k:k + 1], scalar2=None, op0=ALU.is_equal), reads=["LG", "TOP8"], writes=["oh"])
            S.op("dve", lambda e: e.tensor_tensor(out=t3[:, 0, :], in0=oh[:], in1=pos[:], op=ALU.mult), reads=["oh", "pos"], writes=["t3"])
            S.op("dve", lambda e, gt=gt, k=k: e.tensor_reduce(out=D4f[:, gt, k:k + 1], in_=t3[:, 0, :], axis=AX.X, op=ALU.add), reads=["t3"], writes=["D4f"])
            S.op("dve", lambda e, gt=gt: e.tensor_tensor(out=t3[:, 1, :], in0=oh[:], in1=gates[:, gt, :], op=ALU.mult), reads=["oh", "gates"], writes=["t3"])
            S.op("dve", lambda e, gt=gt, k=k: e.tensor_reduce(out=GS4[:, gt, k:k + 1], in_=t3[:, 1, :], axis=AX.X, op=ALU.add), reads=["t3"], writes=["GS4"])
    S.op("dve", lambda e: e.tensor_copy(out=D4i[:], in_=D4f[:]), reads=["D4f"], writes=["D4i"])
    hrow = [sb("hrow%d" % i, [128, D], BF16) for i in range(2)]
    for gt in range(NTT):
        p = gt % 2
        S.dma(lambda e, p=p, gt=gt: e.dma_start(out=hrow[p][:], in_=g.h2tm_scr[gt * 128:(gt + 1) * 128, :]), reads=["h2tm"], writes=["hrow%d" % p])
        for k in range(4):
            S.dma(lambda e, p=p, gt=gt, k=k: e.indirect_dma_start(out=g.xs_scr[:, :], out_offset=bass.IndirectOffsetOnAxis(ap=D4i[:, gt, k:k + 1].bitcast(U32), axis=0),
                                                                 in_=hrow[p][:], in_offset=None), reads=["hrow%d" % p, "D4i", "xs"], writes=["xs"], q="pool")
    S.barrier()
    S.flush()
    stA.close()
    stB = contextlib.ExitStack()
    sb = lambda name, shape, dtype: stB.enter_context(nc.sbuf_tensor("spb_" + name, shape, dtype))
    idxf, idxi = sb("idxf", [128, 4, KC], F32), [sb("idxi%d" % i, [128, 4, KC], I32) for i in range(2)]
    bidf, bidi = sb("bidf", [128, 1], F32), [sb("bidi%d" % i, [128, 1], I32) for i in range(2)]
    xrow = [sb("xrow%d" % i, [128, D], BF16) for i in range(2)]
    hbT = sb("hbT", [128, KC, SBR], BF16)
    actT = sb("sactT", [128, KC, SBR], BF16)
    wg = [sb("swg%d" % i, [128, KC, 512], BF16) for i in range(2)]
    wu = [sb("swu%d" % i, [128, KC, 512], BF16) for i in range(2)]
    bgu = [sb("sbgu%d" % i, [128, 2 * KC], F32) for i in range(2)]
    Gt, Ut, St = sb("sGt", [128, 512], F32), sb("sUt", [128, 512], F32), sb("sSt", [128, 512], F32)
    yst = [sb("yst%d" % i, [128, 512], F32) for i in range(2)]
    wg4 = g.ex_wg.rearrange("e d (q f) -> (e d q) f", f=512)
    wu4 = g.ex_wu.rearrange("e d (q f) -> (e d q) f", f=512)
    wd4 = g.ex_wd.rearrange("e d (q f) -> (e d q) f", f=512)
    bgu2 = g.ex_bgu.rearrange("e p a k -> (e p) (a k)")
    wi = 0
    yi = 0
    for s_ in range(NSB):
        if s_ % 2 == 0:
            S.flush()
        ip = s_ % 2
        S.op("dve", lambda e, s_=s_: e.scalar_tensor_tensor(out=idxf[:], in0=exs[:, s_:s_ + 1].unsqueeze(2).to_broadcast([128, 4, KC]), scalar=8192.0, in1=base4[:], op0=ALU.mult, op1=ALU.add),
             reads=["exs", "base4"], writes=["idxf"])
        S.op("dve", lambda e, ip=ip: e.tensor_copy(out=idxi[ip][:], in_=idxf[:]), reads=["idxf"], writes=["idxi%d" % ip])
        S.op("dve", lambda e, s_=s_: e.scalar_tensor_tensor(out=bidf[:], in0=exs[:, s_:s_ + 1], scalar=128.0, in1=piota[:], op0=ALU.mult, op1=ALU.add), reads=["exs", "piota"], writes=["bidf"])
        S.op("dve", lambda e, ip=ip: e.tensor_copy(out=bidi[ip][:], in_=bidf[:]), reads=["bidf"], writes=["bidi%d" % ip])
        S.dma(lambda e, ip=ip: e.indirect_dma_start(out=bgu[ip][:], out_offset=None, in_=bgu2[:, :], in_offset=bass.IndirectOffsetOnAxis(ap=bidi[ip][:, 0:1].bitcast(U32), axis=0)),
              reads=["bidi%d" % ip], writes=["sbgu%d" % ip], q="pool")
        for tt in range(SBR // 128):
            xp = tt % 2
            S.dma(lambda e, xp=xp, s_=s_, tt=tt: e.dma_start(out=xrow[xp][:], in_=g.xs_scr[s_ * SBR + tt * 128: s_ * SBR + (tt + 1) * 128, :]), reads=["xs"], writes=["xrow%d" % xp])
            for half in range(2):
                for q in range(8):
                    kc = half * 8 + q
                    S.op("pe", lambda e, xp=xp, kc=kc, q=q: e.transpose(PB[:, q * 128:(q + 1) * 128], xrow[xp][:, kc * 128:(kc + 1) * 128], identb[:]), reads=["xrow%d" % xp, "identb"], writes=["PB"])
                S.op("act", lambda e, half=half, tt=tt: e.copy(out=hbT[:, half * 8:(half + 1) * 8, tt * 128:(tt + 1) * 128], in_=PB[:, :].rearrange("p (q f) -> p q f", f=128)),
                     reads=["PB"], writes=["hbT"])
        for fs in range(4):
            p = wi % 2
            wi += 1
            for kc in range(KC):
                S.dma(lambda e, p=p, ip=ip, fs=fs, kc=kc: e.indirect_dma_start(out=wg[p][:, kc, :], out_offset=None, in_=wg4[:, :],
                                                                            in_offset=bass.IndirectOffsetOnAxis(ap=idxi[ip][:, fs, kc:kc + 1].bitcast(U32), axis=0)),
                      reads=["idxi%d" % ip], writes=["swg%d" % p], q="pool")
                S.dma(lambda e, p=p, ip=ip, fs=fs, kc=kc: e.indirect_dma_start(out=wu[p][:, kc, :], out_offset=None, in_=wu4[:, :],
                                                                            in_offset=bass.IndirectOffsetOnAxis(ap=idxi[ip][:, fs, kc:kc + 1].bitcast(U32), axis=0)),
                      reads=["idxi%d" % ip], writes=["swu%d" % p], q="pool")
            for j in range(4):
                fc = fs * 4 + j
                kg, ku = nbk(), nbk()
                for kc in range(KC):
                    S.op("pe", lambda e, p=p, j=j, kc=kc, kg=kg: e.matmul(P[kg][:, 0:SBR], lhsT=wg[p][:, kc, j * 128:(j + 1) * 128], rhs=hbT[:, kc, :], start=(kc == 0), stop=(kc == KC - 1)),
                         reads=["swg%d" % p, "hbT"], writes=["P%d" % kg])
                for kc in range(KC):
                    S.op("pe", lambda e, p=p, j=j, kc=kc, ku=ku: e.matmul(P[ku][:, 0:SBR], lhsT=wu[p][:, kc, j * 128:(j + 1) * 128], rhs=hbT[:, kc, :], start=(kc == 0), stop=(kc == KC - 1)),
                         reads=["swu%d" % p, "hbT"], writes=["P%d" % ku])
                S.op("dve", lambda e, kg=kg, fc=fc, ip=ip: e.tensor_scalar(out=Gt[:], in0=P[kg][:, 0:SBR], scalar1=bgu[ip][:, fc:fc + 1], scalar2=7.0, op0=ALU.add, op1=ALU.min),
                     reads=["P%d" % kg, "sbgu%d" % ip], writes=["sGt"])
                S.op("act", lambda e: e.activation(out=St[:], in_=Gt[:], func=AF.Sigmoid, scale=1.702), reads=["sGt"], writes=["sSt"])
                S.op("dve", lambda e, ku=ku, fc=fc, ip=ip: e.tensor_scalar(out=Ut[:], in0=P[ku][:, 0:SBR], scalar1=bgu[ip][:, KC + fc:KC + fc + 1], scalar2=7.0, op0=ALU.add, op1=ALU.min),
                     reads=["P%d" % ku, "sbgu%d" % ip], writes=["sUt"])
                S.op("dve", lambda e: e.tensor_scalar(out=Ut[:], in0=Ut[:], scalar1=-7.0, scalar2=1.0, op0=ALU.max, op1=ALU.add), reads=["sUt"], writes=["sUt"])
                S.op("dve", lambda e: e.tensor_tensor(out=Gt[:], in0=Gt[:], in1=St[:], op=ALU.mult), reads=["sGt", "sSt"], writes=["sGt"])
                S.op("dve", lambda e, fc=fc: e.tensor_tensor(out=actT[:, fc, :], in0=Ut[:], in1=Gt[:], op=ALU.mult), reads=["sUt", "sGt"], writes=["sactT"])
        for dj in range(4):
            p = wi % 2
            wi += 1
            for fc in range(KC):
                S.dma(lambda e, p=p, ip=ip, dj=dj, fc=fc: e.indirect_dma_start(out=wg[p][:, fc, :], out_offset=None, in_=wd4[:, :],
                                                                            in_offset=bass.IndirectOffsetOnAxis(ap=idxi[ip][:, dj, fc:fc + 1].bitcast(U32), axis=0)),
                      reads=["idxi%d" % ip], writes=["swg%d" % p], q="pool")
            for tt in range(SBR // 128):
                bk = nbk()
                for fc in range(KC):
                    S.op("pe", lambda e, p=p, fc=fc, tt=tt, bk=bk: e.matmul(P[bk][:, 0:512], lhsT=actT[:, fc, tt * 128:(tt + 1) * 128], rhs=wg[p][:, fc, :], start=(fc == 0), stop=(fc == KC - 1)),
                         reads=["sactT", "swg%d" % p], writes=["P%d" % bk])
                yp = yi % 2
                yi += 1
                S.op("act", lambda e, yp=yp, bk=bk: e.copy(out=yst[yp][:], in_=P[bk][:, 0:512]), reads=["P%d" % bk], writes=["yst%d" % yp])
                S.dma(lambda e, yp=yp, s_=s_, tt=tt, dj=dj: e.dma_start(out=g.ys_scr[s_ * SBR + tt * 128: s_ * SBR + (tt + 1) * 128, dj * 512:(dj + 1) * 512], in_=yst[yp][:]),
                      reads=["yst%d" % yp], writes=["ys"])
    S.barrier()
    S.flush()
    stB.close()
    stC = contextlib.ExitStack()
    sb = lambda name, shape, dtype: stC.enter_context(nc.sbuf_tensor("spc_" + name, shape, dtype))
    acc = sb("cacc", [128, D], F32)
    yrow = [sb("yrow%d" % i, [128, D], F32) for i in range(2)]
    x1t = sb("x1t", [128, D], F32)
    m5t = sb("cm5t", [128, D], F32)
    bdall = sb("bdall", [NE, D], F32)
    gT = sb("gT", [NE, 128], F32)
    fsm = sb("cfsm", [128, 2], F32)
    S.dma(lambda e: e.dma_start(out=bdall[:], in_=g.ex_bd[:, :]), writes=["bdall"])
    yk = 0
    for gt in range(NTT):
        if gt % 8 == 0:
            S.flush()
        b = gt // (SEQ // 128)
        if gt % (SEQ // 128) == 0:
            S.dma(lambda e, b=b: e.dma_start(out=m5t[:], in_=g.mod_row[b, 5 * D:6 * D].partition_broadcast(128)), reads=["modrow"], writes=["cm5t"])
        S.dma(lambda e, gt=gt: e.dma_start(out=x1t[:], in_=g.x1_scr[gt * 128:(gt + 1) * 128, :]), reads=["x1scr"], writes=["x1t"])
        bk = nbk()
        S.op("pe", lambda e, gt=gt, bk=bk: e.transpose(P[bk][0:NE, 0:128], gates[:, gt, :], identf[:]), reads=["gates", "identf"], writes=["P%d" % bk])
        S.op("act", lambda e, bk=bk: e.copy(out=gT[:], in_=P[bk][0:NE, 0:128]), reads=["P%d" % bk], writes=["gT"])
        for dj in range(4):
            bk = nbk()
            S.op("pe", lambda e, dj=dj, bk=bk: e.matmul(P[bk][:, 0:512], lhsT=gT[:], rhs=bdall[:, dj * 512:(dj + 1) * 512], start=True, stop=True), reads=["gT", "bdall"], writes=["P%d" % bk])
            S.op("act", lambda e, dj=dj, bk=bk: e.copy(out=acc[:, dj * 512:(dj + 1) * 512], in_=P[bk][:, 0:512]), reads=["P%d" % bk], writes=["cacc"])
        for k in range(4):
            yp = yk % 2
            yk += 1
            S.dma(lambda e, yp=yp, gt=gt, k=k: e.indirect_dma_start(out=yrow[yp][:], out_offset=None, in_=g.ys_scr[:, :],
                                                                   in_offset=bass.IndirectOffsetOnAxis(ap=D4i[:, gt, k:k + 1].bitcast(U32), axis=0)),
                  reads=["ys", "D4i"], writes=["yrow%d" % yp], q="pool")
            S.op("dve", lambda e, yp=yp, gt=gt, k=k: e.scalar_tensor_tensor(out=acc[:], in0=yrow[yp][:], scalar=GS4[:, gt, k:k + 1], in1=acc[:], op0=ALU.mult, op1=ALU.add),
                 reads=["yrow%d" % yp, "GS4", "cacc"], writes=["cacc"])
        S.op("dve", lambda e: e.tensor_tensor(out=acc[:], in0=acc[:], in1=m5t[:], op=ALU.mult), reads=["cacc", "cm5t"], writes=["cacc"])
        S.op("dve", lambda e: e.tensor_tensor(out=acc[:], in0=acc[:], in1=x1t[:], op=ALU.add), reads=["cacc", "x1t"], writes=["cacc"])
        S.op("act", lambda e: e.activation(out=x1t[:], in_=acc[:], func=AF.Square), reads=["cacc"], writes=["x1t"])
        S.op("dve", lambda e: e.tensor_reduce(out=fsm[:, 0:1], in_=x1t[:], axis=AX.X, op=ALU.add), reads=["x1t"], writes=["cfsm"])
        S.op("dve", lambda e: e.tensor_scalar(out=fsm[:, 0:1], in0=fsm[:, 0:1], scalar1=1.0 / D, scalar2=EPS, op0=ALU.mult, op1=ALU.add), reads=["cfsm"], writes=["cfsm"])
        S.op("act", lambda e: e.sqrt(out=fsm[:, 0:1], in_=fsm[:, 0:1]), reads=["cfsm"], writes=["cfsm"])
        S.op("dve", lambda e: e.reciprocal(out=fsm[:, 0:1], in_=fsm[:, 0:1]), reads=["cfsm"], writes=["cfsm"])
        S.op("dve", lambda e: e.scalar_tensor_tensor(out=x1t[:], in0=acc[:], scalar=fsm[:, 0:1], in1=fg[:], op0=ALU.mult, op1=ALU.mult), reads=["cacc", "cfsm", "fg"], writes=["x1t"])
        t_in_b = gt % (SEQ // 128)
        S.dma(lambda e, b=b, t_in_b=t_in_b: e.dma_start(out=g.out[b, t_in_b * 128:(t_in_b + 1) * 128, :], in_=x1t[:]), reads=["x1t"])
    S.barrier()
    S.flush()
    stC.close()
```

```python
import contextlib
import numpy as np
import ml_dtypes
import concourse.bass as bass
import concourse.mybir as mybir
from concourse.bass_utils import run_bass_kernel_spmd

F32 = mybir.dt.float32
BF16 = mybir.dt.bfloat16
I32 = mybir.dt.int32
ALU = mybir.AluOpType
AF = mybir.ActivationFunctionType
AX = mybir.AxisListType

NCORES = 8
D = 2048
KC = 16
SEQ = 2048
CTX = 256
TB = SEQ + CTX
BPC = 2
EPS = 1e-6


class Sched:
    ENGS = ("pe", "act", "dve", "pool", "sp")

    _uid = [0]

    def __init__(self, nc, stack, ndma=24):
        self.nc = nc
        Sched._uid[0] += 1
        u = "s%d_" % Sched._uid[0]
        self.sem = {e: stack.enter_context(nc.semaphore(u + "pg_" + e)) for e in self.ENGS}
        self.dsem = [stack.enter_context(nc.semaphore(u + "dq%d" % i)) for i in range(ndma)]
        self.dval = [0] * ndma
        self.dnext = {"sp": 0, "pool": 0}
        self.dpool = {"sp": list(range(0, ndma // 2)), "pool": list(range(ndma // 2, ndma))}
        self.cnt = {e: 0 for e in self.ENGS}
        self.prog = {e: [] for e in self.ENGS}
        self.waited = {e: {} for e in self.ENGS}
        self.res = {}

    def _r(self, k):
        r = self.res.get(k)
        if r is None:
            r = self.res[k] = {"w": None, "r": {}}
        return r

    def _deps(self, eng, reads, writes):
        need = {}

        def add(sk, v):
            if sk == "pe" and eng == "pe":
                return
            if need.get(sk, 0) < v:
                need[sk] = v
        for k in reads:
            w = self._r(k)["w"]
            if w:
                add(*w)
        for k in writes:
            r = self._r(k)
            if r["w"]:
                add(*r["w"])
            for sk, v in r["r"].items():
                add(sk, v)
        out = []
        wd = self.waited[eng]
        for sk, v in need.items():
            if wd.get(sk, 0) >= v:
                continue
            wd[sk] = v
            out.append((sk, v))
        return out

    def _mark(self, reads, writes, sk, v):
        for k in reads:
            self._r(k)["r"][sk] = v
        for k in writes:
            r = self._r(k)
            r["w"] = (sk, v)
            r["r"] = {}

    alias = {}

    def _x(self, keys):
        out = []
        for k in keys:
            out.extend(self.alias.get(k, (k,)))
        return out

    def op(self, eng, fn, reads=(), writes=()):
        reads, writes = self._x(reads), self._x(writes)
        deps = self._deps(eng, reads, writes)
        self.cnt[eng] += 1
        n = self.cnt[eng]
        self.prog[eng].append((deps, fn, (eng, 1)))
        self._mark(reads, writes, eng, n)

    def dma(self, fn, reads=(), writes=(), q="sp"):
        reads, writes = self._x(reads), self._x(writes)
        pool = self.dpool[q]
        i = pool[self.dnext[q]]
        self.dnext[q] = (self.dnext[q] + 1) % len(pool)
        sk = ("d", i)
        deps = self._deps(q, reads, writes)
        if self.dval[i] > 0 and self.waited[q].get(sk, 0) < self.dval[i]:
            self.waited[q][sk] = self.dval[i]
            deps.append((sk, self.dval[i]))
        self.dval[i] += 16
        self.prog[q].append((deps, fn, (sk, 16)))
        self._mark(reads, writes, sk, self.dval[i])

    def barrier(self):
        for eng in self.ENGS:
            deps = []
            for e in self.ENGS:
                if e != eng and self.cnt[e] and self.waited[eng].get(e, 0) < self.cnt[e]:
                    deps.append((e, self.cnt[e]))
                    self.waited[eng][e] = self.cnt[e]
            for i, v in enumerate(self.dval):
                if v and self.waited[eng].get(("d", i), 0) < v:
                    deps.append((("d", i), v))
                    self.waited[eng][("d", i)] = v
            if deps:
                self.prog[eng].append((deps, None, None))

    def drain(self, eng="sp"):
        deps = []
        for e in self.ENGS:
            if self.cnt[e] and self.waited[eng].get(e, 0) < self.cnt[e] and e != eng:
                deps.append((e, self.cnt[e]))
        for i, v in enumerate(self.dval):
            if v and self.waited[eng].get(("d", i), 0) < v:
                deps.append((("d", i), v))
        self.prog[eng].append((deps, None, None))

    def flush(self, final=False):
        if final:
            self.barrier()
        with self.nc.Block() as block:
            self.emit(block)
        self.prog = {e: [] for e in self.ENGS}

    def _semh(self, sk):
        return self.sem[sk] if isinstance(sk, str) else self.dsem[sk[1]]

    def emit(self, block):
        def run(eng_handle, name):
            for deps, fn, inc in self.prog[name]:
                for sk, v in deps:
                    eng_handle.wait_ge(self._semh(sk), v)
                if fn is not None:
                    ins = fn(eng_handle)
                    ins.then_inc(self._semh(inc[0]), inc[1])

        @block.sync
        def _(e):
            run(e, "sp")

        @block.tensor
        def _(e):
            run(e, "pe")

        @block.vector
        def _(e):
            run(e, "dve")

        @block.scalar
        def _(e):
            run(e, "act")

        @block.gpsimd
        def _(e):
            run(e, "pool")


PW = 6144
NZ = PW // 128
NLORA = 3


def _fm(v, nch):
    return np.ascontiguousarray(np.asarray(v, np.float32).reshape(nch, 128).T)


class Ctx:
    pass


def declare_io(nc, stage):
    g = Ctx()
    dt = nc.dram_tensor
    inp = lambda name, shape, dtype=F32: dt(name, list(shape), dtype, kind="ExternalInput").ap()
    g.x = inp("x", [BPC, SEQ, D])
    g.ctx = inp("ctx", [BPC, CTX, D])
    g.condT = inp("condT", [128, KC, 3])
    g.ada_w = inp("ada_w", [D, 6 * D])
    g.ada_bT = inp("ada_bT", [128, 6 * KC])
    g.g1T = inp("g1T", [128, KC])
    g.g2T = inp("g2T", [128, KC])
    g.in_w = inp("in_w", [D, PW])
    g.lora_w = inp("lora_w", [D, NLORA * 128])
    g.conv_wT = inp("conv_wT", [128, NZ, 3])
    g.conv_bT = inp("conv_bT", [128, NZ])
    g.final_g = inp("final_g", [1, D])
    g.ident_bf = inp("ident_bf", [128, 128], BF16)
    g.ident_f = inp("ident_f", [128, 128], F32)
    g.out = dt("out", [BPC, SEQ, D], F32, kind="ExternalOutput").ap()
    g.ada_brow = inp("ada_brow", [1, 6 * D])
    g.mod_row = (dt("mod_row_scr", [3, 6 * D], F32, kind="ExternalOutput") if stage == "dbgA" else dt("mod_row_scr", [3, 6 * D], F32)).ap()
    g.zT = dt("zT_scr", [BPC, NZ, 128, TB], F32).ap()
    g.loraT = dt("loraT_scr", [BPC, NLORA, 128, TB], F32).ap()
    if stage == "dbgA":
        g.dbg_mod = dt("dbg_mod", [128, 6 * KC, 3], F32, kind="ExternalOutput").ap()
    if stage in ("dbgC",):
        g.dbg_z = dt("dbg_z", [BPC, NZ, 128, TB], F32, kind="ExternalOutput").ap()
        g.dbg_lora = dt("dbg_lora", [BPC, NLORA, 128, TB], F32, kind="ExternalOutput").ap()
    return g


def phase_ABC(nc, g, stage):
    with contextlib.ExitStack() as st:
        S = g.S
        S.barrier()
        sb = lambda name, shape, dtype: st.enter_context(nc.sbuf_tensor(name, shape, dtype))
        ps = [st.enter_context(nc.psum_tensor("ps%d" % i, [128, 512], F32)) for i in range(6)]
        pbf = [st.enter_context(nc.psum_tensor("pbf%d" % i, [128, 1024], BF16)) for i in range(1)]
        pbf_row = st.enter_context(nc.psum_tensor("pbf_row", [128, 512], F32))
        modT = sb("modT", [128, 6 * KC, 3], F32)
        condT = sb("condT_s", [128, KC, 3], F32)
        adab = sb("adab", [128, 6 * KC], F32)
        g1T = sb("g1T_s", [128, KC], F32)
        sc1 = sb("sc1", [128, KC, 3], F32)
        identb = sb("identb", [128, 128], BF16)
        cw = sb("cw", [128, NZ, 3], F32)
        cb = sb("cb", [128, NZ], F32)
        S.dma(lambda e: e.dma_start(out=condT[:], in_=g.condT[:]), writes=["condT"])
        S.dma(lambda e: e.dma_start(out=adab[:], in_=g.ada_bT[:]), writes=["adab"])
        S.dma(lambda e: e.dma_start(out=g1T[:], in_=g.g1T[:]), writes=["g1T"])
        S.dma(lambda e: e.dma_start(out=identb[:], in_=g.ident_bf[:]), writes=["identb"])
        S.dma(lambda e: e.dma_start(out=cw[:], in_=g.conv_wT[:]), writes=["cw"])
        S.dma(lambda e: e.dma_start(out=cb[:], in_=g.conv_bT[:]), writes=["cb"])
        S.op("act", lambda e: e.activation(out=condT[:], in_=condT[:], func=AF.Silu), reads=["condT"], writes=["condT"])
        with contextlib.ExitStack() as stA:
            aw = [stA.enter_context(nc.sbuf_tensor("aw%d" % i, [128, KC, 512], F32)) for i in range(2)]
            mrow = stA.enter_context(nc.sbuf_tensor("mrow", [3, 6 * D], F32))
            S.dma(lambda e: e.dma_start(out=mrow[:], in_=g.ada_brow[0, :].partition_broadcast(3)), writes=["mrow"])
            ada_v = g.ada_w.rearrange("(kc p) f -> p kc f", p=128)
            for sl in range(24):
                p = sl % 2
                S.dma(lambda e, p=p, sl=sl: e.dma_start(out=aw[p][:], in_=ada_v[:, :, sl * 512:(sl + 1) * 512]),
                      writes=["aw%d" % p])
                for kc in range(KC):
                    S.op("pe", lambda e, p=p, kc=kc: e.matmul(pbf_row[0:3, 0:512], lhsT=condT[:, kc, :], rhs=aw[p][:, kc, :],
                                                              start=(kc == 0), stop=(kc == KC - 1)), reads=["aw%d" % p, "condT"], writes=["prow"])
                S.op("dve", lambda e, sl=sl: e.tensor_tensor(out=mrow[:, sl * 512:(sl + 1) * 512], in0=pbf_row[0:3, 0:512], in1=mrow[:, sl * 512:(sl + 1) * 512], op=ALU.add),
                     reads=["prow", "mrow"], writes=["mrow"])
                for j in range(4):
                    ch = sl * 4 + j
                    pb = ch % 6
                    for kc in range(KC):
                        S.op("pe", lambda e, p=p, j=j, kc=kc, pb=pb: e.matmul(
                            ps[pb][:, 0:3], lhsT=aw[p][:, kc, j * 128:(j + 1) * 128], rhs=condT[:, kc, :],
                            start=(kc == 0), stop=(kc == KC - 1)),
                            reads=["aw%d" % p, "condT"], writes=["ps%d" % pb])
                    S.op("dve", lambda e, ch=ch, pb=pb: e.tensor_scalar(
                        out=modT[:, ch, :], in0=ps[pb][:, 0:3], scalar1=adab[:, ch:ch + 1], scalar2=None, op0=ALU.add),
                        reads=["ps%d" % pb, "adab"], writes=["modT"])
            S.dma(lambda e: e.dma_start(out=g.mod_row[:], in_=mrow[:]), reads=["mrow"], writes=["modrow"])
        S.barrier()
        if stage == "dbgA":
            S.dma(lambda e: e.dma_start(out=g.dbg_mod[:], in_=modT[:]), reads=["modT"])
        for n in range(3):
            S.op("dve", lambda e, n=n: e.scalar_tensor_tensor(
                out=sc1[:, :, n], in0=modT[:, KC:2 * KC, n], scalar=1.0, in1=g1T[:], op0=ALU.add, op1=ALU.mult),
                reads=["modT", "g1T"], writes=["sc1"])
        if stage != "dbgA":
            hT = sb("hT", [128, KC, TB], BF16)
            xt = [sb("xt%d" % i, [128, D], F32) for i in range(2)]
            xn = [sb("xn%d" % i, [128, D], BF16) for i in range(2)]
            junk = sb("junk", [128, D], F32)
            ssq = [sb("ssq%d" % i, [128, 1], F32) for i in range(2)]
            wsl = [sb("wsl%d" % i, [128, KC, 256], BF16) for i in range(2)]
            zraw = [sb("zraw%d" % i, [128, TB], F32) for i in range(2)]
            zc = [sb("zc%d" % i, [128, TB], F32) for i in range(2)]
            inw_v = g.in_w.rearrange("(kc p) f -> p kc f", p=128)
            low_v = g.lora_w.rearrange("(kc p) f -> p kc f", p=128)
            it = 0
            for b in range(BPC):
                for t in range(TB // 128):
                    p = it % 2
                    it += 1
                    if t < CTX // 128:
                        src = g.ctx[b, t * 128:(t + 1) * 128, :]
                        n = 2
                    else:
                        src = g.x[b, (t - 2) * 128:(t - 1) * 128, :]
                        n = b
                    X, XN, SS = "xt%d" % p, "xn%d" % p, "ssq%d" % p
                    S.dma(lambda e, p=p, src=src: e.dma_start(out=xt[p][:], in_=src), writes=[X])
                    S.op("act", lambda e, p=p: e.activation(out=junk[:], in_=xt[p][:], func=AF.Square), reads=[X], writes=["junk"])
                    S.op("dve", lambda e, p=p: e.tensor_reduce(out=ssq[p][:], in_=junk[:], axis=AX.X, op=ALU.add), reads=["junk"], writes=[SS])
                    S.op("dve", lambda e, p=p: e.tensor_scalar(out=ssq[p][:], in0=ssq[p][:], scalar1=1.0 / D, scalar2=EPS,
                                                               op0=ALU.mult, op1=ALU.add), reads=[SS], writes=[SS])
                    S.op("act", lambda e, p=p: e.sqrt(out=ssq[p][:], in_=ssq[p][:]), reads=[SS], writes=[SS])
                    S.op("dve", lambda e, p=p: e.reciprocal(out=ssq[p][:], in_=ssq[p][:]), reads=[SS], writes=[SS])
                    S.op("dve", lambda e, p=p: e.tensor_scalar(out=xn[p][:], in0=xt[p][:], scalar1=ssq[p][:], scalar2=None,
                                                               op0=ALU.mult), reads=[X, SS], writes=[XN])
                    for kc in range(KC):
                        q = 0
                        S.op("pe", lambda e, p=p, kc=kc, q=q: e.transpose(
                            pbf[q][:, (kc % 8) * 128:(kc % 8 + 1) * 128], xn[p][:, kc * 128:(kc + 1) * 128], identb[:]),
                            reads=[XN, "identb"], writes=["pbf%d" % q])
                        if kc % 8 == 7:
                            for k2 in range(kc - 7, kc + 1):
                                S.op("act", lambda e, k2=k2, q=q, n=n, t=t: e.activation(
                                    out=hT[:, k2, t * 128:(t + 1) * 128], in_=pbf[q][:, (k2 % 8) * 128:(k2 % 8 + 1) * 128],
                                    func=AF.Identity, scale=sc1[:, k2, n:n + 1], bias=modT[:, k2, n:n + 1]),
                                    reads=["pbf%d" % q, "sc1", "modT"], writes=["hT"])
                S.flush()
                nsl = PW // 256 + 2
                for sl in range(nsl):
                    if sl % 8 == 0 and sl > 0:
                        S.flush()
                    p = sl % 2
                    if sl < PW // 256:
                        srcw = inw_v[:, :, sl * 256:(sl + 1) * 256]
                        ncol = 256
                    elif sl == PW // 256:
                        srcw = low_v[:, :, 0:256]
                        ncol = 256
                    else:
                        srcw = low_v[:, :, 256:384]
                        ncol = 128
                    S.dma(lambda e, p=p, srcw=srcw, ncol=ncol: e.dma_start(out=wsl[p][:, :, 0:ncol], in_=srcw),
                          writes=["wsl%d" % p], q="pool")
                    for j in range(ncol // 128):
                        ch = sl * 2 + j
                        zp = ch % 2
                        Z, ZC = "zraw%d" % zp, "zc%d" % zp
                        toks = [(0, 256)] + [(256 + 512 * i, 512) for i in range(4)]
                        for ti, (t0, tn) in enumerate(toks):
                            pb = (ch * 5 + ti) % 6
                            for kc in range(KC):
                                S.op("pe", lambda e, p=p, j=j, kc=kc, pb=pb, t0=t0, tn=tn: e.matmul(
                                    ps[pb][:, 0:tn], lhsT=wsl[p][:, kc, j * 128:(j + 1) * 128], rhs=hT[:, kc, t0:t0 + tn],
                                    start=(kc == 0), stop=(kc == KC - 1)),
                                    reads=["wsl%d" % p, "hT"], writes=["ps%d" % pb])
                            if ch < NZ:
                                S.op("act", lambda e, zp=zp, pb=pb, t0=t0, tn=tn: e.copy(out=zraw[zp][:, t0:t0 + tn], in_=ps[pb][:, 0:tn]),
                                     reads=["ps%d" % pb], writes=[Z])
                            else:
                                fn = [AF.Tanh, AF.Identity, AF.Sigmoid][ch - NZ]
                                S.op("act", lambda e, zp=zp, pb=pb, t0=t0, tn=tn, fn=fn: e.activation(
                                    out=zc[zp][:, t0:t0 + tn], in_=ps[pb][:, 0:tn], func=fn),
                                    reads=["ps%d" % pb], writes=[ZC])
                        if ch < NZ:
                            S.op("dve", lambda e, zp=zp, ch=ch: e.tensor_scalar(
                                out=zc[zp][:], in0=zraw[zp][:], scalar1=cw[:, ch, 1:2], scalar2=cb[:, ch:ch + 1],
                                op0=ALU.mult, op1=ALU.add), reads=[Z, "cw", "cb"], writes=[ZC])
                            for (s0, s1) in ((0, CTX), (CTX, TB)):
                                S.op("dve", lambda e, zp=zp, ch=ch, s0=s0, s1=s1: e.scalar_tensor_tensor(
                                    out=zc[zp][:, s0 + 1:s1], in0=zraw[zp][:, s0:s1 - 1], scalar=cw[:, ch, 0:1],
                                    in1=zc[zp][:, s0 + 1:s1], op0=ALU.mult, op1=ALU.add), reads=[Z, ZC, "cw"], writes=[ZC])
                                S.op("dve", lambda e, zp=zp, ch=ch, s0=s0, s1=s1: e.scalar_tensor_tensor(
                                    out=zc[zp][:, s0:s1 - 1], in0=zraw[zp][:, s0 + 1:s1], scalar=cw[:, ch, 2:3],
                                    in1=zc[zp][:, s0:s1 - 1], op0=ALU.mult, op1=ALU.add), reads=[Z, ZC, "cw"], writes=[ZC])
                            dst = g.zT[b, ch]
                        else:
                            dst = g.loraT[b, ch - NZ]
                        S.dma(lambda e, zp=zp, dst=dst: e.dma_start(out=dst, in_=zc[zp][:]), reads=[ZC], writes=["scr"])
        S.flush()


def copy_dram(nc, g, pairs):
    with contextlib.ExitStack() as st:
        S = g.S
        S.barrier()
        bufs = [st.enter_context(nc.sbuf_tensor("cpb%d_%d" % (i, id(pairs) % 100000), [128, TB], F32)) for i in range(2)]
        it = 0
        for dst, src in pairs:
            for i in range(dst.shape[0]):
                for j in range(dst.shape[1]):
                    p = it % 2
                    it += 1
                    S.dma(lambda e, p=p, i=i, j=j, src=src: e.dma_start(out=bufs[p][:], in_=src[i, j]), writes=["b%d" % p])
                    S.dma(lambda e, p=p, i=i, j=j, dst=dst: e.dma_start(out=dst[i, j], in_=bufs[p][:]), reads=["b%d" % p])
        S.flush()


RW_NB, RW_NHP = BPC, 8
HY_NB = BPC
F_NEXP, F_NBLK = 32, 4
F_DBG = False


def build_program(stage="final"):
    nc = bass.Bass("TRN2", target_bir_lowering=False)
    g = declare_io(nc, stage)
    gstack = contextlib.ExitStack()
    g.S = Sched(nc, gstack, ndma=48)
    declare_rw(nc, g, stage)
    declare_hy(nc, g, stage)
    if stage in ("final", "simF"):
        declare_F(nc, g, stage)
    if stage not in ("simRW", "simHY", "simF"):
        phase_ABC(nc, g, stage)
    if stage in ("dbgHY", "simHY"):
        phase_HY(nc, g, stage, nb=HY_NB)
    if stage in ("dbgRW", "simRW"):
        phase_RW(nc, g, stage, nb=RW_NB, nhp=RW_NHP)
    if stage in ("final", "dbgMIX"):
        phase_RW(nc, g, stage)
        phase_HY(nc, g, stage)
    if stage == "final":
        phase_F(nc, g, stage, nexp=F_NEXP, nblk=F_NBLK)
    if stage == "simF":
        phase_F(nc, g, stage, nexp=F_NEXP, nblk=F_NBLK)
    if stage == "dbgC":
        copy_dram(nc, g, [(g.dbg_z, g.zT), (g.dbg_lora, g.loraT)])
    g.S.flush(final=True)
    gstack.close()
    return nc


def make_in_maps(inputs, cores=range(NCORES)):
    f = lambda k: np.asarray(inputs[k], np.float32)
    x, ctx, c, c_ctx = f("x"), f("ctx"), f("c"), f("c_ctx")
    lora_w = np.ascontiguousarray(np.concatenate(
        [f("rw_w1")[0, 0], f("rw_w1")[0, 1], f("rw_a1")[0, 0], f("rw_a1")[0, 1], f("rw_g1")[0]], axis=1))
    shared = {
        "ada_w": np.ascontiguousarray(f("ada_w")[0]),
        "ada_bT": _fm(f("ada_b")[0], 6 * KC),
        "g1T": _fm(f("norm1_g")[0], KC),
        "g2T": _fm(f("norm2_g")[0], KC),
        "in_w": np.ascontiguousarray(f("in_w")[0]),
        "lora_w": lora_w,
        "conv_wT": np.ascontiguousarray(f("conv_w")[0].reshape(3, NZ, 128).transpose(2, 1, 0)),
        "conv_bT": _fm(f("conv_b")[0], NZ),
        "final_g": np.ascontiguousarray(f("final_g").reshape(1, D)),
        "ident_bf": np.eye(128, dtype=np.float32).astype(ml_dtypes.bfloat16),
        "ident_f": np.eye(128, dtype=np.float32),
    }
    w2, a2 = f("rw_w2")[0], f("rw_a2")[0]
    w2pad = np.zeros((2, 128, 1024), np.float32)
    a2pad = np.zeros((2, 128, 1024), np.float32)
    for n in range(2):
        w2pad[n, n * 64:(n + 1) * 64] = w2[n]
        a2pad[n, n * 64:(n + 1) * 64] = a2[n]
    rwvec = np.stack([_fm(f("rw_w0")[0, 0], 8), _fm(f("rw_w0")[0, 1], 8), _fm(f("rw_a0")[0, 0], 8), _fm(f("rw_a0")[0, 1], 8),
                      _fm(f("rw_kk")[0], 8), _fm(f("rw_ka")[0], 8), _fm(f("rw_rk")[0].reshape(-1), 8),
                      _fm(f("rw_lnx_g")[0], 8), _fm(f("rw_lnx_b")[0], 8)], axis=1)
    shared.update({"w2pad": w2pad, "a2pad": a2pad, "rw_g2": np.ascontiguousarray(f("rw_g2")[0]),
                   "rwvec": np.ascontiguousarray(rwvec)})
    shared.update(rw_consts())
    shared.update(hy_consts())
    shared.update({"ada_brow": np.ascontiguousarray(f("ada_b")[0].reshape(1, -1)), "out_w": np.ascontiguousarray(f("out_w")[0]),
                   "g2row": np.ascontiguousarray(f("norm2_g")[0].reshape(1, D)), "router_w": np.ascontiguousarray(f("router_w")[0]),
                   "router_b": np.ascontiguousarray(f("router_b")[0].reshape(1, NE))})
    pcol = np.arange(128, dtype=np.float32)[:, None]
    tri = (np.arange(128)[:, None] < np.arange(128)[None, :]).astype(np.float32)
    base4 = 2.0 * (np.arange(KC, dtype=np.float32)[None, None, :] * 128 + pcol[:, :, None]) + np.arange(2, dtype=np.float32)[None, :, None]
    shared.update({"sp_tri": tri, "sp_ones": np.ones((128, 128), np.float32),
                   "sp_sidx": np.ascontiguousarray(np.broadcast_to(np.arange(NSB, dtype=np.float32)[None, :], (128, NSB))),
                   "sp_base4": np.ascontiguousarray(base4.astype(np.float32)), "sp_piota": np.ascontiguousarray(pcol)})
    if "ex_w_gate" in inputs:
        bgu = np.stack([f("ex_b_gate")[0].reshape(NE, KC, 128).transpose(0, 2, 1), f("ex_b_up")[0].reshape(NE, KC, 128).transpose(0, 2, 1)], axis=2)
        shared.update({"ex_w_gate": f("ex_w_gate")[0], "ex_w_up": f("ex_w_up")[0], "ex_w_down": f("ex_w_down")[0],
                       "ex_bgu": np.ascontiguousarray(bgu), "ex_b_down": np.ascontiguousarray(f("ex_b_down")[0])})
    shared.update({"hy_w1": np.ascontiguousarray(f("hy_w1")[0]), "hy_w2": np.ascontiguousarray(f("hy_w2")[0]),
                   "hy_w3": np.ascontiguousarray(f("hy_w3")[0]), "hy_w4": np.ascontiguousarray(f("hy_w4")[0]),
                   "hy_fb": np.ascontiguousarray(np.stack([f("hy_freq")[0], f("hy_b1")[0], f("hy_b2")[0], f("hy_b3")[0]], axis=1)),
                   "hy_vec": np.ascontiguousarray(np.stack([_fm(f("hy_bias")[0], 8), _fm(f("hy_norm_g")[0], 8)], axis=1))})
    maps = []
    for cid in cores:
        b0 = cid * BPC
        cond = np.stack([c[b0], c[b0 + 1], c_ctx], axis=0)
        condT = np.ascontiguousarray(cond.reshape(3, KC, 128).transpose(2, 1, 0))
        m = dict(shared)
        m["x"] = np.ascontiguousarray(x[b0:b0 + BPC])
        m["ctx"] = np.ascontiguousarray(ctx[b0:b0 + BPC])
        m["condT"] = condT
        maps.append(m)
    return maps


def kernel(**inputs):
    nc = build_program()
    in_maps = make_in_maps(inputs)
    res = run_bass_kernel_spmd(nc, in_maps, core_ids=list(range(NCORES)))
    return np.concatenate([r["out"] for r in res.results], axis=0)


NCH = TB // 64
C0 = float(np.exp(-0.5))


def rw_consts():
    j = np.arange(128)[:, None]
    t = np.arange(128)[None, :]
    m = {}
    m["rw_mask0"] = np.concatenate([(j < t), (j <= t)], axis=1).astype(np.float32)
    m["rw_mask1"] = np.concatenate([(j > t), (j >= t)], axis=1).astype(np.float32)
    m["rw_maskT0"] = (t < j).astype(np.float32)
    m["rw_maskT1"] = (t > j).astype(np.float32)
    blk = np.zeros((128, 128), np.float32)
    blk[:64, :64] = 1.0
    blk[64:, 64:] = 1.0
    m["rw_blk"] = blk
    rm = np.ones((128, TB), np.float32)
    rm[:, ::64] = 0.0
    m["rw_rm"] = rm
    return m


def declare_rw(nc, g, stage):
    dt = nc.dram_tensor
    inp = lambda name, shape, dtype=F32: dt(name, list(shape), dtype, kind="ExternalInput").ap()
    g.w2pad = inp("w2pad", [2, 128, 1024])
    g.a2pad = inp("a2pad", [2, 128, 1024])
    g.rw_g2 = inp("rw_g2", [128, 1024])
    g.rwvec = inp("rwvec", [128, 9, 8])
    for k in ("rw_mask0", "rw_mask1"):
        setattr(g, k, inp(k, [128, 256]))
    for k in ("rw_maskT0", "rw_maskT1", "rw_blk"):
        setattr(g, k, inp(k, [128, 128]))
    g.rw_rm = inp("rw_rm", [128, TB])
    g.mixT = (dt("mixT_scr", [BPC, KC, 128, SEQ], BF16, kind="ExternalOutput") if stage == "dbgMIX" else dt("mixT_scr", [BPC, KC, 128, SEQ], BF16)).ap()
    if stage in ("dbgRW", "simRW"):
        g.dbg_rwo = dt("dbg_rwo", [BPC, 8, 128, SEQ], BF16, kind="ExternalOutput").ap()


def phase_RW(nc, g, stage, nb=BPC, nhp=8):
    with contextlib.ExitStack() as st:
        S = g.S
        S.barrier()
        sb = lambda name, shape, dtype: st.enter_context(nc.sbuf_tensor("rws_" + name, shape, dtype))
        PB = st.enter_context(nc.psum_tensor("rpbf", [128, 1024], BF16))
        P = [None] + [st.enter_context(nc.psum_tensor("rps%d" % i, [128, 512], F32)) for i in range(1, 8)]
        ROT = [1, 2, 3, 4, 7]
        rot = [0]
        mask = [sb("mask%d" % n, [128, 256], BF16) for n in range(2)]
        maskT = [sb("maskT%d" % n, [128, 128], BF16) for n in range(2)]
        blk = sb("blk", [128, 128], BF16)
        identb = sb("identb", [128, 128], BF16)
        identf = sb("identf", [128, 128], F32)
        rm = sb("rm", [128, TB], BF16)
        w2b = [sb("w2b%d" % n, [128, 128], BF16) for n in range(2)]
        a2b = [sb("a2b%d" % n, [128, 128], BF16) for n in range(2)]
        g2b = sb("g2b", [128, 128], BF16)
        vec = sb("rwvec_s", [128, 9, 8], F32)
        for n in range(2):
            S.dma(lambda e, n=n: e.dma_start(out=mask[n][:], in_=getattr(g, "rw_mask%d" % n)[:]), writes=["mask%d" % n], q="pool")
            S.dma(lambda e, n=n: e.dma_start(out=maskT[n][:], in_=getattr(g, "rw_maskT%d" % n)[:]), writes=["maskT%d" % n], q="pool")
        S.dma(lambda e: e.dma_start(out=blk[:], in_=g.rw_blk[:]), writes=["blk"], q="pool")
        S.dma(lambda e: e.dma_start(out=identb[:], in_=g.ident_bf[:]), writes=["identb"])
        S.dma(lambda e: e.dma_start(out=identf[:], in_=g.ident_f[:]), writes=["identf"])
        S.dma(lambda e: e.dma_start(out=rm[:], in_=g.rw_rm[:]), writes=["rm"], q="pool")
        S.dma(lambda e: e.dma_start(out=vec[:], in_=g.rwvec[:]), writes=["vec"])
        L = [sb("L%d" % i, [128, TB], BF16) for i in range(3)]
        R_, K_, V_ = sb("R_", [128, TB], F32), sb("K_", [128, TB], F32), sb("V_", [128, TB], F32)
        KKN = sb("KKN", [128, TB], F32)
        SG, AN, CUM = sb("SG", [128, TB], F32), sb("AN", [128, TB], F32), sb("CUM", [128, TB], F32)
        E1, E2 = sb("E1", [128, TB], F32), sb("E2", [128, TB], F32)
        KD = SG
        TBF = sb("TBF", [128, TB], BF16)
        TOT = sb("TOT", [128, NCH], F32)
        GAM = [sb("GAM%d" % n, [128, NCH], F32) for n in range(2)]
        arT = [sb("arT%d" % n, [128, NCH, 256], BF16) for n in range(2)]
        bT = [sb("bT%d" % n, [128, NCH, 128], BF16) for n in range(2)]
        kT = [sb("kT%d" % n, [128, NCH, 128], BF16) for n in range(2)]
        vT = sb("vT", [128, NCH, 128], BF16)
        yacc = sb("yacc", [128, 32, 64], F32)
        s1, s2 = sb("s1", [128, 32], F32), sb("s2", [128, 32], F32)
        TM = [sb("TM%d" % n, [128, 512], BF16) for n in range(2)]
        GB = [sb("GB%d" % n, [128, 256], BF16) for n in range(2)]
        GK = [sb("GK%d" % n, [128, 256], BF16) for n in range(2)]
        PW_ = [[sb("PW%d_%d" % (n, i), [128, 256], BF16) for i in range(2)] for n in range(2)]
        XX = [[sb("XX%d_%d" % (n, i), [128, 256], BF16) for i in range(2)] for n in range(2)]
        MT = [sb("MT%d" % n, [128, 128], BF16) for n in range(2)]
        RH = [sb("RH%d" % n, [128, 128], BF16) for n in range(2)]
        HH = [[sb("HH%d_%d" % (n, i), [128, 128], BF16) for i in range(2)] for n in range(2)]
        for n in range(2):
            for tname, tt in (("arT%d" % n, arT[n]), ("bT%d" % n, bT[n]), ("kT%d" % n, kT[n])):
                S.op("pool", lambda e, tt=tt: e.memset(tt[:], 0.0), writes=[tname])
        S.op("pool", lambda e: e.memset(vT[:], 0.0), writes=["vT"])
        ynbd = kT[0]

        toks = [(0, 256)] + [(256 + 512 * i, 512) for i in range(4)]
        v3 = lambda tile: tile[:].rearrange("p (c t) -> p c t", t=64)

        def bd_write(eng, dst, dst_name, col0, in0, in0_name, in1, in1_name, neg=False):
            for h in range(2):
                rs = slice(h * 64, (h + 1) * 64)
                o = dst[rs, :, col0 + h * 64: col0 + (h + 1) * 64]
                a = in0[rs, :].rearrange("p (c t) -> p c t", t=64)
                b = in1[rs, :].rearrange("p (c t) -> p c t", t=64)
                if neg:
                    S.op(eng, lambda e, o=o, a=a, b=b: e.scalar_tensor_tensor(out=o, in0=a, scalar=-1.0, in1=b, op0=ALU.mult, op1=ALU.mult),
                         reads=[in0_name, in1_name], writes=[dst_name])
                else:
                    S.op(eng, lambda e, o=o, a=a, b=b: e.tensor_tensor(out=o, in0=a, in1=b, op=ALU.mult),
                         reads=[in0_name, in1_name], writes=[dst_name])

        for b in range(nb):
            for i in range(3):
                S.dma(lambda e, i=i, b=b: e.dma_start(out=L[i][:], in_=g.loraT[b, i]), reads=["scr"], writes=["L%d" % i], q="pool")
            for hp in range(nhp):
                S.flush()
                S.dma(lambda e, b=b, hp=hp: e.dma_start(out=R_[:], in_=g.zT[b, 24 + hp]), reads=["scr"], writes=["R_"])
                S.dma(lambda e, b=b, hp=hp: e.dma_start(out=K_[:], in_=g.zT[b, 32 + hp]), reads=["scr"], writes=["K_"])
                S.dma(lambda e, b=b, hp=hp: e.dma_start(out=V_[:], in_=g.zT[b, 40 + hp]), reads=["scr"], writes=["V_"])
                hc = slice(hp * 128, (hp + 1) * 128)
                for n in range(2):
                    S.dma(lambda e, n=n, hc=hc: e.dma_start(out=w2b[n][:], in_=g.w2pad[n, :, hc]), writes=["w2b%d" % n], q="pool")
                    S.dma(lambda e, n=n, hc=hc: e.dma_start(out=a2b[n][:], in_=g.a2pad[n, :, hc]), writes=["a2b%d" % n], q="pool")
                S.dma(lambda e, hc=hc: e.dma_start(out=g2b[:], in_=g.rw_g2[:, hc]), writes=["g2b"], q="pool")
                hc = slice(0, 128)
                S.op("dve", lambda e, hp=hp: e.tensor_scalar(out=KKN[:], in0=K_[:], scalar1=vec[:, 4, hp:hp + 1], scalar2=None, op0=ALU.mult),
                     reads=["K_", "vec"], writes=["KKN"])
                S.op("dve", lambda e: e.tensor_tensor(out=TBF[:], in0=KKN[:], in1=KKN[:], op=ALU.mult), reads=["KKN"], writes=["TBF"])
                for ti, (t0, tn) in enumerate(toks):
                    pb = 1 + ti % 4
                    S.op("pe", lambda e, pb=pb, t0=t0, tn=tn: e.matmul(P[pb][:, 0:tn], lhsT=blk[:], rhs=TBF[:, t0:t0 + tn], start=True, stop=True),
                         reads=["blk", "TBF"], writes=["P%d" % pb])
                    S.op("act", lambda e, pb=pb, t0=t0, tn=tn: e.sqrt(out=E1[:, t0:t0 + tn], in_=P[pb][:, 0:tn]), reads=["P%d" % pb], writes=["E1"])
                S.op("dve", lambda e: e.tensor_scalar(out=E1[:], in0=E1[:], scalar1=1e-12, scalar2=None, op0=ALU.max), reads=["E1"], writes=["E1"])
                S.op("dve", lambda e: e.reciprocal(out=E1[:], in_=E1[:]), reads=["E1"], writes=["E1"])
                S.op("dve", lambda e: e.tensor_tensor(out=KKN[:], in0=KKN[:], in1=E1[:], op=ALU.mult), reads=["KKN", "E1"], writes=["KKN"])
                for h in range(2):
                    rs = slice(h * 64, (h + 1) * 64)
                    S.op("pool", lambda e, rs=rs, h=h: e.tensor_copy(out=vT[rs, :, h * 64:(h + 1) * 64], in_=V_[rs, :].rearrange("p (c t) -> p c t", t=64)),
                         reads=["V_"], writes=["vT"])
                for n in range(2):
                    for ti, (t0, tn) in enumerate(toks):
                        pb = 1 + ti % 4
                        S.op("pe", lambda e, pb=pb, t0=t0, tn=tn, n=n, hc=hc: e.matmul(P[pb][:, 0:tn], lhsT=w2b[n][:, hc], rhs=L[0][:, t0:t0 + tn], start=True, stop=True),
                             reads=["w2b%d" % n, "L0"], writes=["P%d" % pb])
                        S.op("act", lambda e, pb=pb, t0=t0, tn=tn, n=n, hp=hp: e.activation(out=SG[:, t0:t0 + tn], in_=P[pb][:, 0:tn], func=AF.Sigmoid, bias=vec[:, n, hp:hp + 1]),
                             reads=["P%d" % pb, "vec"], writes=["SG"])
                        pb2 = 5 + ti % 2
                        S.op("pe", lambda e, pb2=pb2, t0=t0, tn=tn, n=n, hc=hc: e.matmul(P[pb2][:, 0:tn], lhsT=a2b[n][:, hc], rhs=L[1][:, t0:t0 + tn], start=True, stop=True),
                             reads=["a2b%d" % n, "L1"], writes=["P%d" % pb2])
                        S.op("act", lambda e, pb2=pb2, t0=t0, tn=tn, n=n, hp=hp: e.activation(out=AN[:, t0:t0 + tn], in_=P[pb2][:, 0:tn], func=AF.Sigmoid, bias=vec[:, 2 + n, hp:hp + 1]),
                             reads=["P%d" % pb2, "vec"], writes=["AN"])
                    S.op("dve", lambda e: e.tensor_tensor_scan(out=CUM[:], data0=rm[:], data1=SG[:], initial=0.0, op0=ALU.mult, op1=ALU.add),
                         reads=["rm", "SG"], writes=["CUM"])
                    if n == 1:
                        S.op("dve", lambda e: e.tensor_copy(out=TOT[:], in_=v3(CUM)[:, :, 63]), reads=["CUM"], writes=["TOT"])
                        S.op("dve", lambda e: e.tensor_tensor(out=v3(CUM), in0=TOT[:].unsqueeze(2).to_broadcast([128, NCH, 64]), in1=v3(CUM), op=ALU.subtract),
                             reads=["TOT", "CUM"], writes=["CUM"])
                        S.op("dve", lambda e: e.tensor_tensor(out=CUM[:], in0=CUM[:], in1=SG[:], op=ALU.add), reads=["CUM", "SG"], writes=["CUM"])
                    S.op("dve", lambda e: e.tensor_tensor(out=E2[:], in0=CUM[:], in1=SG[:], op=ALU.subtract), reads=["CUM", "SG"], writes=["E2"])
                    S.op("act", lambda e: e.activation(out=E1[:], in_=E2[:], func=AF.Exp, scale=-C0), reads=["E2"], writes=["E1"])
                    bd_write("dve", arT[n], "arT%d" % n, 0, KKN, "KKN", E1, "E1", neg=True)
                    S.op("act", lambda e: e.activation(out=E1[:], in_=CUM[:], func=AF.Exp, scale=-C0), reads=["CUM"], writes=["E1"])
                    bd_write("dve", arT[n], "arT%d" % n, 128, R_, "R_", E1, "E1")
                    S.op("dve", lambda e, n=n: e.tensor_copy(out=GAM[n][:], in_=v3(E1)[:, :, 63 if n == 0 else 0]), reads=["E1"], writes=["GAM%d" % n])
                    S.op("dve", lambda e: e.tensor_tensor(out=E2[:], in0=KKN[:], in1=AN[:], op=ALU.mult), reads=["KKN", "AN"], writes=["E2"])
                    S.op("act", lambda e: e.activation(out=E1[:], in_=CUM[:], func=AF.Exp, scale=C0), reads=["CUM"], writes=["E1"])
                    bd_write("dve", bT[n], "bT%d" % n, 0, E2, "E2", E1, "E1")
                    S.op("dve", lambda e, hp=hp: e.tensor_scalar(out=KD[:], in0=AN[:], scalar1=-1.0, scalar2=vec[:, 5, hp:hp + 1], op0=ALU.add, op1=ALU.mult),
                         reads=["AN", "vec", "SG"], writes=["SG"])
                    S.op("dve", lambda e: e.scalar_tensor_tensor(out=KD[:], in0=KD[:], scalar=1.0, in1=K_[:], op0=ALU.add, op1=ALU.mult),
                         reads=["SG", "K_"], writes=["SG"])
                    bd_write("dve", kT[n], "kT%d" % n, 0, KD, "SG", E1, "E1")
                    if n == 0:
                        S.op("dve", lambda e, hp=hp: e.scalar_tensor_tensor(out=TBF[:], in0=R_[:], scalar=vec[:, 6, hp:hp + 1], in1=KD[:], op0=ALU.mult, op1=ALU.mult),
                             reads=["R_", "vec", "SG"], writes=["TBF"])
                    else:
                        S.op("dve", lambda e, hp=hp: e.scalar_tensor_tensor(out=E2[:], in0=R_[:], scalar=vec[:, 6, hp:hp + 1], in1=KD[:], op0=ALU.mult, op1=ALU.mult),
                             reads=["R_", "vec", "SG"], writes=["E2"])
                        S.op("dve", lambda e: e.tensor_tensor(out=TBF[:], in0=TBF[:], in1=E2[:], op=ALU.add), reads=["TBF", "E2"], writes=["TBF"])
                S.op("pool", lambda e: e.memset(yacc[:], 0.0), writes=["yacc"])
                for n in range(2):
                    S.op("pool", lambda e, n=n: e.memset(HH[n][0][:], 0.0), writes=["HH%d_0" % n])
                order = [list(range(NCH)), [3, 2, 1, 0] + list(range(NCH - 1, 3, -1))]
                for s in range(NCH):
                    for n in range(2):
                        c = order[n][s]
                        lat = c >= 4
                        nm = lambda x: "%s%d" % (x, n)
                        Hold, Hnew = HH[n][s % 2], HH[n][(s + 1) % 2]
                        Hold_n, Hnew_n = "HH%d_%d" % (n, s % 2), "HH%d_%d" % (n, (s + 1) % 2)
                        aTc, rTc = arT[n][:, c, 0:128], arT[n][:, c, 128:256]
                        for qi, (src, sname) in enumerate(((aTc, nm("arT")), (bT[n][:, c, :], nm("bT")), (kT[n][:, c, :], nm("kT")), (vT[:, c, :], "vT"))):
                            S.op("pe", lambda e, qi=qi, src=src: e.transpose(PB[:, qi * 128:(qi + 1) * 128], src, identb[:]),
                                 reads=[sname, "identb"], writes=["PB"])
                        S.op("act", lambda e, n=n: e.copy(out=TM[n][:], in_=PB[:, 0:512]), reads=["PB"], writes=[nm("TM")])
                        a_, b_, k_, v_ = (TM[n][:, i * 128:(i + 1) * 128] for i in range(4))
                        def nbk():
                            rot[0] = (rot[0] + 1) % len(ROT)
                            return ROT[rot[0]]
                        pw = PW_[n]
                        xx = XX[n]
                        k1 = nbk()
                        S.op("pe", lambda e, n=n, c=c, k1=k1: e.matmul(P[k1][:, 0:256], lhsT=bT[n][:, c, :], rhs=arT[n][:, c, :], start=True, stop=True),
                             reads=[nm("bT"), nm("arT")], writes=["P%d" % k1])
                        S.op("dve", lambda e, n=n, k1=k1: e.tensor_tensor(out=GB[n][:], in0=P[k1][:, 0:256], in1=mask[n][:], op=ALU.mult),
                             reads=["P%d" % k1, nm("mask")], writes=[nm("GB")])
                        k2 = nbk()
                        S.op("pe", lambda e, n=n, c=c, k2=k2: e.matmul(P[k2][:, 0:256], lhsT=kT[n][:, c, :], rhs=arT[n][:, c, :], start=True, stop=True),
                             reads=[nm("kT"), nm("arT")], writes=["P%d" % k2])
                        S.op("dve", lambda e, n=n, k2=k2: e.tensor_tensor(out=GK[n][:], in0=P[k2][:, 0:256], in1=mask[n][:], op=ALU.mult),
                             reads=["P%d" % k2, nm("mask")], writes=[nm("GK")])
                        k3 = nbk()
                        S.op("pe", lambda e, n=n, c=c, aTc=aTc, k3=k3: e.matmul(P[k3][:, 0:128], lhsT=aTc, rhs=bT[n][:, c, :], start=True, stop=True),
                             reads=[nm("bT"), nm("arT")], writes=["P%d" % k3])
                        S.op("dve", lambda e, n=n, pw=pw, k3=k3: e.tensor_tensor(out=pw[0][:, 0:128], in0=P[k3][:, 0:128], in1=maskT[n][:], op=ALU.mult),
                             reads=["P%d" % k3, nm("maskT")], writes=[nm("PW") + "_0"])
                        S.op("pool", lambda e, n=n, pw=pw: e.tensor_copy(out=pw[0][:, 128:256], in_=GB[n][:, 0:128]),
                             reads=[nm("GB")], writes=[nm("PW") + "_0"])
                        k4 = nbk()
                        S.op("pe", lambda e, n=n, v_=v_, k4=k4: e.matmul(P[k4][:, 0:128], lhsT=GK[n][:, 0:128], rhs=v_, start=True, stop=True),
                             reads=[nm("GK"), nm("TM")], writes=["P%d" % k4])
                        S.op("act", lambda e, xx=xx, k4=k4: e.copy(out=xx[0][:, 128:256], in_=P[k4][:, 0:128]), reads=["P%d" % k4], writes=[nm("XX") + "_0"])
                        S.op("pool", lambda e, xx=xx, a_=a_: e.tensor_copy(out=xx[0][:, 0:128], in_=a_), reads=[nm("TM")], writes=[nm("XX") + "_0"])
                        for i in range(6):
                            pc, pn = pw[i % 2], pw[(i + 1) % 2]
                            pcn, pnn = nm("PW") + "_%d" % (i % 2), nm("PW") + "_%d" % ((i + 1) % 2)
                            xc, xn_ = xx[i % 2], xx[(i + 1) % 2]
                            xcn, xnn = nm("XX") + "_%d" % (i % 2), nm("XX") + "_%d" % ((i + 1) % 2)
                            bk = nbk()
                            S.op("pe", lambda e, pc=pc, xc=xc, bk=bk: e.matmul(P[bk][:, 0:256], lhsT=pc[:, 128:256], rhs=xc[:], start=True, stop=True),
                                 reads=[pcn, xcn], writes=["P%d" % bk])
                            S.op("dve", lambda e, xc=xc, xn_=xn_, bk=bk: e.tensor_tensor(out=xn_[:], in0=P[bk][:, 0:256], in1=xc[:], op=ALU.add),
                                 reads=["P%d" % bk, xcn], writes=[xnn])
                            if i < 5:
                                bq = nbk()
                                S.op("pe", lambda e, pc=pc, bq=bq: e.matmul(P[bq][:, 0:128], lhsT=pc[:, 128:256], rhs=pc[:, 0:128], start=True, stop=True),
                                     reads=[pcn], writes=["P%d" % bq])
                                S.op("pe", lambda e, pc=pc, bq=bq: e.matmul(P[bq][:, 128:256], lhsT=pc[:, 0:128], rhs=pc[:, 128:256], start=True, stop=True),
                                     reads=[pcn], writes=["P%d" % bq])
                                S.op("act", lambda e, pn=pn, bq=bq: e.copy(out=pn[:], in_=P[bq][:, 0:256]), reads=["P%d" % bq], writes=[pnn])
                        X = xx[0]
                        Xn = nm("XX") + "_0"
                        Ah, U0 = X[:, 0:128], X[:, 128:256]
                        k5 = nbk()
                        S.op("pe", lambda e, Ah=Ah, b_=b_, k5=k5: e.matmul(P[k5][:, 0:128], lhsT=Ah, rhs=b_, start=True, stop=True),
                             reads=[Xn, nm("TM")], writes=["P%d" % k5])
                        S.op("dve", lambda e, n=n, k5=k5: e.tensor_tensor(out=MT[n][:], in0=P[k5][:, 0:128], in1=identf[:], op=ALU.add),
                             reads=["P%d" % k5, "identf"], writes=[nm("MT")])
                        if lat:
                            k6 = nbk()
                            S.op("pe", lambda e, Ah=Ah, n=n, k6=k6: e.matmul(P[k6][:, 0:128], lhsT=Ah, rhs=GB[n][:, 128:256], start=True, stop=True),
                                 reads=[Xn, nm("GB")], writes=["P%d" % k6])
                            S.op("dve", lambda e, n=n, rTc=rTc, k6=k6: e.tensor_tensor(out=RH[n][:], in0=P[k6][:, 0:128], in1=rTc, op=ALU.add),
                                 reads=["P%d" % k6, nm("arT")], writes=[nm("RH")])
                            S.op("pe", lambda e, n=n, U0=U0: e.matmul(P[6][:, 0:128], lhsT=GB[n][:, 128:256], rhs=U0, start=True, stop=False),
                                 reads=[nm("GB"), Xn], writes=["P6"])
                            S.op("pe", lambda e, n=n, v_=v_: e.matmul(P[6][:, 0:128], lhsT=GK[n][:, 128:256], rhs=v_, start=False, stop=False),
                                 reads=[nm("GK"), nm("TM")], writes=["P6"])
                            S.op("pe", lambda e, n=n, Hold=Hold: e.matmul(P[6][:, 0:128], lhsT=RH[n][:], rhs=Hold[:], start=False, stop=True),
                                 reads=[nm("RH"), Hold_n], writes=["P6"])
                            for h in range(2):
                                rs = slice(h * 64, (h + 1) * 64)
                                S.op("dve", lambda e, c=c, rs=rs, h=h: e.tensor_tensor(out=yacc[rs, c - 4, :], in0=P[6][rs, h * 64:(h + 1) * 64], in1=yacc[rs, c - 4, :], op=ALU.add),
                                     reads=["P6", "yacc"], writes=["yacc"])
                        S.op("pe", lambda e, b_=b_, U0=U0: e.matmul(P[5][:, 0:128], lhsT=b_, rhs=U0, start=True, stop=False),
                             reads=[nm("TM"), Xn], writes=["P5"])
                        S.op("pe", lambda e, k_=k_, v_=v_: e.matmul(P[5][:, 0:128], lhsT=k_, rhs=v_, start=False, stop=False),
                             reads=[nm("TM")], writes=["P5"])
                        S.op("pe", lambda e, n=n, Hold=Hold: e.matmul(P[5][:, 0:128], lhsT=MT[n][:], rhs=Hold[:], start=False, stop=True),
                             reads=[nm("MT"), Hold_n], writes=["P5"])
                        S.op("act", lambda e, n=n, c=c, Hnew=Hnew: e.activation(out=Hnew[:], in_=P[5][:, 0:128], func=AF.Copy, scale=GAM[n][:, c:c + 1]),
                             reads=["P5", nm("GAM")], writes=[Hnew_n])
                S.op("dve", lambda e: e.tensor_reduce(out=s1[:], in_=yacc[:], axis=AX.X, op=ALU.add), reads=["yacc"], writes=["s1"])
                ysq = E1[:, 0:32 * 64].rearrange("p (c v) -> p c v", v=64)
                S.op("dve", lambda e: e.tensor_tensor(out=ysq, in0=yacc[:], in1=yacc[:], op=ALU.mult), reads=["yacc"], writes=["E1"])
                S.op("dve", lambda e: e.tensor_reduce(out=s2[:], in_=ysq, axis=AX.X, op=ALU.add), reads=["E1"], writes=["s2"])
                S.op("dve", lambda e: e.tensor_scalar(out=s1[:], in0=s1[:], scalar1=1.0 / 64, scalar2=None, op0=ALU.mult), reads=["s1"], writes=["s1"])
                S.op("dve", lambda e: e.tensor_tensor(out=TOT[:, 0:32], in0=s1[:], in1=s1[:], op=ALU.mult), reads=["s1"], writes=["TOT"])
                S.op("dve", lambda e: e.scalar_tensor_tensor(out=s2[:], in0=s2[:], scalar=1.0 / 64, in1=TOT[:, 0:32], op0=ALU.mult, op1=ALU.subtract),
                     reads=["s2", "TOT"], writes=["s2"])
                S.op("dve", lambda e: e.tensor_scalar(out=s2[:], in0=s2[:], scalar1=64e-5, scalar2=None, op0=ALU.add), reads=["s2"], writes=["s2"])
                S.op("act", lambda e: e.sqrt(out=s2[:], in_=s2[:]), reads=["s2"], writes=["s2"])
                S.op("dve", lambda e: e.reciprocal(out=s2[:], in_=s2[:]), reads=["s2"], writes=["s2"])
                S.op("dve", lambda e: e.tensor_tensor(out=ysq, in0=yacc[:], in1=s1[:].unsqueeze(2).to_broadcast([128, 32, 64]), op=ALU.subtract),
                     reads=["yacc", "s1"], writes=["E1"])
                for h in range(2):
                    rs = slice(h * 64, (h + 1) * 64)
                    cs = slice(h * 64, (h + 1) * 64)
                    S.op("dve", lambda e, rs=rs, cs=cs: e.tensor_tensor(out=ynbd[rs, 0:32, cs], in0=ysq[rs, :, :], in1=s2[rs, :].unsqueeze(2).to_broadcast([64, 32, 64]), op=ALU.mult),
                         reads=["E1", "s2"], writes=["kT0"])
                for c in range(32):
                    q = c % 8
                    S.op("pe", lambda e, c=c, q=q: e.transpose(PB[:, q * 128:(q + 1) * 128], ynbd[:, c, :], identb[:]), reads=["kT0", "identb"], writes=["PB"])
                    for h in range(2):
                        rs = slice(h * 64, (h + 1) * 64)
                        S.op("act", lambda e, c=c, q=q, rs=rs, h=h, hp=hp: e.activation(
                            out=E2[rs, c * 64:(c + 1) * 64], in_=PB[rs, q * 128 + h * 64:q * 128 + (h + 1) * 64], func=AF.Identity,
                            scale=vec[rs, 7, hp:hp + 1], bias=vec[rs, 8, hp:hp + 1]), reads=["PB", "vec"], writes=["E2"])
                BON = SG
                for ti in range(4):
                    pb = 1 + ti % 4
                    t0 = CTX + 512 * ti
                    S.op("pe", lambda e, pb=pb, t0=t0: e.matmul(P[pb][:, 0:512], lhsT=blk[:], rhs=TBF[:, t0:t0 + 512], start=True, stop=True),
                         reads=["blk", "TBF"], writes=["P%d" % pb])
                    S.op("dve", lambda e, pb=pb, t0=t0, ti=ti: e.tensor_tensor(out=BON[:, ti * 512:(ti + 1) * 512], in0=P[pb][:, 0:512], in1=V_[:, t0:t0 + 512], op=ALU.mult),
                         reads=["P%d" % pb, "V_"], writes=["SG"])
                S.op("dve", lambda e: e.tensor_tensor(out=E2[:, 0:SEQ], in0=E2[:, 0:SEQ], in1=BON[:, 0:SEQ], op=ALU.add), reads=["E2", "SG"], writes=["E2"])
                mixo = TBF
                for ti in range(4):
                    pb = 1 + ti % 4
                    t0 = CTX + 512 * ti
                    S.op("pe", lambda e, pb=pb, t0=t0: e.matmul(P[pb][:, 0:512], lhsT=g2b[:], rhs=L[2][:, t0:t0 + 512], start=True, stop=True),
                         reads=["g2b", "L2"], writes=["P%d" % pb])
                    S.op("dve", lambda e, pb=pb, ti=ti: e.tensor_tensor(out=mixo[:, ti * 512:(ti + 1) * 512], in0=P[pb][:, 0:512], in1=E2[:, ti * 512:(ti + 1) * 512], op=ALU.mult),
                         reads=["P%d" % pb, "E2"], writes=["TBF"])
                S.dma(lambda e, b=b, hp=hp: e.dma_start(out=g.mixT[b, 8 + hp], in_=mixo[:, 0:SEQ]), reads=["TBF"], writes=["mixscr"])
                if stage in ("dbgRW", "simRW"):
                    S.dma(lambda e, b=b, hp=hp: e.dma_start(out=g.dbg_rwo[b, hp], in_=mixo[:, 0:SEQ]), reads=["TBF"])
        S.flush()


NF = 4096
TWO_PI = float(2 * np.pi)


def hy_consts():
    L = SEQ
    t = np.linspace(0.0, 1.0, L, dtype=np.float32)[:, None]
    ang = (2.0 * np.pi / L) * np.arange(L, dtype=np.float32)[:, None]
    bands = np.linspace(1e-4, 15, 16, dtype=np.float32)[None, :]
    feats = np.concatenate([t, np.cos(bands * ang), -np.sin(bands * ang)], axis=-1).astype(np.float32)
    deltas = np.abs(np.linspace(np.log(1e-2) / 1.5, np.log(1e-2) / 0.3, 1024, dtype=np.float32))
    win = np.exp(-t * np.tile(deltas, 2)).astype(np.float32)
    tt = np.arange(L, dtype=np.float64)[:, None]
    ff = np.arange(L, dtype=np.float64)[None, :]
    th = 2 * np.pi * ((tt * ff) % NF) / NF
    C = np.cos(th)
    Sn = np.sin(th)
    wt = np.full((L,), 2.0 / NF)
    wt[0] = 1.0 / NF
    bf = ml_dtypes.bfloat16
    tile = lambda M: np.ascontiguousarray(M.reshape(16, 128, 16, 128).transpose(2, 1, 0, 3)).astype(np.float32).astype(bf)
    m = {
        "hy_featsT": np.ascontiguousarray(feats.T),
        "hy_win": np.ascontiguousarray(win.reshape(16, 128, 2048).transpose(1, 0, 2)),
        "hy_Cfw": tile(C), "hy_Sfw": tile(Sn),
        "hy_Cinv": (C * wt[:, None]).astype(np.float32).astype(bf),
        "hy_Sinv": (Sn * wt[:, None]).astype(np.float32).astype(bf),
        "hy_alt": np.ascontiguousarray(((-1.0) ** np.arange(L)).reshape(16, 128).T.astype(np.float32)).astype(bf),
        "hy_altinv": (((-1.0) ** np.arange(L)) / NF).reshape(1, L).astype(np.float32).astype(bf),
    }
    return m


def declare_hy(nc, g, stage):
    dt = nc.dram_tensor
    inp = lambda name, shape, dtype=F32: dt(name, list(shape), dtype, kind="ExternalInput").ap()
    g.hy_featsT = inp("hy_featsT", [33, SEQ])
    g.hy_win = inp("hy_win", [128, 16, 2048])
    g.hy_Cfw = inp("hy_Cfw", [16, 128, 16, 128], BF16)
    g.hy_Sfw = inp("hy_Sfw", [16, 128, 16, 128], BF16)
    g.hy_Cinv = inp("hy_Cinv", [SEQ, SEQ], BF16)
    g.hy_Sinv = inp("hy_Sinv", [SEQ, SEQ], BF16)
    g.hy_alt = inp("hy_alt", [128, 16], BF16)
    g.hy_altinv = inp("hy_altinv", [1, SEQ], BF16)
    g.hy_w1 = inp("hy_w1", [33, 64])
    g.hy_w2 = inp("hy_w2", [64, 64])
    g.hy_w3 = inp("hy_w3", [64, 64])
    g.hy_w4 = inp("hy_w4", [64, 2048])
    g.hy_fb = inp("hy_fb", [64, 4])
    g.hy_vec = inp("hy_vec", [128, 2, 8])
    g.specT = dt("hy_spec_scr", [2, 17, 128, 1024], F32).ap()
    if stage in ("dbgHY", "simHY"):
        g.dbg_hyo = dt("dbg_hyo", [BPC, 8, 128, SEQ], BF16, kind="ExternalOutput").ap()


def phase_HY(nc, g, stage, nb=BPC):
    with contextlib.ExitStack() as st:
        S = g.S
        S.barrier()
        sb = lambda name, shape, dtype: st.enter_context(nc.sbuf_tensor("hys_" + name, shape, dtype))
        PB = st.enter_context(nc.psum_tensor("hpbf", [128, 1024], BF16))
        P = [None] + [st.enter_context(nc.psum_tensor("hps%d" % i, [128, 512], F32)) for i in range(1, 8)]
        identb = sb("identb", [128, 128], BF16)
        blk = sb("blk", [128, 128], BF16)
        alt = sb("alt", [128, 16], BF16)
        altinv = sb("altinv", [1, SEQ], BF16)
        vec = sb("vec", [128, 2, 8], F32)
        S.dma(lambda e: e.dma_start(out=identb[:], in_=g.ident_bf[:]), writes=["identb"])
        S.dma(lambda e: e.dma_start(out=blk[:], in_=g.rw_blk[:]), writes=["blk"], q="pool")
        S.dma(lambda e: e.dma_start(out=alt[:], in_=g.hy_alt[:]), writes=["alt"])
        S.dma(lambda e: e.dma_start(out=altinv[:], in_=g.hy_altinv[:]), writes=["altinv"])
        S.dma(lambda e: e.dma_start(out=vec[:], in_=g.hy_vec[:]), writes=["vec"])
        uTM = sb("uTM", [128, 16, 512], BF16)
        fw = [[sb("fw%d_%d" % (i, j), [128, 16, 128], BF16) for j in range(2)] for i in range(2)]
        Pc = sb("Pc", [128, 1024], F32)
        Ps = sb("Ps", [128, 1024], F32)
        rot = [0]
        ROT = [1, 2, 3, 4, 5, 6, 7]

        def nbk():
            rot[0] = (rot[0] + 1) % len(ROT)
            return ROT[rot[0]]

        def forward_dft(ncols, sink):
            for fc in range(16):
                bufi = fc % 2
                for part, src in ((0, g.hy_Cfw), (1, g.hy_Sfw)):
                    S.dma(lambda e, part=part, bufi=bufi, src=src, fc=fc: e.dma_start(out=fw[part][bufi][:], in_=src[fc]),
                          writes=["fw%d_%d" % (part, bufi)])
                kc_, ks_ = nbk(), nbk()
                for part, bk in ((0, kc_), (1, ks_)):
                    for tb in range(16):
                        S.op("pe", lambda e, part=part, bufi=bufi, tb=tb, bk=bk: e.matmul(
                            P[bk][:, 0:ncols], lhsT=fw[part][bufi][:, tb, :], rhs=uTM[:, tb, 0:ncols], start=(tb == 0), stop=(tb == 15)),
                            reads=["fw%d_%d" % (part, bufi), "uTM"], writes=["P%d" % bk])
                sink(fc, kc_, ks_)
            kn = nbk()
            for tb in range(16):
                S.op("pe", lambda e, tb=tb, kn=kn: e.matmul(P[kn][0:1, 0:ncols], lhsT=alt[:, tb:tb + 1], rhs=uTM[:, tb, 0:ncols],
                                                            start=(tb == 0), stop=(tb == 15)),
                     reads=["alt", "uTM"], writes=["P%d" % kn])
            sink(16, kn, None)

        with contextlib.ExitStack() as stF:
            sbF = lambda name, shape, dtype: stF.enter_context(nc.sbuf_tensor("hyf_" + name, shape, dtype))
            featsT = sbF("featsT", [33, SEQ], F32)
            w1 = sbF("w1", [33, 64], F32)
            w2 = sbF("w2", [64, 64], F32)
            w3 = sbF("w3", [64, 64], F32)
            w4 = sbF("w4", [64, 2048], F32)
            fb = sbF("fb", [64, 4], F32)
            fbc = sbF("fbc", [64, 3], F32)
            hA = sbF("hA", [64, SEQ], F32)
            hB = sbF("hB", [64, SEQ], F32)
            win = sbF("win", [128, 16, 512], F32)
            for tname, tt, src in (("featsT", featsT, g.hy_featsT), ("w1", w1, g.hy_w1), ("w2", w2, g.hy_w2), ("w3", w3, g.hy_w3),
                                   ("w4", w4, g.hy_w4), ("fb", fb, g.hy_fb)):
                S.dma(lambda e, tt=tt, src=src: e.dma_start(out=tt[:], in_=src[:]), writes=[tname])
            for i in range(3):
                S.op("dve", lambda e, i=i: e.tensor_scalar(out=fbc[:, i:i + 1], in0=fb[:, 1 + i:2 + i], scalar1=fb[:, 0:1], scalar2=None,
                                                           op0=ALU.mult), reads=["fb"], writes=["fbc"])
            layers = ((w1, "w1", featsT, "featsT", 33, hA, "hA"), (w2, "w2", hA, "hA", 64, hB, "hB"), (w3, "w3", hB, "hB", 64, hA, "hA"))
            for li, (w, wn, src, sn, kk, dst, dn) in enumerate(layers):
                for ti in range(4):
                    bk = nbk()
                    S.op("pe", lambda e, w=w, src=src, kk=kk, ti=ti, bk=bk: e.matmul(P[bk][0:64, 0:512], lhsT=w[0:kk, :], rhs=src[0:kk, ti * 512:(ti + 1) * 512],
                                                                                   start=True, stop=True), reads=[wn, sn], writes=["P%d" % bk])
                    MAGIC = 12582912.0
                    S.op("dve", lambda e, ti=ti, bk=bk, li=li: e.tensor_scalar(out=win[0:64, 0, :], in0=P[bk][0:64, 0:512], scalar1=fb[:, 0:1], scalar2=fbc[:, li:li + 1],
                                                                               op0=ALU.mult, op1=ALU.add), reads=["P%d" % bk, "fb", "fbc"], writes=["win"])
                    S.op("dve", lambda e: e.tensor_scalar(out=win[0:64, 1, :], in0=win[0:64, 0, :], scalar1=1.0 / TWO_PI, scalar2=MAGIC,
                                                          op0=ALU.mult, op1=ALU.add), reads=["win"], writes=["win"])
                    S.op("dve", lambda e: e.tensor_scalar(out=win[0:64, 1, :], in0=win[0:64, 1, :], scalar1=-MAGIC, scalar2=None,
                                                          op0=ALU.add), reads=["win"], writes=["win"])
                    S.op("dve", lambda e: e.scalar_tensor_tensor(out=win[0:64, 0, :], in0=win[0:64, 1, :], scalar=-TWO_PI, in1=win[0:64, 0, :],
                                                                 op0=ALU.mult, op1=ALU.add), reads=["win"], writes=["win"])
                    S.op("dve", lambda e: e.tensor_scalar(out=win[0:64, 0, :], in0=win[0:64, 0, :], scalar1=-3.1415925, scalar2=3.1415925,
                                                          op0=ALU.max, op1=ALU.min), reads=["win"], writes=["win"])
                    S.op("act", lambda e, dst=dst, ti=ti: e.activation(out=dst[:, ti * 512:(ti + 1) * 512], in_=win[0:64, 0, :], func=AF.Sin),
                         reads=["win"], writes=[dn])
            h3 = hA
            for cg in range(4):
                S.dma(lambda e, cg=cg: e.dma_start(out=win[:], in_=g.hy_win[:, :, cg * 512:(cg + 1) * 512]), writes=["win"])
                for tb in range(16):
                    bk = nbk()
                    S.op("pe", lambda e, tb=tb, cg=cg, bk=bk: e.matmul(P[bk][:, 0:512], lhsT=h3[:, tb * 128:(tb + 1) * 128], rhs=w4[:, cg * 512:(cg + 1) * 512],
                                                                      start=True, stop=True), reads=["hA", "w4"], writes=["P%d" % bk])
                    S.op("dve", lambda e, tb=tb, bk=bk: e.tensor_tensor(out=uTM[:, tb, :], in0=P[bk][:, 0:512], in1=win[:, tb, :], op=ALU.mult),
                         reads=["P%d" % bk, "win"], writes=["uTM"])
                if cg >= 2:
                    S.op("dve", lambda e: e.memset(uTM[0:1, 0, :], 0.0), writes=["uTM"])
                bwd = cg >= 2
                c0 = (cg % 2) * 512

                def sink(fc, kc_, ks_, bwd=bwd, c0=c0):
                    rows = slice(0, 128) if fc < 16 else slice(0, 1)
                    for part, bk, sign in ((0, kc_, 1.0), (1, ks_, -1.0 if not bwd else 1.0)):
                        if bk is None:
                            continue
                        dst = Pc if part == 0 else Ps
                        dn = "Pc" if part == 0 else "Ps"
                        if not bwd:
                            S.op("act", lambda e, dst=dst, bk=bk, sign=sign, rows=rows: e.activation(out=dst[rows, 0:512], in_=P[bk][rows, 0:512], func=AF.Copy, scale=sign),
                                 reads=["P%d" % bk], writes=[dn])
                        else:
                            S.dma(lambda e, dst=dst, part=part, fc=fc, rows=rows, c0=c0: e.dma_start(out=dst[rows, 0:512], in_=g.specT[part, fc, rows, c0:c0 + 512]),
                                  reads=["spec"], writes=[dn])
                            S.op("dve", lambda e, dst=dst, bk=bk, rows=rows: e.tensor_tensor(out=dst[rows, 0:512], in0=dst[rows, 0:512], in1=P[bk][rows, 0:512], op=ALU.add),
                                 reads=["P%d" % bk, dn], writes=[dn])
                        S.dma(lambda e, dst=dst, part=part, fc=fc, rows=rows, c0=c0: e.dma_start(out=g.specT[part, fc, rows, c0:c0 + 512], in_=dst[rows, 0:512]),
                              reads=[dn], writes=["spec"])
                forward_dft(512, sink)

        S.barrier()
        X0 = sb("X0", [128, SEQ], F32)
        X1 = sb("X1", [128, SEQ], F32)
        VV = sb("VV", [128, SEQ], F32)
        UU = [sb("UU%d" % i, [128, SEQ], F32) for i in range(4)]
        Ub = sb("Ub", [128, SEQ], BF16)
        Yr = sb("Yr", [128, 17, 512], BF16)
        Yi = sb("Yi", [128, 17, 512], BF16)
        Tr = sb("Tr", [128, 512], F32)
        Ti = sb("Ti", [128, 512], F32)
        inv = [[sb("inv%d_%d" % (i, j), [128, SEQ], BF16) for j in range(2)] for i in range(2)]
        osb = sb("osb", [128, SEQ], BF16)
        for b in range(nb):
            for cgp in range(2):
                S.flush()
                for j in range(4):
                    ch = cgp * 4 + j
                    S.dma(lambda e, b=b, ch=ch: e.dma_start(out=X1[:], in_=g.zT[b, 8 + ch, :, CTX:TB]), reads=["scr"], writes=["X1"])
                    S.dma(lambda e, b=b, ch=ch: e.dma_start(out=VV[:], in_=g.zT[b, 16 + ch, :, CTX:TB]), reads=["scr"], writes=["VV"])
                    S.op("dve", lambda e, j=j: e.tensor_tensor(out=UU[j][:], in0=VV[:], in1=X1[:], op=ALU.mult), reads=["VV", "X1"], writes=["UU%d" % j])
                    S.op("pool", lambda e, j=j: e.tensor_copy(out=Ub[:], in_=UU[j][:]), reads=["UU%d" % j], writes=["Ub"])
                    for tb in range(16):
                        q = tb % 8
                        S.op("pe", lambda e, tb=tb, q=q: e.transpose(PB[:, q * 128:(q + 1) * 128], Ub[:, tb * 128:(tb + 1) * 128], identb[:]),
                             reads=["Ub", "identb"], writes=["PB"])
                        if q == 7:
                            S.op("act", lambda e, tb=tb, j=j: e.copy(out=uTM[:, tb - 7:tb + 1, j * 128:(j + 1) * 128],
                                                                    in_=PB[:, :].rearrange("p (q f) -> p q f", f=128)), reads=["PB"], writes=["uTM"])

                def sink2(fc, kc_, ks_, cgp=cgp):
                    rows = slice(0, 128) if fc < 16 else slice(0, 1)
                    c0 = cgp * 512
                    S.dma(lambda e, fc=fc, rows=rows, c0=c0: e.dma_start(out=Tr[rows, :], in_=g.specT[0, fc, rows, c0:c0 + 512]), reads=["spec"], writes=["Tr"])
                    if ks_ is not None:
                        S.dma(lambda e, fc=fc, rows=rows, c0=c0: e.dma_start(out=Ti[rows, :], in_=g.specT[1, fc, rows, c0:c0 + 512]), reads=["spec"], writes=["Ti"])
                    if ks_ is None:
                        S.op("dve", lambda e, rows=rows, kc_=kc_: e.tensor_tensor(out=Yr[rows, 16, :], in0=P[kc_][rows, 0:512], in1=Tr[rows, :], op=ALU.mult),
                             reads=["P%d" % kc_, "Tr"], writes=["Yr"])
                        return
                    S.op("act", lambda e, kc_=kc_: e.copy(out=Pc[:, 0:512], in_=P[kc_][:, 0:512]), reads=["P%d" % kc_], writes=["Pc"])
                    S.op("act", lambda e, ks_=ks_: e.copy(out=Ps[:, 0:512], in_=P[ks_][:, 0:512]), reads=["P%d" % ks_], writes=["Ps"])
                    S.op("dve", lambda e: e.tensor_tensor(out=Pc[:, 512:1024], in0=Pc[:, 0:512], in1=Tr[:], op=ALU.mult), reads=["Pc", "Tr"], writes=["Pc"])
                    S.op("pool", lambda e: e.tensor_tensor(out=Ps[:, 512:1024], in0=Ps[:, 0:512], in1=Ti[:], op=ALU.mult), reads=["Ps", "Ti"], writes=["Ps"])
                    S.op("dve", lambda e, fc=fc: e.tensor_tensor(out=Yr[:, fc, :], in0=Pc[:, 512:1024], in1=Ps[:, 512:1024], op=ALU.add), reads=["Pc", "Ps"], writes=["Yr"])
                    S.op("dve", lambda e: e.tensor_tensor(out=Ps[:, 512:1024], in0=Ps[:, 0:512], in1=Tr[:], op=ALU.mult), reads=["Ps", "Tr"], writes=["Ps"])
                    S.op("pool", lambda e: e.tensor_tensor(out=Pc[:, 512:1024], in0=Pc[:, 0:512], in1=Ti[:], op=ALU.mult), reads=["Pc", "Ti"], writes=["Pc"])
                    S.op("dve", lambda e, fc=fc: e.tensor_tensor(out=Yi[:, fc, :], in0=Ps[:, 512:1024], in1=Pc[:, 512:1024], op=ALU.subtract), reads=["Pc", "Ps"], writes=["Yi"])
                forward_dft(512, sink2)
                acc = [[nbk() for _ in range(4)] for _ in range(1)]
                for j in range(4):
                    ch = cgp * 4 + j
                    banks = [1, 2, 3, 4]
                    for fc in range(16):
                        bufi = fc % 2
                        if j == 0 or True:
                            for part, src in ((0, g.hy_Cinv), (1, g.hy_Sinv)):
                                S.dma(lambda e, part=part, bufi=bufi, src=src, fc=fc: e.dma_start(out=inv[part][bufi][:], in_=src[fc * 128:(fc + 1) * 128, :]),
                                      writes=["inv%d_%d" % (part, bufi)])
                        for ti in range(4):
                            bk = banks[ti]
                            S.op("pe", lambda e, fc=fc, j=j, ti=ti, bk=bk, bufi=bufi: e.matmul(
                                P[bk][:, 0:512], lhsT=Yr[:, fc, j * 128:(j + 1) * 128], rhs=inv[0][bufi][:, ti * 512:(ti + 1) * 512], start=(fc == 0), stop=False),
                                reads=["Yr", "inv0_%d" % bufi], writes=["P%d" % bk])
                            S.op("pe", lambda e, fc=fc, j=j, ti=ti, bk=bk, bufi=bufi: e.matmul(
                                P[bk][:, 0:512], lhsT=Yi[:, fc, j * 128:(j + 1) * 128], rhs=inv[1][bufi][:, ti * 512:(ti + 1) * 512], start=False, stop=False),
                                reads=["Yi", "inv1_%d" % bufi], writes=["P%d" % bk])
                    for ti in range(4):
                        bk = banks[ti]
                        S.op("pe", lambda e, j=j, ti=ti, bk=bk: e.matmul(
                            P[bk][:, 0:512], lhsT=Yr[0:1, 16, j * 128:(j + 1) * 128], rhs=altinv[0:1, ti * 512:(ti + 1) * 512], start=False, stop=True),
                            reads=["Yr", "altinv"], writes=["P%d" % bk])
                    S.dma(lambda e, b=b, ch=ch: e.dma_start(out=X0[:], in_=g.zT[b, ch, :, CTX:TB]), reads=["scr"], writes=["X0"])
                    for ti in range(4):
                        bk = banks[ti]
                        ts_ = slice(ti * 512, (ti + 1) * 512)
                        S.op("dve", lambda e, j=j, bk=bk, ts_=ts_, ch=ch: e.scalar_tensor_tensor(out=UU[j][:, ts_], in0=UU[j][:, ts_], scalar=vec[:, 0, ch:ch + 1], in1=P[bk][:, 0:512],
                                                                                           op0=ALU.mult, op1=ALU.add), reads=["UU%d" % j, "vec", "P%d" % bk], writes=["UU%d" % j])
                    S.op("dve", lambda e, j=j: e.tensor_tensor(out=UU[j][:], in0=UU[j][:], in1=X0[:], op=ALU.mult), reads=["UU%d" % j, "X0"], writes=["UU%d" % j])
                    S.op("pool", lambda e, j=j: e.tensor_tensor(out=Ub[:], in0=UU[j][:], in1=UU[j][:], op=ALU.mult), reads=["UU%d" % j], writes=["Ub"])
                    for ti in range(4):
                        bk = 5 + ti % 3
                        ts_ = slice(ti * 512, (ti + 1) * 512)
                        S.op("pe", lambda e, bk=bk, ts_=ts_: e.matmul(P[bk][:, 0:512], lhsT=blk[:], rhs=Ub[:, ts_], start=True, stop=True), reads=["blk", "Ub"], writes=["P%d" % bk])
                        S.op("dve", lambda e, bk=bk, ts_=ts_: e.tensor_scalar(out=X0[:, ts_], in0=P[bk][:, 0:512], scalar1=1.0 / 64, scalar2=EPS, op0=ALU.mult, op1=ALU.add),
                             reads=["P%d" % bk], writes=["X0"])
                    S.op("act", lambda e: e.sqrt(out=X0[:], in_=X0[:]), reads=["X0"], writes=["X0"])
                    S.op("dve", lambda e: e.reciprocal(out=X0[:], in_=X0[:]), reads=["X0"], writes=["X0"])
                    S.op("dve", lambda e, j=j, ch=ch: e.scalar_tensor_tensor(out=osb[:], in0=UU[j][:], scalar=vec[:, 1, ch:ch + 1], in1=X0[:], op0=ALU.mult, op1=ALU.mult),
                         reads=["UU%d" % j, "vec", "X0"], writes=["osb"])
                    S.dma(lambda e, b=b, ch=ch: e.dma_start(out=g.mixT[b, ch], in_=osb[:]), reads=["osb"], writes=["mixscr"])
                    if stage in ("dbgHY", "simHY"):
                        S.dma(lambda e, b=b, ch=ch: e.dma_start(out=g.dbg_hyo[b, ch], in_=osb[:]), reads=["osb"])
        S.flush()


NE = 32
SPARSE = True
SBR = 512
NSB = (NTOK_ := BPC * SEQ) * 4 // SBR + 32
TBK = 1024
NTOK = BPC * SEQ


def declare_F(nc, g, stage):
    dt = nc.dram_tensor
    inp = lambda name, shape, dtype=F32: dt(name, list(shape), dtype, kind="ExternalInput").ap()
    g.out_w = inp("out_w", [D, D])
    g.g2row = inp("g2row", [1, D])
    g.router_w = inp("router_w", [D, NE])
    g.router_b = inp("router_b", [1, NE])
    g.ex_wg = inp("ex_w_gate", [NE, D, D])
    g.ex_wu = inp("ex_w_up", [NE, D, D])
    g.ex_wd = inp("ex_w_down", [NE, D, D])
    g.ex_bgu = inp("ex_bgu", [NE, 128, 2, KC])
    g.ex_bd = inp("ex_b_down", [NE, D])
    g.sp_tri = inp("sp_tri", [128, 128])
    g.sp_ones = inp("sp_ones", [128, 128])
    g.sp_sidx = inp("sp_sidx", [128, NSB])
    g.sp_base4 = inp("sp_base4", [128, 2, KC])
    g.sp_piota = inp("sp_piota", [128, 1])
    g.h2tm_scr = dt("h2tm_scr", [NTOK, D], BF16).ap()
    g.xs_scr = dt("xs_scr", [NSB * SBR, D], BF16).ap()
    g.ys_scr = dt("ys_scr", [NSB * SBR, D], F32).ap()
    kd = {"kind": "ExternalOutput"} if F_DBG else {}
    g.x1_scr = dt("x1_scr", [NTOK, D], F32, **kd).ap()
    g.h2T_scr = dt("h2T_scr", [KC, 128, NTOK], BF16, **kd).ap()
    if stage == "simF" or F_DBG:
        g.dbg_gates = dt("dbg_gates", [128, NTOK // 128, NE], F32, kind="ExternalOutput").ap()


def phase_F(nc, g, stage, nexp=NE, nblk=NTOK // TBK):
    with contextlib.ExitStack() as st:
        S = g.S
        S.barrier()
        sb = lambda name, shape, dtype: st.enter_context(nc.sbuf_tensor("fs_" + name, shape, dtype))
        PB = st.enter_context(nc.psum_tensor("fpbf", [128, 1024], BF16))
        P = [None] + [st.enter_context(nc.psum_tensor("fps%d" % i, [128, 512], F32)) for i in range(1, 8)]
        rot = [0]
        ROT = [1, 2, 3, 4, 5, 6, 7]

        def nbk():
            rot[0] = (rot[0] + 1) % len(ROT)
            return ROT[rot[0]]
        gates = sb("gates", [128, NTOK // 128, NE], F32)
        NTT = NTOK // 128
        if SPARSE:
            LG = sb("LG", [128, NTT, NE], F32)
            MK = sb("MK", [128, NTT, NE], F32)
            TOP8 = sb("TOP8", [128, NTT, 8], F32)
        identf = sb("identf", [128, 128], F32)
        fg = sb("fg", [128, D], F32)
        m5t = None if SPARSE else sb("m5t", [128, D], F32)
        S.dma(lambda e: e.dma_start(out=identf[:], in_=g.ident_f[:]), writes=["identf"])
        S.dma(lambda e: e.dma_start(out=fg[:], in_=g.final_g[0, :].partition_broadcast(128)), writes=["fg"])
        with contextlib.ExitStack() as s1:
            sb1 = lambda name, shape, dtype: s1.enter_context(nc.sbuf_tensor("f1_" + name, shape, dtype))
            ow = sb1("ow", [128, KC, D], BF16)
            for kc in range(KC):
                S.dma(lambda e, kc=kc: e.dma_start(out=ow[:, kc, :], in_=g.out_w[kc * 128:(kc + 1) * 128, :]), writes=["ow"], q="pool")
            rw_ = sb1("rw", [128, KC, NE], F32)
            S.dma(lambda e: e.dma_start(out=rw_[:], in_=g.router_w.rearrange("(kc p) e -> p kc e", p=128)), writes=["rw"])
            rb = sb1("rb", [128, NE], F32)
            S.dma(lambda e: e.dma_start(out=rb[:], in_=g.router_b[0, :].partition_broadcast(128)), writes=["rb"])
            m2 = sb1("m2", [128, D], F32)
            A3 = sb1("A3", [128, D], F32)
            B3 = sb1("B3", [128, D], F32)
            mixt = [sb1("mixt%d" % i, [128, KC, 128], BF16) for i in range(2)]
            xt = [sb1("xt%d" % i, [128, D], F32) for i in range(2)]
            h2 = sb1("h2", [128, D], F32)
            junk = sb1("junk", [128, D], F32)
            h2Tf = sb1("h2Tf", [128, KC, 128], F32)
            h2Tb = sb1("h2Tb", [128, KC, 128], BF16)
            sm = sb1("sm", [128, 16], F32)
            lg = sb1("lg", [128, NE], F32)
            ex = sb1("ex", [128, NE], F32)
            it = 0
            for b in range(BPC):
                S.dma(lambda e, b=b: e.dma_start(out=m2[:], in_=g.mod_row[b, 2 * D:3 * D].partition_broadcast(128)), reads=["modrow"], writes=["m2"])
                S.dma(lambda e, b=b: e.dma_start(out=A3[:], in_=g.mod_row[b, 4 * D:5 * D].partition_broadcast(128)), reads=["modrow"], writes=["A3"])
                S.dma(lambda e, b=b: e.dma_start(out=B3[:], in_=g.mod_row[b, 3 * D:4 * D].partition_broadcast(128)), reads=["modrow"], writes=["B3"])
                S.dma(lambda e: e.dma_start(out=junk[:], in_=g.g2row[0, :].partition_broadcast(128)), writes=["junk"])
                S.op("dve", lambda e: e.scalar_tensor_tensor(out=A3[:], in0=A3[:], scalar=1.0, in1=junk[:], op0=ALU.add, op1=ALU.mult), reads=["A3", "junk"], writes=["A3"])
                for t in range(SEQ // 128):
                    if t % 8 == 0:
                        S.flush()
                    p = it % 2
                    it += 1
                    gt = b * (SEQ // 128) + t
                    X, MX = "xt%d" % p, "mixt%d" % p
                    S.dma(lambda e, p=p, b=b, t=t: e.dma_start(out=xt[p][:], in_=g.x[b, t * 128:(t + 1) * 128, :]), writes=[X])
                    S.dma(lambda e, p=p, b=b, t=t: e.dma_start(out=mixt[p][:], in_=g.mixT[b, :, :, t * 128:(t + 1) * 128].rearrange("k p t -> p k t")),
                          reads=["mixscr"], writes=[MX])
                    for dj in range(4):
                        bk = nbk()
                        for kc in range(KC):
                            S.op("pe", lambda e, p=p, kc=kc, dj=dj, bk=bk: e.matmul(P[bk][:, 0:512], lhsT=mixt[p][:, kc, :], rhs=ow[:, kc, dj * 512:(dj + 1) * 512],
                                                                                   start=(kc == 0), stop=(kc == KC - 1)), reads=[MX, "ow"], writes=["P%d" % bk])
                        ds_ = slice(dj * 512, (dj + 1) * 512)
                        S.op("dve", lambda e, bk=bk, ds_=ds_: e.tensor_tensor(out=h2[:, ds_], in0=P[bk][:, 0:512], in1=m2[:, ds_], op=ALU.mult), reads=["P%d" % bk, "m2"], writes=["h2"])
                    S.op("dve", lambda e, p=p: e.tensor_tensor(out=xt[p][:], in0=xt[p][:], in1=h2[:], op=ALU.add), reads=[X, "h2"], writes=[X])
                    S.dma(lambda e, p=p, gt=gt: e.dma_start(out=g.x1_scr[gt * 128:(gt + 1) * 128, :], in_=xt[p][:]), reads=[X], writes=["x1scr"])
                    S.op("act", lambda e, p=p: e.activation(out=junk[:], in_=xt[p][:], func=AF.Square), reads=[X], writes=["junk"])
                    S.op("dve", lambda e: e.tensor_reduce(out=sm[:, 0:1], in_=junk[:], axis=AX.X, op=ALU.add), reads=["junk"], writes=["sm"])
                    S.op("dve", lambda e: e.tensor_scalar(out=sm[:, 0:1], in0=sm[:, 0:1], scalar1=1.0 / D, scalar2=EPS, op0=ALU.mult, op1=ALU.add), reads=["sm"], writes=["sm"])
                    S.op("act", lambda e: e.sqrt(out=sm[:, 0:1], in_=sm[:, 0:1]), reads=["sm"], writes=["sm"])
                    S.op("dve", lambda e: e.reciprocal(out=sm[:, 0:1], in_=sm[:, 0:1]), reads=["sm"], writes=["sm"])
                    S.op("dve", lambda e, p=p: e.scalar_tensor_tensor(out=h2[:], in0=xt[p][:], scalar=sm[:, 0:1], in1=A3[:], op0=ALU.mult, op1=ALU.mult),
                         reads=[X, "sm", "A3"], writes=["h2"])
                    S.op("dve", lambda e: e.tensor_tensor(out=h2[:], in0=h2[:], in1=B3[:], op=ALU.add), reads=["h2", "B3"], writes=["h2"])
                    for kc in range(KC):
                        bk = nbk()
                        S.op("pe", lambda e, kc=kc, bk=bk: e.transpose(P[bk][:, 0:128], h2[:, kc * 128:(kc + 1) * 128], identf[:]), reads=["h2", "identf"], writes=["P%d" % bk])
                        S.op("act", lambda e, kc=kc, bk=bk: e.copy(out=h2Tf[:, kc, :], in_=P[bk][:, 0:128]), reads=["P%d" % bk], writes=["h2Tf"])
                    S.op("pool", lambda e: e.tensor_copy(out=h2Tb[:], in_=h2Tf[:]), reads=["h2Tf"], writes=["h2Tb"])
                    S.dma(lambda e, gt=gt: e.dma_start(out=g.h2T_scr[:, :, gt * 128:(gt + 1) * 128].rearrange("k p t -> p k t"), in_=h2Tb[:]), reads=["h2Tb"], writes=["h2scr"])
                    bk = nbk()
                    for kc in range(KC):
                        S.op("pe", lambda e, kc=kc, bk=bk: e.matmul(P[bk][:, 0:NE], lhsT=h2Tf[:, kc, :], rhs=rw_[:, kc, :], start=(kc == 0), stop=(kc == KC - 1)),
                             reads=["h2Tf", "rw"], writes=["P%d" % bk])
                    S.op("dve", lambda e, bk=bk: e.tensor_tensor(out=lg[:], in0=P[bk][:, 0:NE], in1=rb[:], op=ALU.add), reads=["P%d" % bk, "rb"], writes=["lg"])
                    S.op("dve", lambda e: e.max(out=sm[:, 8:16], in_=lg[:]), reads=["lg"], writes=["sm"])
                    S.op("dve", lambda e: e.tensor_scalar(out=sm[:, 1:2], in0=sm[:, 8:9], scalar1=-1.0, scalar2=None, op0=ALU.mult), reads=["sm"], writes=["sm"])
                    S.op("act", lambda e: e.activation(out=ex[:], in_=lg[:], func=AF.Exp, bias=sm[:, 1:2]), reads=["lg", "sm"], writes=["ex"])
                    S.op("dve", lambda e: e.scalar_tensor_tensor(out=ex[:], in0=lg[:], scalar=sm[:, 11:12], in1=ex[:], op0=ALU.is_ge, op1=ALU.mult),
                         reads=["lg", "sm", "ex"], writes=["ex"])
                    S.op("dve", lambda e: e.tensor_reduce(out=sm[:, 2:3], in_=ex[:], axis=AX.X, op=ALU.add), reads=["ex"], writes=["sm"])
                    S.op("dve", lambda e: e.reciprocal(out=sm[:, 2:3], in_=sm[:, 2:3]), reads=["sm"], writes=["sm"])
                    S.op("dve", lambda e, gt=gt: e.tensor_scalar(out=gates[:, gt, :], in0=ex[:], scalar1=sm[:, 2:3], scalar2=None, op0=ALU.mult), reads=["ex", "sm"], writes=["gates"])
                    if SPARSE:
                        S.op("pool", lambda e, gt=gt: e.tensor_copy(out=LG[:, gt, :], in_=lg[:]), reads=["lg"], writes=["LG"])
                        S.op("pool", lambda e, gt=gt: e.tensor_copy(out=TOP8[:, gt, :], in_=sm[:, 8:16]), reads=["sm"], writes=["TOP8"])
                        S.op("dve", lambda e, gt=gt: e.tensor_scalar(out=MK[:, gt, :], in0=lg[:], scalar1=sm[:, 11:12], scalar2=None, op0=ALU.is_ge), reads=["lg", "sm"], writes=["MK"])
                        S.op("pool", lambda e: e.tensor_copy(out=junk[:].bitcast(BF16)[:, 0:D], in_=h2[:]), reads=["h2"], writes=["junk"])
                        S.dma(lambda e, gt=gt: e.dma_start(out=g.h2tm_scr[gt * 128:(gt + 1) * 128, :], in_=junk[:].bitcast(BF16)[:, 0:D]), reads=["junk"], writes=["h2tm"])
        if stage == "simF" or F_DBG:
            S.dma(lambda e: e.dma_start(out=g.dbg_gates[:], in_=gates[:]), reads=["gates"])
        if SPARSE:
            moe_sparse(nc, g, S, sb, P, PB, nbk, gates, LG, MK, TOP8, identf, fg)
            return
        S.barrier()
        NT = TBK // 128
        hb = sb("hb", [128, KC, TBK], BF16)
        actT = sb("actT", [128, KC, TBK], BF16)
        acc = sb("acc", [128, NT, D], F32)
        wg = [sb("wg%d" % i, [128, KC, 128], BF16) for i in range(2)]
        wu = [sb("wu%d" % i, [128, KC, 128], BF16) for i in range(2)]
        wd = [sb("wd%d" % i, [128, KC, 256], BF16) for i in range(2)]
        bgu = sb("bgu", [128, 2, KC], F32)
        bdr = sb("bdr", [1, D], BF16)
        ones = sb("ones", [1, 128], BF16)
        Gt, Ut, St = sb("Gt", [128, 512], F32), sb("Ut", [128, 512], F32), sb("St", [128, 512], F32)
        S.op("pool", lambda e: e.memset(ones[:], 1.0), writes=["ones"])
        acc_x = sb("acc_x", [128, D], F32)
        fsm = sb("fsm", [128, 2], F32)
        wv = lambda w, e_: w[e_].rearrange("(kc p) f -> p kc f", p=128)
        wi = 0
        for blk_i in range(nblk):
            t0 = blk_i * TBK
            S.dma(lambda e, t0=t0: e.dma_start(out=hb[:], in_=g.h2T_scr[:, :, t0:t0 + TBK].rearrange("k p t -> p k t")), reads=["h2scr"], writes=["hb"])
            S.op("pool", lambda e: e.memset(acc[:], 0.0), writes=["acc"])
            bb = t0 // SEQ
            S.dma(lambda e, bb=bb: e.dma_start(out=m5t[:], in_=g.mod_row[bb, 5 * D:6 * D].partition_broadcast(128)), reads=["modrow"], writes=["m5t"])
            for ex_i in range(nexp):
                if ex_i % 4 == 0:
                    S.flush()
                S.dma(lambda e, ex_i=ex_i: e.dma_start(out=bgu[:], in_=g.ex_bgu[ex_i]), writes=["bgu"])
                S.dma(lambda e, ex_i=ex_i: e.dma_start(out=bdr[:], in_=g.ex_bd[ex_i:ex_i + 1, :]), writes=["bdr"], q="pool")
                for fs in range(D // 128):
                    p = wi % 2
                    wi += 1
                    S.dma(lambda e, p=p, ex_i=ex_i, fs=fs: e.dma_start(out=wg[p][:], in_=wv(g.ex_wg, ex_i)[:, :, fs * 128:(fs + 1) * 128]), writes=["wg%d" % p], q="pool")
                    S.dma(lambda e, p=p, ex_i=ex_i, fs=fs: e.dma_start(out=wu[p][:], in_=wv(g.ex_wu, ex_i)[:, :, fs * 128:(fs + 1) * 128]), writes=["wu%d" % p], q="pool")
                    for j in range(1):
                        fc = fs
                        for tg in range(TBK // 512):
                            ts_ = slice(tg * 512, (tg + 1) * 512)
                            kg, ku = nbk(), nbk()
                            for kc in range(KC):
                                S.op("pe", lambda e, p=p, j=j, kc=kc, ts_=ts_, kg=kg: e.matmul(P[kg][:, 0:512], lhsT=wg[p][:, kc, j * 128:(j + 1) * 128], rhs=hb[:, kc, ts_],
                                                                                              start=(kc == 0), stop=(kc == KC - 1)), reads=["wg%d" % p, "hb"], writes=["P%d" % kg])
                            for kc in range(KC):
                                S.op("pe", lambda e, p=p, j=j, kc=kc, ts_=ts_, ku=ku: e.matmul(P[ku][:, 0:512], lhsT=wu[p][:, kc, j * 128:(j + 1) * 128], rhs=hb[:, kc, ts_],
                                                                                              start=(kc == 0), stop=(kc == KC - 1)), reads=["wu%d" % p, "hb"], writes=["P%d" % ku])
                            S.op("dve", lambda e, kg=kg, fc=fc: e.tensor_scalar(out=Gt[:], in0=P[kg][:, 0:512], scalar1=bgu[:, 0, fc:fc + 1], scalar2=7.0, op0=ALU.add, op1=ALU.min),
                                 reads=["P%d" % kg, "bgu"], writes=["Gt"])
                            S.op("act", lambda e: e.activation(out=St[:], in_=Gt[:], func=AF.Sigmoid, scale=1.702), reads=["Gt"], writes=["St"])
                            S.op("dve", lambda e, ku=ku, fc=fc: e.tensor_scalar(out=Ut[:], in0=P[ku][:, 0:512], scalar1=bgu[:, 1, fc:fc + 1], scalar2=7.0, op0=ALU.add, op1=ALU.min),
                                 reads=["P%d" % ku, "bgu"], writes=["Ut"])
                            S.op("dve", lambda e: e.tensor_scalar(out=Ut[:], in0=Ut[:], scalar1=-7.0, scalar2=1.0, op0=ALU.max, op1=ALU.add), reads=["Ut"], writes=["Ut"])
                            S.op("dve", lambda e: e.tensor_tensor(out=Gt[:], in0=Gt[:], in1=St[:], op=ALU.mult), reads=["Gt", "St"], writes=["Gt"])
                            S.op("dve", lambda e, fc=fc, ts_=ts_: e.tensor_tensor(out=actT[:, fc, ts_], in0=Ut[:], in1=Gt[:], op=ALU.mult), reads=["Ut", "Gt"], writes=["actT"])
                for dj in range(8):
                    p = dj % 2
                    S.dma(lambda e, p=p, ex_i=ex_i, dj=dj: e.dma_start(out=wd[p][:], in_=wv(g.ex_wd, ex_i)[:, :, dj * 256:(dj + 1) * 256]), writes=["wd%d" % p], q="pool")
                    ds_ = slice(dj * 256, (dj + 1) * 256)
                    for tt in range(NT):
                        bk = nbk()
                        gt = blk_i * NT + tt
                        for fc in range(KC):
                            S.op("pe", lambda e, p=p, fc=fc, tt=tt, bk=bk: e.matmul(P[bk][:, 0:256], lhsT=actT[:, fc, tt * 128:(tt + 1) * 128], rhs=wd[p][:, fc, :],
                                                                                   start=(fc == 0), stop=False), reads=["actT", "wd%d" % p], writes=["P%d" % bk])
                        S.op("pe", lambda e, bk=bk, ds_=ds_: e.matmul(P[bk][:, 0:256], lhsT=ones[0:1, :], rhs=bdr[0:1, ds_], start=False, stop=True),
                             reads=["ones", "bdr"], writes=["P%d" % bk])
                        S.op("dve", lambda e, bk=bk, tt=tt, ds_=ds_, gt=gt, ex_i=ex_i: e.scalar_tensor_tensor(
                            out=acc[:, tt, ds_], in0=P[bk][:, 0:256], scalar=gates[:, gt, ex_i:ex_i + 1], in1=acc[:, tt, ds_], op0=ALU.mult, op1=ALU.add),
                            reads=["P%d" % bk, "gates", "acc"], writes=["acc"])
            S.flush()
            for tt in range(NT):
                gt = blk_i * NT + tt
                b = gt // (SEQ // 128)
                S.dma(lambda e, gt=gt: e.dma_start(out=acc_x[:], in_=g.x1_scr[gt * 128:(gt + 1) * 128, :]), reads=["x1scr"], writes=["accx"])
                S.op("dve", lambda e, tt=tt, b=b: e.tensor_tensor(out=acc[:, tt, :], in0=acc[:, tt, :], in1=m5t[:], op=ALU.mult), reads=["acc", "m5t"], writes=["acc"])
                S.op("dve", lambda e, tt=tt: e.tensor_tensor(out=acc[:, tt, :], in0=acc[:, tt, :], in1=acc_x[:], op=ALU.add), reads=["acc", "accx"], writes=["acc"])
                S.op("act", lambda e, tt=tt: e.activation(out=acc_x[:], in_=acc[:, tt, :], func=AF.Square), reads=["acc"], writes=["accx"])
                S.op("dve", lambda e: e.tensor_reduce(out=fsm[:, 0:1], in_=acc_x[:], axis=AX.X, op=ALU.add), reads=["accx"], writes=["fsm"])
                S.op("dve", lambda e: e.tensor_scalar(out=fsm[:, 0:1], in0=fsm[:, 0:1], scalar1=1.0 / D, scalar2=EPS, op0=ALU.mult, op1=ALU.add), reads=["fsm"], writes=["fsm"])
                S.op("act", lambda e: e.sqrt(out=fsm[:, 0:1], in_=fsm[:, 0:1]), reads=["fsm"], writes=["fsm"])
                S.op("dve", lambda e: e.reciprocal(out=fsm[:, 0:1], in_=fsm[:, 0:1]), reads=["fsm"], writes=["fsm"])
                S.op("dve", lambda e, tt=tt: e.scalar_tensor_tensor(out=acc[:, tt, :], in0=acc[:, tt, :], scalar=fsm[:, 0:1], in1=fg[:], op0=ALU.mult, op1=ALU.mult),
                     reads=["acc", "fsm", "fg"], writes=["acc"])
                t_in_b = gt % (SEQ // 128)
                S.dma(lambda e, tt=tt, b=b, t_in_b=t_in_b: e.dma_start(out=g.out[b, t_in_b * 128:(t_in_b + 1) * 128, :], in_=acc[:, tt, :]), reads=["acc"])
        S.flush()


def moe_sparse(nc, g, S, sb, P, PB, nbk, gates, LG, MK, TOP8, identf, fg):
    U32 = mybir.dt.uint32
    MAGIC = 12582912.0
    NTT = NTOK // 128
    S.barrier()
    stA = contextlib.ExitStack()
    sbp = sb
    sb = lambda name, shape, dtype: stA.enter_context(nc.sbuf_tensor("spa_" + name, shape, dtype))
    base4, piota = sbp("base4", [128, 2, KC], F32), sbp("piota", [128, 1], F32)
    exs = sbp("exs", [128, NSB], F32)
    identb = sbp("sidentb", [128, 128], BF16)
    D4i, GS4 = sbp("D4i", [128, NTOK // 128, 4], I32), sbp("GS4", [128, NTOK // 128, 4], F32)
    tri, onesf = sb("tri", [128, 128], F32), sb("onesf", [128, 128], F32)
    S.dma(lambda e: e.dma_start(out=identb[:], in_=g.ident_bf[:]), writes=["identb"])
    sidx = sb("sidx", [128, NSB], F32)
    for tname, tt, src in (("tri", tri, g.sp_tri), ("onesf", onesf, g.sp_ones), ("sidx", sidx, g.sp_sidx), ("base4", base4, g.sp_base4), ("piota", piota, g.sp_piota)):
        S.dma(lambda e, tt=tt, src=src: e.dma_start(out=tt[:], in_=src[:]), writes=[tname])
    cnt, nbt, pend, pstart = sb("cnt", [128, NE], F32), sb("nbt", [128, NE], F32), sb("pend", [128, NE], F32), sb("pstart", [128, NE], F32)
    t3 = sb("t3", [128, NSB, NE], F32)
    D4f = sb("D4f", [128, NTT, 4], F32)
    pos, oh = sb("pos", [128, NE], F32), sb("oh", [128, NE], F32)
    zt = sb("zt", [128, D], BF16)
    S.op("pool", lambda e: e.memset(zt[:], 0.0), writes=["zt"])
    for i in range(NSB * SBR // 128):
        S.dma(lambda e, i=i: e.dma_start(out=g.xs_scr[i * 128:(i + 1) * 128, :], in_=zt[:]), reads=["zt"], writes=["xs"])
    bk = nbk()
    for g2 in range(NTT):
        S.op("pe", lambda e, g2=g2, bk=bk: e.matmul(P[bk][:, 0:NE], lhsT=onesf[:], rhs=MK[:, g2, :], start=(g2 == 0), stop=(g2 == NTT - 1)), reads=["onesf", "MK"], writes=["P%d" % bk])
    S.op("dve", lambda e, bk=bk: e.tensor_copy(out=cnt[:], in_=P[bk][:, 0:NE]), reads=["P%d" % bk], writes=["cnt"])
    S.op("dve", lambda e: e.tensor_scalar(out=nbt[:], in0=cnt[:], scalar1=1.0 / SBR, scalar2=(SBR - 1.0) / SBR - 0.5 + 0.5 / SBR, op0=ALU.mult, op1=ALU.add), reads=["cnt"], writes=["nbt"])
    S.op("dve", lambda e: e.tensor_scalar(out=nbt[:], in0=nbt[:], scalar1=MAGIC, scalar2=None, op0=ALU.add), reads=["nbt"], writes=["nbt"])
    S.op("dve", lambda e: e.tensor_scalar(out=nbt[:], in0=nbt[:], scalar1=-MAGIC, scalar2=None, op0=ALU.add), reads=["nbt"], writes=["nbt"])
    S.op("dve", lambda e: e.tensor_tensor_scan(out=pend[:], data0=onesf[:, 0:NE], data1=nbt[:], initial=0.0, op0=ALU.mult, op1=ALU.add), reads=["onesf", "nbt"], writes=["pend"])
    S.op("dve", lambda e: e.tensor_tensor(out=pstart[:], in0=pend[:], in1=nbt[:], op=ALU.subtract), reads=["pend", "nbt"], writes=["pstart"])
    S.op("dve", lambda e: e.tensor_scalar(out=pstart[:], in0=pstart[:], scalar1=float(SBR), scalar2=None, op0=ALU.mult), reads=["pstart"], writes=["pstart"])
    S.op("dve", lambda e: e.tensor_tensor(out=t3[:], in0=pend[:].unsqueeze(1).to_broadcast([128, NSB, NE]), in1=sidx[:].unsqueeze(2).to_broadcast([128, NSB, NE]), op=ALU.is_le),
         reads=["pend", "sidx"], writes=["t3"])
    S.op("dve", lambda e: e.tensor_reduce(out=exs[:], in_=t3[:], axis=AX.X, op=ALU.add), reads=["t3"], writes=["exs"])
    S.op("dve", lambda e: e.tensor_scalar(out=exs[:], in0=exs[:], scalar1=float(NE - 1), scalar2=None, op0=ALU.min), reads=["exs"], writes=["exs"])
    for gt in range(NTT):
        bk = nbk()
        S.op("pe", lambda e, gt=gt, bk=bk: e.matmul(P[bk][:, 0:NE], lhsT=tri[:], rhs=MK[:, gt, :], start=True, stop=(gt == 0)), reads=["tri", "MK"], writes=["P%d" % bk])
        for g2 in range(gt):
            S.op("pe", lambda e, g2=g2, bk=bk, gt=gt: e.matmul(P[bk][:, 0:NE], lhsT=onesf[:], rhs=MK[:, g2, :], start=False, stop=(g2 == gt - 1)), reads=["onesf", "MK"], writes=["P%d" % bk])
        S.op("dve", lambda e, bk=bk: e.tensor_tensor(out=pos[:], in0=P[bk][:, 0:NE], in1=pstart[:], op=ALU.add), reads=["P%d" % bk, "pstart"], writes=["pos"])
        for k in range(4):
            S.op("dve", lambda e, gt=gt, k=k: e.tensor_scalar(out=oh[:], in0=LG[:, gt, :], scalar1=TOP8[:, gt, k:k + 1], scalar2=None, op0=ALU.is_equal), reads=["LG", "TOP8"], writes=["oh"])
            S.op("dve", lambda e: e.tensor_tensor(out=t3[:, 0, :], in0=oh[:], in1=pos[:], op=ALU.mult), reads=["oh", "pos"], writes=["t3"])
            S.op("dve", lambda e, gt=gt, k=k: e.tensor_reduce(out=D4f[:, gt, k:k + 1], in_=t3[:, 0, :], axis=AX.X, op=ALU.add), reads=["t3"], writes=["D4f"])
            S.op("dve", lambda e, gt=gt: e.tensor_tensor(out=t3[:, 1, :], in0=oh[:], in1=gates[:, gt, :], op=ALU.mult), reads=["oh", "gates"], writes=["t3"])
            S.op("dve", lambda e, gt=gt, k=k: e.tensor_reduce(out=GS4[:, gt, k:k + 1], in_=t3[:, 1, :], axis=AX.X, op=ALU.add), reads=["t3"], writes=["GS4"])
    S.op("dve", lambda e: e.tensor_copy(out=D4i[:], in_=D4f[:]), reads=["D4f"], writes=["D4i"])
    hrow = [sb("hrow%d" % i, [128, D], BF16) for i in range(2)]
    for gt in range(NTT):
        p = gt % 2
        S.dma(lambda e, p=p, gt=gt: e.dma_start(out=hrow[p][:], in_=g.h2tm_scr[gt * 128:(gt + 1) * 128, :]), reads=["h2tm"], writes=["hrow%d" % p])
        for k in range(4):
            S.dma(lambda e, p=p, gt=gt, k=k: e.indirect_dma_start(out=g.xs_scr[:, :], out_offset=bass.IndirectOffsetOnAxis(ap=D4i[:, gt, k:k + 1].bitcast(U32), axis=0),
                                                                 in_=hrow[p][:], in_offset=None), reads=["hrow%d" % p, "D4i", "xs"], writes=["xs"], q="pool")
    S.barrier()
    S.flush()
    stA.close()
    stB = contextlib.ExitStack()
    sb = lambda name, shape, dtype: stB.enter_context(nc.sbuf_tensor("spb_" + name, shape, dtype))
    idxf, idxi = sb("idxf", [128, 2, KC], F32), [sb("idxi%d" % i, [128, 2, KC], I32) for i in range(2)]
    bidf, bidi = sb("bidf", [128, 1], F32), [sb("bidi%d" % i, [128, 1], I32) for i in range(2)]
    xrow = [sb("xrow%d" % i, [128, D], BF16) for i in range(2)]
    hbT = sb("hbT", [128, KC, SBR], BF16)
    actT = sb("sactT", [128, KC, SBR], BF16)
    wg = [sb("swg%d" % i, [128, KC, 1024], BF16) for i in range(2)]
    wu = [sb("swu%d" % i, [128, KC, 1024], BF16) for i in range(2)]
    bgu = [sb("sbgu%d" % i, [128, 2 * KC], F32) for i in range(2)]
    Gt, Ut, St = sb("sGt", [128, 512], F32), sb("sUt", [128, 512], F32), sb("sSt", [128, 512], F32)
    yst = [sb("yst%d" % i, [128, 512], F32) for i in range(2)]
    wg4 = g.ex_wg.rearrange("e d (q f) -> (e d q) f", f=1024)
    wu4 = g.ex_wu.rearrange("e d (q f) -> (e d q) f", f=1024)
    wd4 = g.ex_wd.rearrange("e d (q f) -> (e d q) f", f=1024)
    bgu2 = g.ex_bgu.rearrange("e p a k -> (e p) (a k)")
    wi = 0
    yi = 0
    for s_ in range(NSB):
        if s_ % 2 == 0:
            S.flush()
        ip = s_ % 2
        S.op("dve", lambda e, s_=s_: e.scalar_tensor_tensor(out=idxf[:], in0=exs[:, s_:s_ + 1].unsqueeze(2).to_broadcast([128, 2, KC]), scalar=4096.0, in1=base4[:], op0=ALU.mult, op1=ALU.add),
             reads=["exs", "base4"], writes=["idxf"])
        S.op("dve", lambda e, ip=ip: e.tensor_copy(out=idxi[ip][:], in_=idxf[:]), reads=["idxf"], writes=["idxi%d" % ip])
        S.op("dve", lambda e, s_=s_: e.scalar_tensor_tensor(out=bidf[:], in0=exs[:, s_:s_ + 1], scalar=128.0, in1=piota[:], op0=ALU.mult, op1=ALU.add), reads=["exs", "piota"], writes=["bidf"])
        S.op("dve", lambda e, ip=ip: e.tensor_copy(out=bidi[ip][:], in_=bidf[:]), reads=["bidf"], writes=["bidi%d" % ip])
        S.dma(lambda e, ip=ip: e.indirect_dma_start(out=bgu[ip][:], out_offset=None, in_=bgu2[:, :], in_offset=bass.IndirectOffsetOnAxis(ap=bidi[ip][:, 0:1].bitcast(U32), axis=0)),
              reads=["bidi%d" % ip], writes=["sbgu%d" % ip], q="pool")
        for tt in range(SBR // 128):
            xp = tt % 2
            S.dma(lambda e, xp=xp, s_=s_, tt=tt: e.dma_start(out=xrow[xp][:], in_=g.xs_scr[s_ * SBR + tt * 128: s_ * SBR + (tt + 1) * 128, :]), reads=["xs"], writes=["xrow%d" % xp])
            for half in range(2):
                for q in range(8):
                    kc = half * 8 + q
                    S.op("pe", lambda e, xp=xp, kc=kc, q=q: e.transpose(PB[:, q * 128:(q + 1) * 128], xrow[xp][:, kc * 128:(kc + 1) * 128], identb[:]), reads=["xrow%d" % xp, "identb"], writes=["PB"])
                S.op("act", lambda e, half=half, tt=tt: e.copy(out=hbT[:, half * 8:(half + 1) * 8, tt * 128:(tt + 1) * 128], in_=PB[:, :].rearrange("p (q f) -> p q f", f=128)),
                     reads=["PB"], writes=["hbT"])
        for fs in range(2):
            p = wi % 2
            wi += 1
            for kc in range(KC):
                S.dma(lambda e, p=p, ip=ip, fs=fs, kc=kc: e.indirect_dma_start(out=wg[p][:, kc, :], out_offset=None, in_=wg4[:, :],
                                                                            in_offset=bass.IndirectOffsetOnAxis(ap=idxi[ip][:, fs, kc:kc + 1].bitcast(U32), axis=0)),
                      reads=["idxi%d" % ip], writes=["swg%d" % p], q="pool")
                S.dma(lambda e, p=p, ip=ip, fs=fs, kc=kc: e.indirect_dma_start(out=wu[p][:, kc, :], out_offset=None, in_=wu4[:, :],
                                                                            in_offset=bass.IndirectOffsetOnAxis(ap=idxi[ip][:, fs, kc:kc + 1].bitcast(U32), axis=0)),
                      reads=["idxi%d" % ip], writes=["swu%d" % p], q="pool")
            for j in range(8):
                fc = fs * 8 + j
                kg, ku = nbk(), nbk()
                for kc in range(KC):
                    S.op("pe", lambda e, p=p, j=j, kc=kc, kg=kg: e.matmul(P[kg][:, 0:SBR], lhsT=wg[p][:, kc, j * 128:(j + 1) * 128], rhs=hbT[:, kc, :], start=(kc == 0), stop=(kc == KC - 1)),
                         reads=["swg%d" % p, "hbT"], writes=["P%d" % kg])
                for kc in range(KC):
                    S.op("pe", lambda e, p=p, j=j, kc=kc, ku=ku: e.matmul(P[ku][:, 0:SBR], lhsT=wu[p][:, kc, j * 128:(j + 1) * 128], rhs=hbT[:, kc, :], start=(kc == 0), stop=(kc == KC - 1)),
                         reads=["swu%d" % p, "hbT"], writes=["P%d" % ku])
                S.op("dve", lambda e, kg=kg, fc=fc, ip=ip: e.tensor_scalar(out=Gt[:], in0=P[kg][:, 0:SBR], scalar1=bgu[ip][:, fc:fc + 1], scalar2=7.0, op0=ALU.add, op1=ALU.min),
                     reads=["P%d" % kg, "sbgu%d" % ip], writes=["sGt"])
                S.op("act", lambda e: e.activation(out=St[:], in_=Gt[:], func=AF.Sigmoid, scale=1.702), reads=["sGt"], writes=["sSt"])
                S.op("dve", lambda e, ku=ku, fc=fc, ip=ip: e.tensor_scalar(out=Ut[:], in0=P[ku][:, 0:SBR], scalar1=bgu[ip][:, KC + fc:KC + fc + 1], scalar2=7.0, op0=ALU.add, op1=ALU.min),
                     reads=["P%d" % ku, "sbgu%d" % ip], writes=["sUt"])
                S.op("dve", lambda e: e.tensor_scalar(out=Ut[:], in0=Ut[:], scalar1=-7.0, scalar2=1.0, op0=ALU.max, op1=ALU.add), reads=["sUt"], writes=["sUt"])
                S.op("dve", lambda e: e.tensor_tensor(out=Gt[:], in0=Gt[:], in1=St[:], op=ALU.mult), reads=["sGt", "sSt"], writes=["sGt"])
                S.op("dve", lambda e, fc=fc: e.tensor_tensor(out=actT[:, fc, :], in0=Ut[:], in1=Gt[:], op=ALU.mult), reads=["sUt", "sGt"], writes=["sactT"])
        for dj in range(2):
            p = wi % 2
            wi += 1
            for fc in range(KC):
                S.dma(lambda e, p=p, ip=ip, dj=dj, fc=fc: e.indirect_dma_start(out=wg[p][:, fc, :], out_offset=None, in_=wd4[:, :],
                                                                            in_offset=bass.IndirectOffsetOnAxis(ap=idxi[ip][:, dj, fc:fc + 1].bitcast(U32), axis=0)),
                      reads=["idxi%d" % ip], writes=["swg%d" % p], q="pool")
            for hh in range(2):
                for tt in range(SBR // 128):
                    bk = nbk()
                    for fc in range(KC):
                        S.op("pe", lambda e, p=p, fc=fc, tt=tt, bk=bk, hh=hh: e.matmul(P[bk][:, 0:512], lhsT=actT[:, fc, tt * 128:(tt + 1) * 128], rhs=wg[p][:, fc, hh * 512:(hh + 1) * 512],
                                                                                      start=(fc == 0), stop=(fc == KC - 1)), reads=["sactT", "swg%d" % p], writes=["P%d" % bk])
                    yp = yi % 2
                    yi += 1
                    c0 = dj * 1024 + hh * 512
                    S.op("act", lambda e, yp=yp, bk=bk: e.copy(out=yst[yp][:], in_=P[bk][:, 0:512]), reads=["P%d" % bk], writes=["yst%d" % yp])
                    S.dma(lambda e, yp=yp, s_=s_, tt=tt, c0=c0: e.dma_start(out=g.ys_scr[s_ * SBR + tt * 128: s_ * SBR + (tt + 1) * 128, c0:c0 + 512], in_=yst[yp][:]),
                          reads=["yst%d" % yp], writes=["ys"])
    S.barrier()
    S.flush()
    stB.close()
    stC = contextlib.ExitStack()
    sb = lambda name, shape, dtype: stC.enter_context(nc.sbuf_tensor("spc_" + name, shape, dtype))
    acc = sb("cacc", [128, D], F32)
    yrow = [sb("yrow%d" % i, [128, D], F32) for i in range(2)]
    x1t = sb("x1t", [128, D], F32)
    m5t = sb("cm5t", [128, D], F32)
    bdall = sb("bdall", [NE, D], F32)
    gT = sb("gT", [NE, 128], F32)
    fsm = sb("cfsm", [128, 2], F32)
    S.dma(lambda e: e.dma_start(out=bdall[:], in_=g.ex_bd[:, :]), writes=["bdall"])
    yk = 0
    for gt in range(NTT):
        if gt % 8 == 0:
            S.flush()
        b = gt // (SEQ // 128)
        if gt % (SEQ // 128) == 0:
            S.dma(lambda e, b=b: e.dma_start(out=m5t[:], in_=g.mod_row[b, 5 * D:6 * D].partition_broadcast(128)), reads=["modrow"], writes=["cm5t"])
        S.dma(lambda e, gt=gt: e.dma_start(out=x1t[:], in_=g.x1_scr[gt * 128:(gt + 1) * 128, :]), reads=["x1scr"], writes=["x1t"])
        bk = nbk()
        S.op("pe", lambda e, gt=gt, bk=bk: e.transpose(P[bk][0:NE, 0:128], gates[:, gt, :], identf[:]), reads=["gates", "identf"], writes=["P%d" % bk])
        S.op("act", lambda e, bk=bk: e.copy(out=gT[:], in_=P[bk][0:NE, 0:128]), reads=["P%d" % bk], writes=["gT"])
        for dj in range(4):
            bk = nbk()
            S.op("pe", lambda e, dj=dj, bk=bk: e.matmul(P[bk][:, 0:512], lhsT=gT[:], rhs=bdall[:, dj * 512:(dj + 1) * 512], start=True, stop=True), reads=["gT", "bdall"], writes=["P%d" % bk])
            S.op("act", lambda e, dj=dj, bk=bk: e.copy(out=acc[:, dj * 512:(dj + 1) * 512], in_=P[bk][:, 0:512]), reads=["P%d" % bk], writes=["cacc"])
        for k in range(4):
            yp = yk % 2
            yk += 1
            S.dma(lambda e, yp=yp, gt=gt, k=k: e.indirect_dma_start(out=yrow[yp][:], out_offset=None, in_=g.ys_scr[:, :],
                                                                   in_offset=bass.IndirectOffsetOnAxis(ap=D4i[:, gt, k:k + 1].bitcast(U32), axis=0)),
                  reads=["ys", "D4i"], writes=["yrow%d" % yp], q="pool")
            S.op("dve", lambda e, yp=yp, gt=gt, k=k: e.scalar_tensor_tensor(out=acc[:], in0=yrow[yp][:], scalar=GS4[:, gt, k:k + 1], in1=acc[:], op0=ALU.mult, op1=ALU.add),
                 reads=["yrow%d" % yp, "GS4", "cacc"], writes=["cacc"])
        S.op("dve", lambda e: e.tensor_tensor(out=acc[:], in0=acc[:], in1=m5t[:], op=ALU.mult), reads=["cacc", "cm5t"], writes=["cacc"])
        S.op("dve", lambda e: e.tensor_tensor(out=acc[:], in0=acc[:], in1=x1t[:], op=ALU.add), reads=["cacc", "x1t"], writes=["cacc"])
        S.op("act", lambda e: e.activation(out=x1t[:], in_=acc[:], func=AF.Square), reads=["cacc"], writes=["x1t"])
        S.op("dve", lambda e: e.tensor_reduce(out=fsm[:, 0:1], in_=x1t[:], axis=AX.X, op=ALU.add), reads=["x1t"], writes=["cfsm"])
        S.op("dve", lambda e: e.tensor_scalar(out=fsm[:, 0:1], in0=fsm[:, 0:1], scalar1=1.0 / D, scalar2=EPS, op0=ALU.mult, op1=ALU.add), reads=["cfsm"], writes=["cfsm"])
        S.op("act", lambda e: e.sqrt(out=fsm[:, 0:1], in_=fsm[:, 0:1]), reads=["cfsm"], writes=["cfsm"])
        S.op("dve", lambda e: e.reciprocal(out=fsm[:, 0:1], in_=fsm[:, 0:1]), reads=["cfsm"], writes=["cfsm"])
        S.op("dve", lambda e: e.scalar_tensor_tensor(out=x1t[:], in0=acc[:], scalar=fsm[:, 0:1], in1=fg[:], op0=ALU.mult, op1=ALU.mult), reads=["cacc", "cfsm", "fg"], writes=["x1t"])
        t_in_b = gt % (SEQ // 128)
        S.dma(lambda e, b=b, t_in_b=t_in_b: e.dma_start(out=g.out[b, t_in_b * 128:(t_in_b + 1) * 128, :], in_=x1t[:]), reads=["x1t"])
    S.barrier()
    S.flush()
    stC.close()
```
